# Optimizing a Trainium2 kernel written in Bass

```python
import jax, jax.numpy as jnp
from jax import lax
import numpy as np

D_MODEL = 4096
BATCH = 4
SEQ = 4096
DEPTH = 1

CTX_LEN = 256
GRID_W = 64

NA_HEADS = 16
NA_HEAD_DIM = 128
NA_WIDTH = NA_HEADS * NA_HEAD_DIM
WIN_H = 8
WIN_W = 16
SG_GROUPS = 16
SG_GROUP_DIM = 128
SG_WIDTH = SG_GROUPS * SG_GROUP_DIM
CHUNK = 128
MIX_WIDTH = NA_WIDTH + SG_WIDTH
IN_WIDTH = 3 * NA_WIDTH + 2 * SG_WIDTH
N_EXPERTS = 64
TOP_K = 8
N_GROUPS = 8
TOPK_GROUPS = 4
EXPERT_DIM = 512
SHARED_DIM = 512
ROUTED_SCALE = 2.5
EPS = 1e-6

kernel_name = "hybrid_na_sgu_moe_dit_layer"


def rmsnorm(x, g):
    xf = x.astype(jnp.float32)
    y = xf * lax.rsqrt(jnp.mean(xf * xf, axis=-1, keepdims=True) + EPS)
    return (y * g.astype(jnp.float32)).astype(x.dtype)


def layernorm(x, g, b):
    xf = x.astype(jnp.float32)
    mu = jnp.mean(xf, axis=-1, keepdims=True)
    var = jnp.mean(jnp.square(xf - mu), axis=-1, keepdims=True)
    y = (xf - mu) * lax.rsqrt(var + EPS)
    return (y * g.astype(jnp.float32) + b.astype(jnp.float32)).astype(x.dtype)


def modulate(h, shift, scale):
    return h * (1 + scale) + shift


def to_heads(t):
    return t.reshape(t.shape[0], t.shape[1], NA_HEADS, NA_HEAD_DIM)


def split_in_proj(h, w_in):
    proj = h @ w_in
    q, k, v, u, z = jnp.split(proj, [NA_WIDTH, 2 * NA_WIDTH, 3 * NA_WIDTH, 3 * NA_WIDTH + SG_WIDTH], axis=-1)
    return to_heads(q), to_heads(k), to_heads(v), jax.nn.gelu(u), jax.nn.gelu(z)


def context_kv(hc, w_in):
    kc, vc = jnp.split(hc @ w_in[:, NA_WIDTH:3 * NA_WIDTH], 2, axis=-1)
    return to_heads(kc), to_heads(vc)


def neighbourhood_attention(q, k, v, kc, vc, rpb):
    B, S, H, Dh = q.shape
    rows = S // GRID_W
    kh = min(WIN_H, rows)
    q = q * (Dh ** -0.5)
    qg = q.reshape(B, rows, GRID_W, H, Dh).transpose(1, 0, 3, 2, 4)
    kg = k.reshape(B, rows, GRID_W, H, Dh).transpose(0, 3, 1, 2, 4)
    vg = v.reshape(B, rows, GRID_W, H, Dh).transpose(0, 3, 1, 2, 4)
    kct = kc.transpose(0, 2, 1, 3)
    vct = vc.transpose(0, 2, 1, 3)
    cols = jnp.arange(GRID_W)
    col_start = jnp.clip(cols - WIN_W // 2, 0, GRID_W - WIN_W)
    col_idx = col_start[:, None] + jnp.arange(WIN_W)[None, :]
    col_off = col_idx - cols[:, None] + (WIN_W - 1)
    n_win = kh * WIN_W

    def row_block(args):
        r, q_r = args
        r0 = jnp.clip(r - kh // 2, 0, rows - kh)
        k_rows = lax.dynamic_slice_in_dim(kg, r0, kh, axis=2)
        v_rows = lax.dynamic_slice_in_dim(vg, r0, kh, axis=2)
        k_win = k_rows[:, :, :, col_idx, :]
        v_win = v_rows[:, :, :, col_idx, :]
        row_off = r0 + jnp.arange(kh) - r + (WIN_H - 1)
        bias = rpb[:, row_off[None, :, None], col_off[:, None, :]]
        s_win = jnp.einsum('bhqd,bhiqjd->bhqij', q_r, k_win) + bias[None]
        s_ctx = jnp.einsum('bhqd,bhcd->bhqc', q_r, kct)
        s = jnp.concatenate([s_win.reshape(B, H, GRID_W, n_win), s_ctx], axis=-1)
        p = jax.nn.softmax(s.astype(jnp.float32), axis=-1).astype(v.dtype)
        p_win = p[..., :n_win].reshape(B, H, GRID_W, kh, WIN_W)
        p_ctx = p[..., n_win:]
        return (jnp.einsum('bhqij,bhiqjd->bhqd', p_win, v_win)
                + jnp.einsum('bhqc,bhcd->bhqd', p_ctx, vct))

    out = lax.map(row_block, (jnp.arange(rows), qg))
    return out.transpose(1, 0, 3, 2, 4).reshape(B, S, H * Dh)


def context_attention(qc, kc, vc):
    B, C, H, Dh = qc.shape
    s = jnp.einsum('bqhd,bkhd->bhqk', qc * (Dh ** -0.5), kc)
    p = jax.nn.softmax(s.astype(jnp.float32), axis=-1).astype(vc.dtype)
    return jnp.einsum('bhqk,bkhd->bqhd', p, vc).reshape(B, C, H * Dh)


def spatial_gating(u, z, ln_g, ln_b, w_s, b_s):
    B, L, _ = z.shape
    z = layernorm(z, ln_g, ln_b)
    zc = z.reshape(B, L // CHUNK, CHUNK, SG_GROUPS, SG_GROUP_DIM)
    mixed = jnp.einsum('gpq,bnqgd->bnpgd', w_s, zc) + b_s.T[:, :, None]
    return u * mixed.reshape(B, L, SG_WIDTH)


def merge_heads(attn, sgu, g_na, g_sg, w_out):
    return jnp.concatenate([rmsnorm(attn, g_na), rmsnorm(sgu, g_sg)], axis=-1) @ w_out


def swiglu(t, w_gate, w_up, w_down):
    return (jax.nn.silu(t @ w_gate) * (t @ w_up)) @ w_down


def moe_ffn(x, w_router, router_bias, we_gate, we_up, we_down, ws_gate, ws_up, ws_down):
    B, L, D = x.shape
    t = x.reshape(B * L, D)
    n = t.shape[0]
    scores = jax.nn.sigmoid((t @ w_router).astype(jnp.float32))
    choice = scores + router_bias.astype(jnp.float32)
    grouped = choice.reshape(n, N_GROUPS, N_EXPERTS // N_GROUPS)
    group_score = lax.top_k(grouped, 2)[0].sum(axis=-1)
    _, top_groups = lax.top_k(group_score, TOPK_GROUPS)
    group_mask = jax.nn.one_hot(top_groups, N_GROUPS).sum(axis=1) > 0
    expert_mask = jnp.repeat(group_mask, N_EXPERTS // N_GROUPS, axis=1)
    _, top_idx = lax.top_k(jnp.where(expert_mask, choice, -jnp.inf), TOP_K)
    w = jnp.take_along_axis(scores, top_idx, axis=-1)
    w = w / jnp.sum(w, axis=-1, keepdims=True) * ROUTED_SCALE
    gates = jnp.sum(jax.nn.one_hot(top_idx, N_EXPERTS, dtype=jnp.float32) * w[..., None], axis=1).astype(x.dtype)
    out = swiglu(t, ws_gate, ws_up, ws_down)
    for e in range(N_EXPERTS):
        out = out + gates[:, e:e + 1] * swiglu(t, we_gate[e], we_up[e], we_down[e])
    return out.reshape(B, L, D)


def setup_inputs(seed: int = 0) -> dict:
    key = jax.random.key(seed)
    ks = iter(jax.random.split(key, 32))
    D, L = D_MODEL, DEPTH

    def nrm(shape, scale):
        return jax.random.normal(next(ks), shape, jnp.float32) * scale

    return {
        "x": nrm((BATCH, SEQ, D), 1.0),
        "c": nrm((BATCH, D), 1.0),
        "ctx": nrm((BATCH, CTX_LEN, D), 1.0),
        "c_ctx": nrm((D,), 1.0),
        "w_ada": nrm((L, D, 6 * D), 0.5 * D ** -0.5),
        "b_ada": nrm((L, 6 * D), 0.02),
        "norm_mix_g": 1.0 + nrm((L, D), 0.02),
        "norm_ffn_g": 1.0 + nrm((L, D), 0.02),
        "w_in": nrm((L, D, IN_WIDTH), D ** -0.5),
        "na_rpb": nrm((L, NA_HEADS, 2 * WIN_H - 1, 2 * WIN_W - 1), 0.1),
        "sg_ln_g": 1.0 + nrm((L, SG_WIDTH), 0.02),
        "sg_ln_b": nrm((L, SG_WIDTH), 0.02),
        "sg_w_s": nrm((L, SG_GROUPS, CHUNK, CHUNK), CHUNK ** -0.5),
        "sg_b_s": 1.0 + nrm((L, SG_GROUPS, CHUNK), 0.01),
        "out_g_na": 1.0 + nrm((L, NA_WIDTH), 0.02),
        "out_g_sg": 1.0 + nrm((L, SG_WIDTH), 0.02),
        "w_out": nrm((L, MIX_WIDTH, D), MIX_WIDTH ** -0.5),
        "w_router": nrm((L, D, N_EXPERTS), D ** -0.5),
        "router_bias": nrm((L, N_EXPERTS), 0.01),
        "we_gate": nrm((L, N_EXPERTS, D, EXPERT_DIM), D ** -0.5),
        "we_up": nrm((L, N_EXPERTS, D, EXPERT_DIM), D ** -0.5),
        "we_down": nrm((L, N_EXPERTS, EXPERT_DIM, D), EXPERT_DIM ** -0.5),
        "ws_gate": nrm((L, D, SHARED_DIM), D ** -0.5),
        "ws_up": nrm((L, D, SHARED_DIM), D ** -0.5),
        "ws_down": nrm((L, SHARED_DIM, D), SHARED_DIM ** -0.5),
        "final_g": 1.0 + nrm((D,), 0.02),
    }


def reference(x, c, ctx, c_ctx, w_ada, b_ada, norm_mix_g, norm_ffn_g, w_in, na_rpb,
              sg_ln_g, sg_ln_b, sg_w_s, sg_b_s, out_g_na, out_g_sg, w_out,
              w_router, router_bias, we_gate, we_up, we_down, ws_gate, ws_up, ws_down, final_g):
    h_ctx = ctx
    for l in range(DEPTH):
        update_ctx = l + 1 < DEPTH
        mod = (jax.nn.silu(c) @ w_ada[l] + b_ada[l])[:, None, :]
        sh_m, sc_m, g_m, sh_f, sc_f, g_f = jnp.split(mod, 6, axis=-1)
        mod_c = jax.nn.silu(c_ctx) @ w_ada[l] + b_ada[l]
        csh_m, csc_m, cg_m, csh_f, csc_f, cg_f = jnp.split(mod_c, 6, axis=-1)

        hx = modulate(rmsnorm(x, norm_mix_g[l]), sh_m, sc_m)
        hc = modulate(rmsnorm(h_ctx, norm_mix_g[l]), csh_m, csc_m)
        q, k, v, u, z = split_in_proj(hx, w_in[l])
        if update_ctx:
            qc, kc, vc, uc, zc = split_in_proj(hc, w_in[l])
        else:
            kc, vc = context_kv(hc, w_in[l])
        attn = neighbourhood_attention(q, k, v, kc, vc, na_rpb[l])
        sgu = spatial_gating(u, z, sg_ln_g[l], sg_ln_b[l], sg_w_s[l], sg_b_s[l])
        x = x + g_m * merge_heads(attn, sgu, out_g_na[l], out_g_sg[l], w_out[l])

        ffn_w = (w_router[l], router_bias[l], we_gate[l], we_up[l], we_down[l], ws_gate[l], ws_up[l], ws_down[l])
        hf = modulate(rmsnorm(x, norm_ffn_g[l]), sh_f, sc_f)
        x = x + g_f * moe_ffn(hf, *ffn_w)

        if update_ctx:
            attn_c = context_attention(qc, kc, vc)
            sgu_c = spatial_gating(uc, zc, sg_ln_g[l], sg_ln_b[l], sg_w_s[l], sg_b_s[l])
            h_ctx = h_ctx + cg_m * merge_heads(attn_c, sgu_c, out_g_na[l], out_g_sg[l], w_out[l])
            hcf = modulate(rmsnorm(h_ctx, norm_ffn_g[l]), csh_f, csc_f)
            h_ctx = h_ctx + cg_f * moe_ffn(hcf, *ffn_w)
    return rmsnorm(x, final_g)
```

```python
import contextlib
import numpy as np
import concourse.bass as bass
import concourse.mybir as mybir
from concourse.bass_utils import run_bass_kernel_spmd

F32 = mybir.dt.float32
BF16 = mybir.dt.bfloat16
AF = mybir.ActivationFunctionType
ALU = mybir.AluOpType
AX = mybir.AxisListType

D = 4096
NTOK = 2048
NPOS = 2816
NH = 16
NE = 64
EPS = 1e-6
TCLS = {0: 1, 1: 2, 14: 3, 15: 4}


class Eng:
    def __init__(s, name, h, sem):
        s.name, s.h, s.sem, s.n, s.seen = name, h, sem, 0, {}


class DSem:
    def __init__(s, sem):
        s.sem, s.n = sem, 0


class Dep:
    __slots__ = ("w", "r")

    def __init__(s):
        s.w = None
        s.r = {}


class K:
    def __init__(s, nc, es):
        s.nc, s.es = nc, es
        s.nsem = 0
        s.pe = Eng("pe", nc.tensor, s._sem())
        s.act = Eng("act", nc.scalar, s._sem())
        s.dve = Eng("dve", nc.vector, s._sem())
        s.pool = Eng("pool", nc.gpsimd, s._sem())
        s.sp = Eng("sp", nc.sync, s._sem())
        s.engs = [s.pe, s.act, s.dve, s.pool, s.sp]
        s.dsems = []

    def _sem(s):
        s.nsem += 1
        return s.es.enter_context(s.nc.semaphore(f"sem{s.nsem}"))

    def dsem(s):
        d = DSem(s._sem())
        s.dsems.append(d)
        return d

    def need(s, eng, st):
        if st is None:
            return
        obj, cnt = st
        if cnt <= 0 or eng.seen.get(obj, 0) >= cnt:
            return
        if obj is eng and eng.name in ("pe", "sp"):
            return
        if isinstance(obj, Eng):
            assert obj.n >= cnt, f"pending stamp on {obj.name}"
        eng.h.wait_ge(obj.sem, cnt)
        eng.seen[obj] = cnt

    def op(s, eng, reads, writes, fn, inc=True):
        for d in reads:
            s.need(eng, d.w)
        for d in writes:
            s.need(eng, d.w)
            for o, c in d.r.items():
                if o is not eng:
                    s.need(eng, (o, c))
        ins = fn(eng.h)
        if inc:
            eng.n += 1
            ins.then_inc(eng.sem, 1)
            st = (eng, eng.n)
        else:
            st = (eng, eng.n + 1)
        for d in reads:
            if d.r.get(eng, 0) < st[1]:
                d.r[eng] = st[1]
        for d in writes:
            d.w = st
            d.r = {}
        return ins

    def dma(s, q, out_ap, in_ap, reads, writes, ds):
        for d in reads:
            s.need(q, d.w)
        for d in writes:
            s.need(q, d.w)
            for o, c in d.r.items():
                s.need(q, (o, c))
        ins = q.h.dma_start(out=out_ap, in_=in_ap)
        ds.n += 16
        ins.then_inc(ds.sem, 16)
        for d in reads:
            d.r[ds] = ds.n
        for d in writes:
            d.w = (ds, ds.n)
            d.r = {}

    def idma(s, out_ap, out_off, in_ap, in_off, reads, writes, ds, **kw):
        q = s.pool
        for d in reads:
            s.need(q, d.w)
        for d in writes:
            s.need(q, d.w)
            for o, c in d.r.items():
                s.need(q, (o, c))
        ins = q.h.indirect_dma_start(out=out_ap, out_offset=out_off, in_=in_ap, in_offset=in_off, **kw)
        ds.n += 16
        ins.then_inc(ds.sem, 16)
        for d in reads:
            d.r[ds] = ds.n
        for d in writes:
            d.w = (ds, ds.n)
            d.r = {}

    def barrier(s):
        sts = [(e, e.n) for e in s.engs if e.n > 0] + [(d, d.n) for d in s.dsems if d.n > 0]
        for e in s.engs:
            for st in sts:
                s.need(e, st)


class Buf:
    def __init__(s, t):
        s.t = t
        s.d = Dep()


def build_nc(dbg=False, stages="0ABCDEFS", nblocks=4, nexp=65, sparse=True, ntiles=96):
    if sparse:
        nexp = 1
    nc = bass.Bass("TRN2", target_bir_lowering=False)
    es = contextlib.ExitStack()

    def din(name, shape, dt=F32):
        return nc.dram_tensor(name, list(shape), dt, kind="ExternalInput").ap()

    def dscr(name, shape, dt):
        return nc.dram_tensor(name, list(shape), dt, kind="ExternalOutput" if dbg else "Internal").ap()

    x_own = din("x_own", [NTOK, D])
    x_hc = din("x_hc", [768, D])
    cT = din("cT", [128, 32, 2])
    w_ada = din("w_ada", [D, 6 * D])
    b_ada2 = din("b_ada2", [2, 6 * D])
    gmix_fm = din("gmix_fm", [128, 32])
    gffn_fm = din("gffn_fm", [128, 32])
    w_in = din("w_in", [D, 10240])
    bias_tab = din("bias_tab", [NH, 128, 5, 5, 128])
    mask_tab = din("mask_tab", [128, 5, 5, 128])
    ln_g = din("ln_g", [1, 2048])
    ln_b = din("ln_b", [1, 2048])
    wsT = din("wsT", [128, 16, 128])
    bsT = din("bsT", [128, 16])
    gout = din("gout", [1, D])
    w_out = din("w_out", [D, D])
    w_router = din("w_router", [D, NE])
    rbias = din("rbias", [1, NE])
    we_gate = din("we_gate", [NE, D, 512])
    we_up = din("we_up", [NE, D, 512])
    we_down = din("we_down", [NE, 512, D])
    ws_gate = din("ws_gate", [D, 512])
    ws_up = din("ws_up", [D, 512])
    ws_down = din("ws_down", [512, D])
    final_g = din("final_g", [1, D])
    consts = din("consts", [128, 512])
    tri_in = din("tri_in", [128, 128])
    gffn_pk = din("gffn_pk", [128, 32])
    gffn_row = din("gffn_row", [1, D])
    out = nc.dram_tensor("out", [NTOK, D], F32, kind="ExternalOutput").ap()

    modrows = dscr("modrows", [2, 6 * D], F32)
    qT_s = dscr("qT_s", [NH, 128, NTOK], BF16)
    kT_s = dscr("kT_s", [NH, 128, NPOS], BF16)
    v_s = dscr("v_s", [NPOS, 2048], BF16)
    u_s = dscr("u_s", [NTOK, 2048], BF16)
    z_s = dscr("z_s", [NTOK, 2048], BF16)
    attn_s = dscr("attn_s", [NTOK, 2048], BF16)
    x1_s = dscr("x1_s", [NTOK, D], F32)
    gates_s = dscr("gates_s", [NTOK, 65], F32) if dbg else None
    I32 = mybir.dt.int32
    NSLOT = ntiles * 512
    hfn_s = dscr("hfn_s", [NTOK + 128, D], BF16)
    gts_s = dscr("gts_s", [NTOK + 128, NE], F32)
    tos_d = nc.dram_tensor("tos_d", [96 * 512, 16], I32, kind="ExternalOutput" if dbg else "Internal").ap()
    y_s = dscr("y_s", [8 * NTOK, D], F32)

    with es:
        k = K(nc, es)
        pe, act, dve, pool, sp = k.pe, k.act, k.dve, k.pool, k.sp

        uid = [0]

        def sb(name, shape, dt=F32, stack=es):
            uid[0] += 1
            return Buf(stack.enter_context(nc.sbuf_tensor(f"{name}_{uid[0]}", list(shape), dt)))

        def ps(name, shape, dt=F32, stack=es):
            uid[0] += 1
            return Buf(stack.enter_context(nc.psum_tensor(f"{name}_{uid[0]}", list(shape), dt)))

        NR = 4
        ring = [sb(f"ring{i}", [128, 4096 * 2], BF16) for i in range(NR)]
        ring_ds = [k.dsem() for _ in range(NR)]
        ring_i = [0]

        def ring_load(src_ap, view):
            i = ring_i[0] % NR
            ring_i[0] += 1
            a, b = view
            dst = ring[i].t[:, 0:a * b].rearrange("p (a b) -> p a b", a=a)
            k.dma(pool, dst, src_ap, [], [ring[i].d], ring_ds[i])
            return ring[i], dst

        actT = sb("actT", [128, 32, 512], BF16)
        modfm = sb("modfm", [128, 192, 2])
        gmm = sb("gmm", [128, 32, 2])
        gmf = sb("gmf", [128, 32])
        gmix_sb = sb("gmix_sb", [128, 32])
        gffn_sb = sb("gffn_sb", [128, 32])
        identf = sb("identf", [128, 128])
        identb = sb("identb", [128, 128], BF16)
        sm = sb("sm", [128, 16])
        misc_ds = k.dsem()

        k.op(pool, [], [identf.d], lambda h: h.memset(identf.t[:], 0.0))
        k.op(pool, [], [identf.d], lambda h: h.affine_select(
            out=identf.t[:], in_=identf.t[:], pattern=[[-1, 128]], compare_op=ALU.not_equal,
            fill=1.0, base=0, channel_multiplier=1))
        k.op(dve, [identf.d], [identb.d], lambda h: h.tensor_copy(out=identb.t[:], in_=identf.t[:]))
        k.dma(sp, gmix_sb.t[:], gmix_fm, [], [gmix_sb.d], misc_ds)
        k.dma(sp, gffn_sb.t[:], gffn_fm, [], [gffn_sb.d], misc_ds)

        def zero(ap):
            k.op(dve, [], [sm.d], lambda h: h.memset(ap, 0.0))

        def rstd_from_ssq(ssq_ap, out_ap, n, deps_r, deps_w):
            k.op(dve, deps_r, deps_w, lambda h: h.tensor_scalar(
                out=out_ap, in0=ssq_ap, scalar1=1.0 / n, scalar2=EPS, op0=ALU.mult, op1=ALU.add))
            k.op(act, deps_w, deps_w, lambda h: h.activation(out=out_ap, in_=out_ap, func=AF.Sqrt))
            k.op(dve, deps_w, deps_w, lambda h: h.reciprocal(out=out_ap, in_=out_ap))

        def mm_group(out_ap, pairs, psd, rdeps):
            n = len(pairs)
            for i, (l, r) in enumerate(pairs):
                k.op(pe, rdeps, [psd], lambda h, l=l, r=r, i=i: h.matmul(
                    out_ap, lhsT=l, rhs=r, start=(i == 0), stop=(i == n - 1)), inc=(i == n - 1))

        if "0" in stages:
            with contextlib.ExitStack() as st0:
                cs = sb("cs", [128, 32, 2], F32, st0)
                csb = sb("csb", [128, 32, 2], BF16, st0)
                id2 = sb("id2", [2, 2], F32, st0)
                bch = [sb(f"bch{i}", [2, 256], F32, st0) for i in range(2)]
                bch_ds = [k.dsem() for _ in range(2)]
                mev = [sb(f"mev{i}", [2, 256], F32, st0) for i in range(2)]
                mev_ds = [k.dsem() for _ in range(2)]
                pmod = [ps(f"pmod{i}", [2, 256], F32, st0) for i in range(2)]
                pfm = [ps(f"pfm{i}", [128, 2, 2], F32, st0) for i in range(2)]
                if sparse:
                    zrow = sb("zrow", [128, D], BF16, st0)
                    k.op(pool, [], [zrow.d], lambda h: h.memset(zrow.t[:], 0.0))
                    k.dma(sp, hfn_s[NTOK:NTOK + 128, :], zrow.t[:], [zrow.d], [], misc_ds)
                    zg = sb("zg", [128, NE], F32, st0)
                    k.op(pool, [], [zg.d], lambda h: h.memset(zg.t[:], 0.0))
                    k.dma(sp, gts_s[NTOK:NTOK + 128, :], zg.t[:], [zg.d], [], misc_ds)
                k.dma(sp, cs.t[:], cT, [], [cs.d], misc_ds)
                k.op(act, [cs.d], [csb.d], lambda h: h.activation(out=csb.t[:], in_=cs.t[:], func=AF.Silu))
                k.op(dve, [identf.d], [id2.d], lambda h: h.tensor_copy(out=id2.t[:], in_=identf.t[0:2, 0:2]))
                for cc in range(96):
                    c0 = cc * 256
                    i = cc % 2
                    slot, wv = ring_load(w_ada[:, c0:c0 + 256].rearrange("(k p) f -> p k f", p=128), (32, 256))
                    k.dma(sp, bch[i].t[:], b_ada2[:, c0:c0 + 256], [], [bch[i].d], bch_ds[i])
                    mm_group(pmod[i].t[:], [(csb.t[:, kk, :], wv[:, kk, :]) for kk in range(32)],
                             pmod[i].d, [csb.d, slot.d])
                    k.op(dve, [pmod[i].d, bch[i].d], [mev[i].d], lambda h, i=i: h.tensor_tensor(
                        out=mev[i].t[:], in0=pmod[i].t[:], in1=bch[i].t[:], op=ALU.add))
                    k.dma(sp, modrows[:, c0:c0 + 256], mev[i].t[:], [mev[i].d], [], mev_ds[i])
                    for hh in range(2):
                        mm_group(pfm[i].t[:, hh, :], [(mev[i].t[0:2, hh * 128:(hh + 1) * 128], id2.t[:])],
                                 pfm[i].d, [mev[i].d, id2.d])
                    k.op(act, [pfm[i].d], [modfm.d], lambda h, i=i, cc=cc: h.copy(
                        out=modfm.t[:, 2 * cc:2 * cc + 2, :], in_=pfm[i].t[:]))
                for j in range(2):
                    k.op(dve, [modfm.d, gmix_sb.d], [gmm.d], lambda h, j=j: h.scalar_tensor_tensor(
                        out=gmm.t[:, :, j], in0=modfm.t[:, 32:64, j], scalar=1.0, in1=gmix_sb.t[:],
                        op0=ALU.add, op1=ALU.mult))
                k.op(dve, [modfm.d, gffn_sb.d], [gmf.d], lambda h: h.scalar_tensor_tensor(
                    out=gmf.t[:], in0=modfm.t[:, 128:160, 0], scalar=1.0, in1=gffn_sb.t[:],
                    op0=ALU.add, op1=ALU.mult))
            k.barrier()

        if "A" in stages:
            with contextlib.ExitStack() as sA:
                xt = [sb(f"xt{i}", [128, D], F32, sA) for i in range(2)]
                xt_ds = [k.dsem() for _ in range(2)]
                junk = sb("junkA", [128, D], BF16, sA)
                stg = [sb(f"stgA{i}", [128, 4, 256], BF16, sA) for i in range(2)]
                stg_ds = [k.dsem() for _ in range(2)]
                ptr = [ps(f"ptrA{i}", [128, 4, 128], F32, sA) for i in range(2)]
                pmm = [ps(f"pmmA{i}", [128, 512], F32, sA) for i in range(4)]
                tix = [0]
                pix = [0]
                six = [0]
                for g in range(6):
                    ntok = 256 if g == 5 else 512
                    nst = ntok // 128
                    j = 1 if g == 5 else 0
                    for st in range(nst):
                        xb = xt[tix[0] % 2]
                        xds = xt_ds[tix[0] % 2]
                        tix[0] += 1
                        src = x_own[g * 512 + st * 128: g * 512 + (st + 1) * 128, :] if g < 4 else \
                            x_hc[(g - 4) * 512 + st * 128:(g - 4) * 512 + (st + 1) * 128, :]
                        k.dma(sp, xb.t[:], src, [], [xb.d], xds)
                        zero(sm.t[:, 0:1])
                        k.op(act, [xb.d], [junk.d, sm.d], lambda h, xb=xb: h.activation(
                            out=junk.t[:], in_=xb.t[:], func=AF.Square, accum_out=sm.t[:, 0:1]))
                        rstd_from_ssq(sm.t[:, 0:1], sm.t[:, 1:2], D, [sm.d], [sm.d])
                        k.op(dve, [sm.d, xb.d], [xb.d], lambda h, xb=xb: h.tensor_scalar(
                            out=xb.t[:], in0=xb.t[:], scalar1=sm.t[:, 1:2], scalar2=None, op0=ALU.mult))
                        for kk in range(32):
                            p = ptr[(kk // 4) % 2]
                            k.op(pe, [xb.d, identf.d], [p.d], lambda h, p=p, kk=kk, xb=xb: h.transpose(
                                p.t[:, kk % 4, :], xb.t[:, kk * 128:(kk + 1) * 128], identf.t[:]),
                                inc=(kk % 4 == 3))
                            if kk % 4 == 3:
                                for q4 in range(4):
                                    k4 = kk - 3 + q4
                                    k.op(act, [p.d, gmm.d, modfm.d], [actT.d], lambda h, p=p, q4=q4, k4=k4, st=st, j=j: h.activation(
                                        out=actT.t[:, k4, st * 128:(st + 1) * 128], in_=p.t[:, q4, :], func=AF.Identity,
                                        scale=gmm.t[:, k4, j:j + 1], bias=modfm.t[:, k4, j:j + 1]))
                    chunks = range(40) if g < 4 else range(8, 24)
                    for cc in chunks:
                        c0 = cc * 256
                        slot, wv = ring_load(w_in[:, c0:c0 + 256].rearrange("(k p) f -> p k f", p=128), (32, 256))
                        if cc < 16:
                            for hh in range(2):
                                head = (cc % 8) * 2 + hh
                                pm = pmm[pix[0] % 4]
                                pix[0] += 1
                                mm_group(pm.t[:, 0:ntok], [(wv[:, kk, hh * 128:(hh + 1) * 128], actT.t[:, kk, 0:ntok])
                                                            for kk in range(32)], pm.d, [slot.d, actT.d])
                                sg_ = stg[six[0] % 2]
                                sds = stg_ds[six[0] % 2]
                                six[0] += 1
                                sview = sg_.t[:].rearrange("p a b -> p (a b)")[:, 0:512]
                                if cc < 8:
                                    k.op(act, [pm.d], [sg_.d], lambda h, pm=pm, sview=sview, ntok=ntok: h.activation(
                                        out=sview[:, 0:ntok], in_=pm.t[:, 0:ntok], func=AF.Copy, scale=float(128 ** -0.5)))
                                    k.dma(sp, qT_s[head, :, g * 512:(g + 1) * 512], sview, [sg_.d], [], sds)
                                else:
                                    k.op(dve, [pm.d], [sg_.d], lambda h, pm=pm, sview=sview, ntok=ntok: h.tensor_copy(
                                        out=sview[:, 0:ntok], in_=pm.t[:, 0:ntok]))
                                    if g < 4:
                                        segs = [(0, 512, 256 + 512 * g)]
                                    elif g == 4:
                                        segs = [(0, 256, 0), (256, 512, 2304)]
                                    else:
                                        segs = [(0, 256, 2560)]
                                    for (a, b, p0) in segs:
                                        k.dma(sp, kT_s[head, :, p0:p0 + (b - a)], sview[:, a:b], [sg_.d], [], sds)
                        else:
                            sg_ = stg[six[0] % 2]
                            sds = stg_ds[six[0] % 2]
                            six[0] += 1
                            for st in range(nst):
                                pm = pmm[pix[0] % 4]
                                pix[0] += 1
                                mm_group(pm.t[:, 0:256], [(actT.t[:, kk, st * 128:(st + 1) * 128], wv[:, kk, :])
                                                           for kk in range(32)], pm.d, [slot.d, actT.d])
                                if cc < 24:
                                    k.op(dve, [pm.d], [sg_.d], lambda h, pm=pm, sg_=sg_, st=st: h.tensor_copy(
                                        out=sg_.t[:, st, :], in_=pm.t[:, 0:256]))
                                else:
                                    k.op(act, [pm.d], [sg_.d], lambda h, pm=pm, sg_=sg_, st=st: h.activation(
                                        out=sg_.t[:, st, :], in_=pm.t[:, 0:256], func=AF.Gelu_apprx_tanh))
                            if cc < 24:
                                vc = (cc - 16) * 256
                                if g < 4:
                                    rows = [(0, 4, 256 + 512 * g)]
                                elif g == 4:
                                    rows = [(0, 2, 0), (2, 4, 2304)]
                                else:
                                    rows = [(0, 2, 2560)]
                                for (a, b, p0) in rows:
                                    k.dma(sp, v_s[p0:p0 + (b - a) * 128, vc:vc + 256].rearrange("(s p) f -> p s f", p=128),
                                          sg_.t[:, a:b, :], [sg_.d], [], sds)
                            else:
                                dst = u_s if cc < 32 else z_s
                                uc = ((cc - 24) % 8) * 256
                                k.dma(sp, dst[g * 512:(g + 1) * 512, uc:uc + 256].rearrange("(s p) f -> p s f", p=128),
                                      sg_.t[:], [sg_.d], [], sds)
            k.barrier()

        if "B" in stages:
            with contextlib.ExitStack() as sB:
                mask = sb("mask", [128, 5, 640], F32, sB)
                k.dma(sp, mask.t[:], mask_tab.rearrange("p c b q -> p c (b q)"), [], [mask.d], misc_ds)
                KT = [sb(f"KT{i}", [128, NPOS], BF16, sB) for i in range(2)]
                QT = [sb(f"QT{i}", [128, NTOK], BF16, sB) for i in range(2)]
                VH = [sb(f"VH{i}", [128, 22, 132], BF16, sB) for i in range(2)]
                BI = [sb(f"BI{i}", [128, 5, 640], F32, sB) for i in range(2)]
                AH = [sb(f"AH{i}", [128, 16, 128], BF16, sB) for i in range(2)]
                hd_ds = [k.dsem() for _ in range(2)]
                ah_ds = [k.dsem() for _ in range(2)]
                ssb = [sb(f"ssb{i}", [128, 640], F32, sB) for i in range(2)]
                e1 = [sb(f"e1{i}", [128, 640], F32, sB) for i in range(2)]
                pT = [sb(f"pT{i}", [128, 896], BF16, sB) for i in range(2)]
                rec = sb("rec", [128, 2], F32, sB)
                Sps = [ps(f"Sps{i}", [128, 1024], F32, sB) for i in range(2)]
                Ops = [ps(f"Ops{i}", [128, 512], F32, sB) for i in range(2)]
                for i in range(2):
                    k.op(pool, [], [VH[i].d], lambda h, i=i: h.memset(VH[i].t[:, :, 128:129], 1.0))
                ui = 0
                for hd in range(NH):
                    b_ = hd % 2
                    deps_w = [KT[b_].d, QT[b_].d, VH[b_].d, BI[b_].d]
                    k.dma(sp, KT[b_].t[:], kT_s[hd], [], [KT[b_].d], hd_ds[b_])
                    k.dma(sp, QT[b_].t[:], qT_s[hd], [], [QT[b_].d], hd_ds[b_])
                    k.dma(sp, VH[b_].t[:, :, 0:128], v_s[:, hd * 128:(hd + 1) * 128].rearrange("(b p) d -> p b d", p=128),
                          [], [VH[b_].d], hd_ds[b_])
                    k.dma(sp, BI[b_].t[:], bias_tab[hd].rearrange("p c b q -> p c (b q)"), [], [BI[b_].d], hd_ds[b_])
                    fin = (hd_ds[b_], hd_ds[b_].n)
                    for d in deps_w:
                        d.w = fin
                    for j in range(16):
                        cls = TCLS.get(j, 0)
                        u2 = ui % 2
                        ui += 1
                        S, O = Sps[u2], Ops[u2]
                        blocks = [j + bl for bl in range(5)] + [20, 21]
                        for bi_, pb in enumerate(blocks):
                            k.op(pe, [KT[b_].d, QT[b_].d], [S.d], lambda h, S=S, bi_=bi_, pb=pb, j=j, b_=b_: h.matmul(
                                S.t[:, bi_ * 128:(bi_ + 1) * 128], lhsT=KT[b_].t[:, pb * 128:(pb + 1) * 128],
                                rhs=QT[b_].t[:, j * 128:(j + 1) * 128], start=True, stop=True), inc=(bi_ == 6))
                        k.op(dve, [S.d, BI[b_].d], [ssb[u2].d], lambda h, S=S, u2=u2, cls=cls, b_=b_: h.tensor_tensor(
                            out=ssb[u2].t[:], in0=S.t[:, 0:640], in1=BI[b_].t[:, cls, :], op=ALU.add))
                        k.op(act, [ssb[u2].d], [e1[u2].d], lambda h, u2=u2: h.activation(
                            out=e1[u2].t[:], in_=ssb[u2].t[:], func=AF.Exp))
                        k.op(act, [S.d], [pT[u2].d], lambda h, S=S, u2=u2: h.activation(
                            out=pT[u2].t[:, 640:896], in_=S.t[:, 640:896], func=AF.Exp))
                        k.op(pool, [e1[u2].d, mask.d], [pT[u2].d], lambda h, u2=u2, cls=cls: h.tensor_tensor(
                            out=pT[u2].t[:, 0:640], in0=e1[u2].t[:], in1=mask.t[:, cls, :], op=ALU.mult))
                        for bi_, pb in enumerate(blocks):
                            k.op(pe, [pT[u2].d, VH[b_].d], [O.d], lambda h, O=O, bi_=bi_, pb=pb, u2=u2, b_=b_: h.matmul(
                                O.t[:, 0:129], lhsT=pT[u2].t[:, bi_ * 128:(bi_ + 1) * 128], rhs=VH[b_].t[:, pb, 0:129],
                                start=(bi_ == 0), stop=(bi_ == 6)), inc=(bi_ == 6))
                        k.op(dve, [O.d], [rec.d], lambda h, O=O, u2=u2: h.reciprocal(
                            out=rec.t[:, u2:u2 + 1], in_=O.t[:, 128:129]))
                        k.op(dve, [O.d, rec.d], [AH[b_].d], lambda h, O=O, u2=u2, j=j, b_=b_: h.tensor_scalar(
                            out=AH[b_].t[:, j, :], in0=O.t[:, 0:128], scalar1=rec.t[:, u2:u2 + 1], scalar2=None, op0=ALU.mult))
                    k.dma(sp, attn_s[:, hd * 128:(hd + 1) * 128].rearrange("(j p) d -> p j d", p=128), AH[b_].t[:],
                          [AH[b_].d], [], ah_ds[b_])
            k.barrier()

        gall = sb("gall", [128, 16, NE])
        cst = sb("cst", [128, 512])
        eot = sb("eot", [128, 96])
        eotk = sb("eotk", [128, 96])
        sBlk = contextlib.ExitStack()
        wr_b = sb("wr_b", [128, 32, NE], BF16, sBlk)
        rb_bc = sb("rb_bc", [128, NE], F32, sBlk)
        wsT_b = sb("wsT_b", [128, 16, 128], BF16, sBlk)
        bsT_sb = sb("bsT_sb", [128, 16], F32, sBlk)
        if sparse:
            k.dma(sp, cst.t[:], consts, [], [cst.d], misc_ds)
        if any(s_ in stages for s_ in "CDEF"):
            k.dma(pool, wr_b.t[:], w_router.rearrange("(k p) e -> p k e", p=128), [], [wr_b.d], misc_ds)
            k.dma(sp, rb_bc.t[:], rbias.to_broadcast([128, NE]), [], [rb_bc.d], misc_ds)
            k.dma(pool, wsT_b.t[:], wsT, [], [wsT_b.d], misc_ds)
            k.dma(sp, bsT_sb.t[:], bsT, [], [bsT_sb.d], misc_ds)
            fin = (misc_ds, misc_ds.n)
            for b_ in (wr_b, rb_bc, wsT_b, bsT_sb):
                b_.d.w = fin

        for tb in range(nblocks if any(s_ in stages for s_ in "CDEF") else 0):
            if "C" in stages:
                with contextlib.ExitStack() as sC:
                    lng = sb("lng", [128, 2048], F32, sC)
                    lnb = sb("lnb", [128, 2048], F32, sC)
                    gob = sb("gob", [128, D], F32, sC)
                    tds = k.dsem() if tb == 0 else tds_keep[0]
                    if tb == 0:
                        tds_keep = [tds]
                    k.dma(sp, lng.t[:], ln_g.to_broadcast([128, 2048]), [], [lng.d], tds)
                    k.dma(sp, lnb.t[:], ln_b.to_broadcast([128, 2048]), [], [lnb.d], tds)
                    k.dma(sp, gob.t[:], gout.to_broadcast([128, D]), [], [gob.d], tds)
                    fin = (tds, tds.n)
                    for b_ in (lng, lnb, gob):
                        b_.d.w = fin
                    if tb == 0:
                        ld_ds = [k.dsem() for _ in range(2)]
                    ut = [sb(f"ut{i}", [128, 2048], BF16, sC) for i in range(2)]
                    zt = [sb(f"zt{i}", [128, 2048], BF16, sC) for i in range(2)]
                    at = [sb(f"at{i}", [128, 2048], BF16, sC) for i in range(2)]
                    fa = sb("fa", [128, 2048], F32, sC)
                    zn = sb("zn", [128, 2048], BF16, sC)
                    y = sb("y", [128, D], BF16, sC)
                    mix = ps("mix", [128, 2048], F32, sC)
                    ptc = [ps(f"ptc{i}", [128, 8, 128], BF16, sC) for i in range(2)]
                    for st in range(4):
                        r0 = tb * 512 + st * 128
                        i2 = st % 2
                        k.dma(sp, ut[i2].t[:], u_s[r0:r0 + 128, :], [], [ut[i2].d], ld_ds[i2])
                        k.dma(sp, zt[i2].t[:], z_s[r0:r0 + 128, :], [], [zt[i2].d], ld_ds[i2])
                        k.dma(sp, at[i2].t[:], attn_s[r0:r0 + 128, :], [], [at[i2].d], ld_ds[i2])
                        fin = (ld_ds[i2], ld_ds[i2].n)
                        for b_ in (ut[i2], zt[i2], at[i2]):
                            b_.d.w = fin
                        U, Z, A_ = ut[i2], zt[i2], at[i2]
                        zero(sm.t[:, 2:4])
                        k.op(act, [Z.d], [fa.d, sm.d], lambda h, Z=Z: h.activation(
                            out=fa.t[:], in_=Z.t[:], func=AF.Identity, accum_out=sm.t[:, 2:3]))
                        k.op(act, [Z.d], [y.d, sm.d], lambda h, Z=Z: h.activation(
                            out=y.t[:, 0:2048], in_=Z.t[:], func=AF.Square, accum_out=sm.t[:, 3:4]))
                        k.op(dve, [sm.d], [sm.d], lambda h: h.tensor_scalar(
                            out=sm.t[:, 4:5], in0=sm.t[:, 2:3], scalar1=1.0 / 2048, scalar2=None, op0=ALU.mult))
                        k.op(dve, [sm.d], [sm.d], lambda h: h.tensor_tensor(
                            out=sm.t[:, 5:6], in0=sm.t[:, 4:5], in1=sm.t[:, 4:5], op=ALU.mult))
                        k.op(dve, [sm.d], [sm.d], lambda h: h.scalar_tensor_tensor(
                            out=sm.t[:, 6:7], in0=sm.t[:, 3:4], scalar=1.0 / 2048, in1=sm.t[:, 5:6],
                            op0=ALU.mult, op1=ALU.subtract))
                        k.op(dve, [sm.d], [sm.d], lambda h: h.tensor_scalar(
                            out=sm.t[:, 6:7], in0=sm.t[:, 6:7], scalar1=EPS, scalar2=None, op0=ALU.add))
                        k.op(act, [sm.d], [sm.d], lambda h: h.activation(out=sm.t[:, 6:7], in_=sm.t[:, 6:7], func=AF.Sqrt))
                        k.op(dve, [sm.d], [sm.d], lambda h: h.reciprocal(out=sm.t[:, 6:7], in_=sm.t[:, 6:7]))
                        k.op(dve, [sm.d], [sm.d], lambda h: h.scalar_tensor_tensor(
                            out=sm.t[:, 7:8], in0=sm.t[:, 4:5], scalar=-1.0, in1=sm.t[:, 6:7],
                            op0=ALU.mult, op1=ALU.mult))
                        k.op(act, [sm.d, fa.d], [fa.d], lambda h: h.activation(
                            out=fa.t[:], in_=fa.t[:], func=AF.Identity, scale=sm.t[:, 6:7], bias=sm.t[:, 7:8]))
                        k.op(dve, [fa.d, lng.d], [fa.d], lambda h: h.tensor_tensor(
                            out=fa.t[:], in0=fa.t[:], in1=lng.t[:], op=ALU.mult))
                        k.op(pool, [fa.d, lnb.d], [zn.d], lambda h: h.tensor_tensor(
                            out=zn.t[:], in0=fa.t[:], in1=lnb.t[:], op=ALU.add))
                        for g in range(16):
                            k.op(pe, [zn.d, wsT_b.d], [mix.d], lambda h, g=g: h.matmul(
                                mix.t[:, g * 128:(g + 1) * 128], lhsT=wsT_b.t[:, g, :], rhs=zn.t[:, g * 128:(g + 1) * 128],
                                start=True, stop=True), inc=(g == 15))
                        for g in range(16):
                            k.op(dve, [mix.d, bsT_sb.d, U.d], [fa.d], lambda h, g=g, U=U: h.scalar_tensor_tensor(
                                out=fa.t[:, g * 128:(g + 1) * 128], in0=mix.t[:, g * 128:(g + 1) * 128],
                                scalar=bsT_sb.t[:, g:g + 1], in1=U.t[:, g * 128:(g + 1) * 128], op0=ALU.add, op1=ALU.mult))
                        zero(sm.t[:, 8:10])
                        k.op(act, [fa.d], [y.d, sm.d], lambda h: h.activation(
                            out=y.t[:, 2048:4096], in_=fa.t[:], func=AF.Square, accum_out=sm.t[:, 9:10]))
                        k.op(act, [A_.d], [y.d, sm.d], lambda h, A_=A_: h.activation(
                            out=y.t[:, 0:2048], in_=A_.t[:], func=AF.Square, accum_out=sm.t[:, 8:9]))
                        rstd_from_ssq(sm.t[:, 8:10], sm.t[:, 10:12], 2048, [sm.d], [sm.d])
                        k.op(dve, [A_.d, sm.d, gob.d], [y.d], lambda h, A_=A_: h.scalar_tensor_tensor(
                            out=y.t[:, 0:2048], in0=A_.t[:], scalar=sm.t[:, 10:11], in1=gob.t[:, 0:2048],
                            op0=ALU.mult, op1=ALU.mult))
                        k.op(dve, [fa.d, sm.d, gob.d], [y.d], lambda h: h.scalar_tensor_tensor(
                            out=y.t[:, 2048:4096], in0=fa.t[:], scalar=sm.t[:, 11:12], in1=gob.t[:, 2048:4096],
                            op0=ALU.mult, op1=ALU.mult))
                        for k8 in range(4):
                            p = ptc[k8 % 2]
                            for q8 in range(8):
                                kk = k8 * 8 + q8
                                k.op(pe, [y.d, identb.d], [p.d], lambda h, p=p, q8=q8, kk=kk: h.transpose(
                                    p.t[:, q8, :], y.t[:, kk * 128:(kk + 1) * 128], identb.t[:]), inc=(q8 == 7))
                            k.op(act, [p.d], [actT.d], lambda h, p=p, k8=k8, st=st: h.copy(
                                out=actT.t[:, k8 * 8:(k8 + 1) * 8, st * 128:(st + 1) * 128], in_=p.t[:]))
                k.barrier()

            with contextlib.ExitStack() as sD:
                acc = [sb(f"acc{i}", [128, D], F32, sD) for i in range(4)]
                gates = sb("gates", [128, 4, 65], F32, sD)
                if tb == 0:
                    acc_ds = [k.dsem() for _ in range(4)]
                    pc_ds = [k.dsem() for _ in range(2)]
                    x1_ds = [k.dsem() for _ in range(4)]
                    o_ds = [k.dsem() for _ in range(2)]
                x1d = [Dep() for _ in range(4)]
                if "D" in stages:
                    with contextlib.ExitStack() as sDD:
                        gmp = [sb(f"gmp{i}", [128, 256], F32, sDD) for i in range(2)]
                        tmp = [sb(f"tmpD{i}", [128, 256], F32, sDD) for i in range(2)]
                        hfn = [sb(f"hfn{i}", [128, 512], F32, sDD) for i in range(2)]
                        junk = sb("junkD", [128, D], BF16, sDD)
                        hfrow = junk
                        mp = [sb(f"mp{i}", [128, 512], F32, sDD) for i in range(3)] if sparse else None
                        if tb == 0:
                            hf_ds = k.dsem()
                            hf_keep = [hf_ds]
                            mp_ds = [k.dsem() for _ in range(3)]
                        hf_ds = hf_keep[0]
                        rt = sb("rt", [128, 512], F32, sDD)
                        pmm = [ps(f"pmmD{i}", [128, 512], F32, sDD) for i in range(4)]
                        ptr = [ps(f"ptrD{i}", [128, 4, 128], F32, sDD) for i in range(2)]
                        plg = ps("plg", [128, NE], F32, sDD)
                        for st in range(4):
                            r0 = tb * 512 + st * 128
                            k.dma(sp, acc[st].t[:], x_own[r0:r0 + 128, :], [], [acc[st].d], acc_ds[st])
                        pix = 0
                        for cc in range(16):
                            c0 = cc * 256
                            slot, wv = ring_load(w_out[:, c0:c0 + 256].rearrange("(k p) f -> p k f", p=128), (32, 256))
                            gp = gmp[cc % 2]
                            k.dma(sp, gp.t[:], modrows[0:1, 2 * D + c0:2 * D + c0 + 256].to_broadcast([128, 256]),
                                  [], [gp.d], pc_ds[cc % 2])
                            for st in range(4):
                                pm = pmm[pix % 4]
                                tm = tmp[pix % 2]
                                pix += 1
                                mm_group(pm.t[:, 0:256], [(actT.t[:, kk, st * 128:(st + 1) * 128], wv[:, kk, :])
                                                           for kk in range(32)], pm.d, [slot.d, actT.d])
                                k.op(dve, [pm.d, gp.d], [tm.d], lambda h, pm=pm, tm=tm, gp=gp: h.tensor_tensor(
                                    out=tm.t[:], in0=pm.t[:, 0:256], in1=gp.t[:], op=ALU.mult))
                                k.op(pool, [tm.d, acc[st].d], [acc[st].d], lambda h, tm=tm, st=st, c0=c0: h.tensor_tensor(
                                    out=acc[st].t[:, c0:c0 + 256], in0=acc[st].t[:, c0:c0 + 256], in1=tm.t[:], op=ALU.add))
                        for st in range(4):
                            r0 = tb * 512 + st * 128
                            A_ = acc[st]
                            k.dma(sp, x1_s[r0:r0 + 128, :], A_.t[:], [A_.d], [x1d[st]], x1_ds[st])
                            zero(sm.t[:, 0:1])
                            k.op(act, [A_.d], [junk.d, sm.d], lambda h, A_=A_: h.activation(
                                out=junk.t[:], in_=A_.t[:], func=AF.Square, accum_out=sm.t[:, 0:1]))
                            rstd_from_ssq(sm.t[:, 0:1], sm.t[:, 1:2], D, [sm.d], [sm.d])
                            for k4 in range(8):
                                hb = hfn[k4 % 2]
                                p = ptr[k4 % 2]
                                k.op(dve, [A_.d, sm.d], [hb.d], lambda h, A_=A_, hb=hb, k4=k4: h.tensor_scalar(
                                    out=hb.t[:], in0=A_.t[:, k4 * 512:(k4 + 1) * 512], scalar1=sm.t[:, 1:2], scalar2=None,
                                    op0=ALU.mult))
                                if sparse:
                                    cs_ = slice(k4 * 512, (k4 + 1) * 512)
                                    k.dma(sp, mp[0].t[:], modrows[0:1, 4 * D + k4 * 512:4 * D + (k4 + 1) * 512].to_broadcast([128, 512]), [], [mp[0].d], mp_ds[0])
                                    k.dma(sp, mp[1].t[:], gffn_row[0:1, cs_].to_broadcast([128, 512]), [], [mp[1].d], mp_ds[1])
                                    k.dma(sp, mp[2].t[:], modrows[0:1, 3 * D + k4 * 512:3 * D + (k4 + 1) * 512].to_broadcast([128, 512]), [], [mp[2].d], mp_ds[2])
                                    k.op(dve, [mp[0].d, mp[1].d], [mp[0].d], lambda h: h.scalar_tensor_tensor(
                                        out=mp[0].t[:], in0=mp[0].t[:], scalar=1.0, in1=mp[1].t[:], op0=ALU.add, op1=ALU.mult))
                                    k.op(pool, [mp[0].d, hb.d], [mp[0].d], lambda h, hb=hb: h.tensor_tensor(
                                        out=mp[0].t[:], in0=mp[0].t[:], in1=hb.t[:], op=ALU.mult))
                                    k.op(pool, [mp[0].d, mp[2].d], [hfrow.d], lambda h, cs_=cs_: h.tensor_tensor(
                                        out=hfrow.t[:, cs_], in0=mp[0].t[:], in1=mp[2].t[:], op=ALU.add))
                                for q4 in range(4):
                                    k.op(pe, [hb.d, identf.d], [p.d], lambda h, p=p, q4=q4, hb=hb: h.transpose(
                                        p.t[:, q4, :], hb.t[:, q4 * 128:(q4 + 1) * 128], identf.t[:]), inc=(q4 == 3))
                                for q4 in range(4):
                                    kk = k4 * 4 + q4
                                    k.op(act, [p.d, gmf.d, modfm.d], [actT.d], lambda h, p=p, q4=q4, kk=kk, st=st: h.activation(
                                        out=actT.t[:, kk, st * 128:(st + 1) * 128], in_=p.t[:, q4, :], func=AF.Identity,
                                        scale=gmf.t[:, kk:kk + 1], bias=modfm.t[:, 96 + kk, 0:1]))
                            if sparse:
                                k.dma(sp, hfn_s[r0:r0 + 128, :], hfrow.t[:], [hfrow.d], [], hf_ds)
                            mm_group(plg.t[:], [(actT.t[:, kk, st * 128:(st + 1) * 128], wr_b.t[:, kk, :]) for kk in range(32)],
                                     plg.d, [actT.d, wr_b.d])
                            S_ = rt.t[:, 0:64]
                            C_ = rt.t[:, 64:128]
                            M8 = rt.t[:, 128:192]
                            GS = rt.t[:, 192:200]
                            G8 = rt.t[:, 200:208]
                            GM = rt.t[:, 208:216]
                            CM = rt.t[:, 256:320]
                            T8 = rt.t[:, 320:328]
                            SEL = rt.t[:, 384:448]
                            WS = rt.t[:, 448:450]
                            rd = [rt.d]
                            k.op(act, [plg.d], rd, lambda h: h.activation(out=S_, in_=plg.t[:], func=AF.Sigmoid))
                            k.op(dve, rd + [rb_bc.d], rd, lambda h: h.tensor_tensor(out=C_, in0=S_, in1=rb_bc.t[:], op=ALU.add))
                            for g in range(8):
                                k.op(dve, rd, rd, lambda h, g=g: h.max(out=rt.t[:, 128 + g * 8:136 + g * 8],
                                                                       in_=rt.t[:, 64 + g * 8:72 + g * 8]))
                            m3 = M8.rearrange("p (g e) -> p g e", e=8)
                            k.op(dve, rd, rd, lambda h: h.tensor_tensor(out=GS, in0=m3[:, :, 0], in1=m3[:, :, 1], op=ALU.add))
                            k.op(dve, rd, rd, lambda h: h.max(out=G8, in_=GS))
                            k.op(dve, rd, rd, lambda h: h.tensor_scalar(out=GM, in0=GS, scalar1=rt.t[:, 203:204], scalar2=None,
                                                                        op0=ALU.is_ge))
                            for g in range(8):
                                k.op(dve, rd, rd, lambda h, g=g: h.tensor_scalar(
                                    out=rt.t[:, 256 + g * 8:264 + g * 8], in0=rt.t[:, 64 + g * 8:72 + g * 8], scalar1=10.0,
                                    scalar2=rt.t[:, 208 + g:209 + g], op0=ALU.add, op1=ALU.mult))
                            k.op(dve, rd, rd, lambda h: h.max(out=T8, in_=CM))
                            k.op(dve, rd, rd, lambda h: h.tensor_scalar(out=SEL, in0=CM, scalar1=rt.t[:, 327:328], scalar2=None,
                                                                        op0=ALU.is_ge))
                            k.op(dve, rd, rd, lambda h: h.tensor_tensor(out=SEL, in0=SEL, in1=S_, op=ALU.mult))
                            k.op(dve, rd, rd, lambda h: h.tensor_reduce(out=WS[:, 0:1], in_=SEL, axis=AX.X, op=ALU.add))
                            k.op(dve, rd, rd, lambda h: h.reciprocal(out=WS[:, 1:2], in_=WS[:, 0:1]))
                            k.op(dve, rd, [gates.d], lambda h, st=st: h.tensor_scalar(
                                out=gates.t[:, st, 0:64], in0=SEL, scalar1=rt.t[:, 449:450], scalar2=2.5, op0=ALU.mult, op1=ALU.mult))
                            k.op(dve, [], [gates.d], lambda h, st=st: h.memset(gates.t[:, st, 64:65], 1.0))
                            if sparse:
                                k.op(dve, [gates.d], [gall.d], lambda h, st=st, tb=tb: h.tensor_copy(
                                    out=gall.t[:, tb * 4 + st, :], in_=gates.t[:, st, 0:NE]))
                                k.dma(sp, gts_s[r0:r0 + 128, :], gates.t[:, st, 0:NE], [gates.d], [], hf_ds)
                            if dbg:
                                k.dma(sp, gates_s[r0:r0 + 128, :], gates.t[:, st, :], [gates.d], [], misc_ds)
                    k.barrier()

                if "E" in stages:
                    with contextlib.ExitStack() as sE:
                        actb = [sb(f"actb{i}", [128, 4, 512], BF16, sE) for i in range(2)]
                        slb = sb("slb", [128, 4, 512], BF16, sE)
                        sl_d = [Dep() for _ in range(4)]
                        pg = [ps(f"pg{i}", [128, 512], F32, sE) for i in range(2)]
                        pu = [ps(f"pu{i}", [128, 512], F32, sE) for i in range(2)]
                        pd = [ps(f"pd{i}", [128, 512], F32, sE) for i in range(4)]
                        gi = ui_ = di = 0
                        for e in range(nexp):
                            ee = e if nexp == 65 else (e if e < nexp - 1 else 64)
                            wg = we_gate[ee] if ee < 64 else ws_gate
                            wu = we_up[ee] if ee < 64 else ws_up
                            wd = we_down[ee] if ee < 64 else ws_down
                            ab = actb[e % 2]
                            for half in range(2):
                                c0 = half * 256
                                sG, vG = ring_load(wg[:, c0:c0 + 256].rearrange("(k p) f -> p k f", p=128), (32, 256))
                                sU, vU = ring_load(wu[:, c0:c0 + 256].rearrange("(k p) f -> p k f", p=128), (32, 256))
                                for cl in range(2):
                                    c = half * 2 + cl
                                    p = pg[gi % 2]
                                    gi += 1
                                    mm_group(p.t[:], [(vG[:, kk, cl * 128:(cl + 1) * 128], actT.t[:, kk, :]) for kk in range(32)],
                                             p.d, [sG.d, actT.d])
                                    k.op(act, [p.d], [sl_d[c]], lambda h, p=p, c=c: h.activation(
                                        out=slb.t[:, c, :], in_=p.t[:], func=AF.Silu))
                                for cl in range(2):
                                    c = half * 2 + cl
                                    p = pu[ui_ % 2]
                                    ui_ += 1
                                    mm_group(p.t[:], [(vU[:, kk, cl * 128:(cl + 1) * 128], actT.t[:, kk, :]) for kk in range(32)],
                                             p.d, [sU.d, actT.d])
                                    k.op(dve, [p.d, sl_d[c]], [ab.d], lambda h, p=p, c=c, ab=ab: h.tensor_tensor(
                                        out=ab.t[:, c, :], in0=slb.t[:, c, :], in1=p.t[:], op=ALU.mult))
                            for dh in range(2):
                                d0 = dh * 2048
                                sD_, vD = ring_load(wd[:, d0:d0 + 2048].rearrange("(c p) d -> p c d", p=128), (4, 2048))
                                for st in range(4):
                                    for db in range(4):
                                        p = pd[di % 4]
                                        di += 1
                                        mm_group(p.t[:], [(ab.t[:, c, st * 128:(st + 1) * 128], vD[:, c, db * 512:(db + 1) * 512])
                                                          for c in range(4)], p.d, [sD_.d, ab.d])
                                        oc = d0 + db * 512
                                        if e == 0:
                                            k.op(dve, [p.d, gates.d, x1d[st]], [acc[st].d], lambda h, p=p, st=st, oc=oc, ee=ee: h.tensor_scalar(
                                                out=acc[st].t[:, oc:oc + 512], in0=p.t[:], scalar1=gates.t[:, st, ee:ee + 1], scalar2=None,
                                                op0=ALU.mult))
                                        else:
                                            k.op(dve, [p.d, gates.d], [acc[st].d], lambda h, p=p, st=st, oc=oc, ee=ee: h.scalar_tensor_tensor(
                                                out=acc[st].t[:, oc:oc + 512], in0=p.t[:], scalar=gates.t[:, st, ee:ee + 1],
                                                in1=acc[st].t[:, oc:oc + 512], op0=ALU.mult, op1=ALU.add))
                    k.barrier()

                if "F" in stages:
                    with contextlib.ExitStack() as sF:
                        x1p = [sb(f"x1p{i}", [128, 512], F32, sF) for i in range(2)]
                        gfp = [sb(f"gfp{i}", [128, 512], F32, sF) for i in range(2)]
                        fgp = [sb(f"fgp{i}", [128, 512], F32, sF) for i in range(2)]
                        ob = [sb(f"ob{i}", [128, 512], F32, sF) for i in range(2)]
                        junk = sb("junkF", [128, D], BF16, sF) if not sparse else None
                        if tb == 0:
                            f_ds = [k.dsem() for _ in range(6)]
                        for st in range(4):
                            r0 = tb * 512 + st * 128
                            A_ = acc[st]
                            for pc in range(8):
                                i2 = pc % 2
                                k.dma(sp, x1p[i2].t[:], x1_s[r0:r0 + 128, pc * 512:(pc + 1) * 512], [x1d[st]], [x1p[i2].d], f_ds[i2])
                                k.dma(sp, gfp[i2].t[:], modrows[0:1, 5 * D + pc * 512:5 * D + (pc + 1) * 512].to_broadcast([128, 512]),
                                      [], [gfp[i2].d], f_ds[2 + i2])
                                k.op(dve, [A_.d, gfp[i2].d], [A_.d], lambda h, A_=A_, pc=pc, i2=i2: h.tensor_tensor(
                                    out=A_.t[:, pc * 512:(pc + 1) * 512], in0=A_.t[:, pc * 512:(pc + 1) * 512], in1=gfp[i2].t[:], op=ALU.mult))
                                k.op(pool, [A_.d, x1p[i2].d], [A_.d], lambda h, A_=A_, pc=pc, i2=i2: h.tensor_tensor(
                                    out=A_.t[:, pc * 512:(pc + 1) * 512], in0=A_.t[:, pc * 512:(pc + 1) * 512], in1=x1p[i2].t[:], op=ALU.add))
                            zero(sm.t[:, 12:13])
                            if sparse:
                                k.dma(sp, x1_s[r0:r0 + 128, :], A_.t[:], [A_.d], [x1d[st]], x1_ds[st])
                                continue
                            k.op(act, [A_.d], [junk.d, sm.d], lambda h, A_=A_: h.activation(
                                out=junk.t[:], in_=A_.t[:], func=AF.Square, accum_out=sm.t[:, 12:13]))
                            rstd_from_ssq(sm.t[:, 12:13], sm.t[:, 13:14], D, [sm.d], [sm.d])
                            for pc in range(8):
                                i2 = pc % 2
                                k.dma(sp, fgp[i2].t[:], final_g[0:1, pc * 512:(pc + 1) * 512].to_broadcast([128, 512]),
                                      [], [fgp[i2].d], f_ds[4 + i2])
                                k.op(dve, [A_.d, sm.d, fgp[i2].d], [ob[i2].d], lambda h, A_=A_, pc=pc, i2=i2: h.scalar_tensor_tensor(
                                    out=ob[i2].t[:], in0=A_.t[:, pc * 512:(pc + 1) * 512], scalar=sm.t[:, 13:14], in1=fgp[i2].t[:],
                                    op0=ALU.mult, op1=ALU.mult))
                                k.dma(sp, out[r0:r0 + 128, pc * 512:(pc + 1) * 512], ob[i2].t[:], [ob[i2].d], [], o_ds[i2])
                    k.barrier()
        sBlk.close()
        if sparse and "S" in stages:
            I32_ = mybir.dt.int32
            reg_tos = nc.gpsimd.to_reg(96 * 512 - 1)
            reg_y = nc.gpsimd.to_reg(16 * NTOK - 1)
            iota_e = cst.t[:, 0:64]
            iota_j = cst.t[:, 64:160]
            pidx = cst.t[:, 160:161]
            base16 = cst.t[:, 192:208]
            with contextlib.ExitStack() as s2:
                selb = sb("selb", [128, 16, NE], BF16, s2)
                trif = sb("trif", [128, 128], F32, s2)
                trib = sb("trib", [128, 128], BF16, s2)
                oneb = sb("oneb", [128, 128], BF16, s2)
                cnt = sb("cnt", [128, NE], F32, s2)
                tl = sb("tl", [128, NE], F32, s2)
                stt = sb("stt", [128, NE + 1], F32, s2)
                wk = sb("wk", [128, 256], F32, s2)
                sI = [sb(f"sI{i}", [128, 8], I32_, s2) for i in range(2)]
                trow = [sb(f"trow{i}", [128, 16], I32_, s2) for i in range(2)]
                tinit = sb("tinit", [128, 384, 16], I32_, s2)
                pcn = ps("pcn", [128, NE], F32, s2)
                ppo = [ps(f"ppo{i}", [128, NE], F32, s2) for i in range(2)]
                tosD = Dep()
                sc_ds = [k.dsem() for _ in range(2)]
                k.dma(sp, trif.t[:], tri_in, [], [trif.d], misc_ds)
                k.op(dve, [trif.d], [trib.d], lambda h: h.tensor_copy(out=trib.t[:], in_=trif.t[:]))
                k.op(dve, [], [oneb.d], lambda h: h.memset(oneb.t[:], 1.0))
                k.op(pool, [], [tinit.d], lambda h: h.memset(tinit.t[:], NTOK))
                k.dma(sp, tos_d.rearrange("(a p) c -> p a c", p=128), tinit.t[:], [tinit.d], [tosD], misc_ds)
                k.op(dve, [gall.d], [selb.d], lambda h: h.tensor_scalar(
                    out=selb.t[:], in0=gall.t[:], scalar1=0.0, scalar2=None, op0=ALU.is_gt))
                mm_group(pcn.t[:], [(oneb.t[:], selb.t[:, i, :]) for i in range(16)], pcn.d, [oneb.d, selb.d])
                k.op(dve, [pcn.d], [cnt.d], lambda h: h.tensor_copy(out=cnt.t[:], in_=pcn.t[:]))
                k.op(dve, [cnt.d], [tl.d], lambda h: h.tensor_scalar(out=tl.t[:], in0=cnt.t[:], scalar1=0.0, scalar2=None, op0=ALU.is_gt))
                for jj in range(1, 4):
                    k.op(dve, [cnt.d, tl.d], [tl.d], lambda h, jj=jj: h.scalar_tensor_tensor(
                        out=tl.t[:], in0=cnt.t[:], scalar=512.0 * jj, in1=tl.t[:], op0=ALU.is_gt, op1=ALU.add))
                k.op(dve, [], [stt.d], lambda h: h.memset(stt.t[:, 0:1], 0.0))
                for e in range(NE):
                    k.op(dve, [tl.d, stt.d], [stt.d], lambda h, e=e: h.tensor_tensor(
                        out=stt.t[:, e + 1:e + 2], in0=stt.t[:, e:e + 1], in1=tl.t[:, e:e + 1], op=ALU.add))
                k.op(dve, [], [eot.d], lambda h: h.memset(eot.t[:], -1.0))
                for e in range(NE):
                    k.op(dve, [stt.d, cst.d, eot.d], [eot.d], lambda h, e=e: h.scalar_tensor_tensor(
                        out=eot.t[:], in0=iota_j, scalar=stt.t[:, e:e + 1], in1=eot.t[:], op0=ALU.is_ge, op1=ALU.add))
                k.op(dve, [eot.d], [eotk.d], lambda h: h.tensor_scalar(
                    out=eotk.t[:], in0=eot.t[:], scalar1=1024.0, scalar2=None, op0=ALU.mult))
                k.op(dve, [stt.d, cst.d], [wk.d], lambda h: h.tensor_scalar(
                    out=wk.t[:, 0:96], in0=iota_j, scalar1=stt.t[:, NE:NE + 1], scalar2=1.0e7, op0=ALU.is_ge, op1=ALU.mult))
                pass
                for i in range(16):
                    pp = ppo[i % 2]
                    pairs = [(oneb.t[:], selb.t[:, i2, :]) for i2 in range(i)] + [(trib.t[:], selb.t[:, i, :])]
                    mm_group(pp.t[:], pairs, pp.d, [oneb.d, trib.d, selb.d])
                    W = wk
                    k.op(dve, [stt.d, pp.d], [W.d], lambda h, pp=pp: h.scalar_tensor_tensor(
                        out=wk.t[:, 0:64], in0=stt.t[:, 0:NE], scalar=512.0, in1=pp.t[:], op0=ALU.mult, op1=ALU.add))
                    k.op(dve, [gall.d], [W.d], lambda h, i=i: h.tensor_scalar(
                        out=wk.t[:, 64:128], in0=gall.t[:, i, :], scalar1=0.0, scalar2=None, op0=ALU.is_gt))
                    k.op(dve, [W.d], [W.d], lambda h: h.scalar_tensor_tensor(
                        out=wk.t[:, 128:192], in0=wk.t[:, 0:64], scalar=1.0, in1=wk.t[:, 64:128], op0=ALU.add, op1=ALU.mult))
                    k.op(dve, [W.d], [W.d], lambda h: h.max(out=wk.t[:, 192:200], in_=wk.t[:, 128:192]))
                    k.op(dve, [W.d], [W.d], lambda h: h.tensor_scalar(
                        out=wk.t[:, 200:208], in0=wk.t[:, 192:200], scalar1=-1.0, scalar2=None, op0=ALU.add))
                    si = sI[i % 2]
                    tr = trow[i % 2]
                    k.op(dve, [W.d], [si.d], lambda h, si=si: h.tensor_copy(out=si.t[:], in_=wk.t[:, 200:208]))
                    k.op(dve, [cst.d], [W.d], lambda h, i=i: h.tensor_scalar(
                        out=wk.t[:, 208:224], in0=cst.t[:, 160:161].to_broadcast([128, 16]), scalar1=float(128 * i), scalar2=None, op0=ALU.add))
                    k.op(dve, [W.d], [tr.d], lambda h, tr=tr: h.tensor_copy(out=tr.t[:], in_=wk.t[:, 208:224]))
                    for kk in range(8):
                        k.idma(tos_d[:, :], bass.IndirectOffsetOnAxis(ap=si.t[:, kk:kk + 1], axis=0), tr.t[:, :], None,
                               [si.d, tr.d, tosD], [], sc_ds[i % 2], bounds_check=reg_tos, oob_is_err=False)
            k.barrier()

            with contextlib.ExitStack() as s3:
                gmfk = sb("gmfk", [128, 32], F32, s3)
                shfk = sb("shfk", [128, 32], F32, s3)
                gfk = sb("gfk", [128, 32], F32, s3)
                k.dma(sp, gfk.t[:], gffn_pk, [], [gfk.d], misc_ds)
                k.dma(sp, gmfk.t[:], modrows[0:1, 4 * D:5 * D].rearrange("o (p k) -> (o p) k", k=32), [], [gmfk.d], misc_ds)
                k.dma(sp, shfk.t[:], modrows[0:1, 3 * D:4 * D].rearrange("o (p k) -> (o p) k", k=32), [], [shfk.d], misc_ds)
                fin = (misc_ds, misc_ds.n)
                for b_ in (gfk, gmfk, shfk):
                    b_.d.w = fin
                k.op(dve, [gmfk.d, gfk.d], [gmfk.d], lambda h: h.scalar_tensor_tensor(
                    out=gmfk.t[:], in0=gmfk.t[:], scalar=1.0, in1=gfk.t[:], op0=ALU.add, op1=ALU.mult))
                hg = [sb(f"hg{i}", [128, D], BF16, s3) for i in range(2)]
                grow = [sb(f"grow{i}", [128, NE], F32, s3) for i in range(2)]
                tokI = [sb(f"tokI{i}", [128, 16], I32_, s3) for i in range(2)]
                idxf = sb("idxf", [128, 16], F32, s3)
                idxi = [sb(f"idxi{i}", [128, 16], I32_, s3) for i in range(2)]
                ohg = sb("ohg", [128, 2, NE], F32, s3)
                w3 = sb("w3", [128, 256], F32, s3)
                gsl = [sb(f"gsl{i}", [128, 4], F32, s3) for i in range(2)]
                dsti = [sb(f"dsti{i}", [128, 4, 2], I32_, s3) for i in range(2)]
                actb = [sb(f"actbS{i}", [128, 4, 512], BF16, s3) for i in range(2)]
                slb = sb("slbS", [128, 4, 512], BF16, s3)
                sl_d = [Dep() for _ in range(4)]
                pgu = [ps(f"pgu{i}", [128, 512], F32, s3) for i in range(4)]
                pd = [ps(f"pdS{i}", [128, 512], F32, s3) for i in range(2)]
                ptr = [ps(f"ptrS{i}", [128, 8, 128], BF16, s3) for i in range(2)]
                tk_ds = [k.dsem() for _ in range(2)]
                hg_ds = [k.dsem() for _ in range(2)]
                gr_ds = [k.dsem() for _ in range(2)]
                wg2 = we_gate.rearrange("e (p k) f -> (e p) (k f)", k=32).rearrange("r (q c) -> (r q) c", c=2048)
                wu2 = we_up.rearrange("e (p k) f -> (e p) (k f)", k=32).rearrange("r (q c) -> (r q) c", c=2048)
                wd2 = we_down.rearrange("e f (h c) -> (e f h) c", c=2048)

                def ring_gather(src2, cols):
                    i = ring_i[0] % NR
                    ring_i[0] += 1
                    for q, c in enumerate(cols):
                        k.idma(ring[i].t[:, q * 2048:(q + 1) * 2048], None, src2,
                               bass.IndirectOffsetOnAxis(ap=c, axis=0), [ixd], [ring[i].d], ring_ds[i])
                    ring[i].d.w = (ring_ds[i], ring_ds[i].n)
                    return ring[i]

                ring.extend([sb(f"ringx{i}", [128, 4096 * 2], BF16, s3) for i in range(2)])
                ring_ds.extend([k.dsem() for _ in range(2)])
                NR3 = 6
                ring_i[0] = 0
                ohgs = [ohg, sb("ohg2", [128, 2, NE], F32, s3)]
                reg_w = nc.gpsimd.to_reg(NE * 1024 - 1)
                ybq = [sb(f"ybq{i}", [128, 2048], F32, s3) for i in range(4)]
                ybq_ds = [k.dsem() for _ in range(4)]
                y2 = y_s.rearrange("r (h c) -> (r h) c", h=2)
                cnts = {"ui": 0, "di": 0, "yi": 0}

                def ring_gather2(src2, cols, ixd):
                    i = ring_i[0] % NR3
                    ring_i[0] += 1
                    rd = ring[i].d
                    k.need(pool, rd.w)
                    for o, c_ in rd.r.items():
                        k.need(pool, (o, c_))
                    for q, c in enumerate(cols):
                        k.idma(ring[i].t[:, q * 2048:(q + 1) * 2048], None, src2,
                               bass.IndirectOffsetOnAxis(ap=c, axis=0), [ixd], [], ring_ds[i])
                    rd.w = (ring_ds[i], ring_ds[i].n)
                    rd.r = {}
                    return ring[i]

                def prep(j):
                    ix = idxi[j % 2]
                    og = ohgs[j % 2]
                    k.op(dve, [eotk.d, cst.d], [idxf.d], lambda h: h.tensor_scalar(
                        out=idxf.t[:], in0=base16, scalar1=eotk.t[:, j:j + 1], scalar2=None, op0=ALU.add))
                    k.op(dve, [idxf.d], [ix.d], lambda h: h.tensor_copy(out=ix.t[:], in_=idxf.t[:]))
                    k.op(dve, [eot.d, cst.d], [og.d], lambda h: h.tensor_scalar(
                        out=og.t[:, 0, :], in0=iota_e, scalar1=eot.t[:, j:j + 1], scalar2=None, op0=ALU.is_equal))
                    k.op(dve, [eot.d, cst.d], [og.d], lambda h: h.tensor_scalar(
                        out=og.t[:, 1, :], in0=iota_e, scalar1=eot.t[:, j:j + 1], scalar2=None, op0=ALU.is_gt))
                    gs_, dsi = gsl[j % 2], dsti[j % 2]
                    for st in range(4):
                        u2 = cnts["ui"] % 2
                        cnts["ui"] += 1
                        s0 = j * 512 + st * 128
                        T, H, G = tokI[u2], hg[u2], grow[u2]
                        k.dma(sp, T.t[:], tos_d[s0:s0 + 128, :], [], [T.d], tk_ds[u2])
                        k.idma(H.t[:], None, hfn_s[:, :], bass.IndirectOffsetOnAxis(ap=T.t[:, 0:1], axis=0), [T.d], [H.d], hg_ds[u2])
                        k.idma(G.t[:], None, gts_s[:, :], bass.IndirectOffsetOnAxis(ap=T.t[:, 0:1], axis=0), [T.d], [G.d], gr_ds[u2])
                        wd_ = [w3.d]
                        k.op(dve, [G.d, og.d], wd_, lambda h, G=G: h.tensor_tensor(
                            out=w3.t[:, 0:64], in0=G.t[:], in1=og.t[:, 0, :], op=ALU.mult))
                        k.op(dve, wd_, [gs_.d], lambda h, st=st: h.tensor_reduce(
                            out=gs_.t[:, st:st + 1], in_=w3.t[:, 0:64], axis=AX.X, op=ALU.add))
                        k.op(dve, [G.d], wd_, lambda h, G=G: h.tensor_scalar(
                            out=w3.t[:, 64:128], in0=G.t[:], scalar1=0.0, scalar2=None, op0=ALU.is_gt))
                        k.op(dve, wd_ + [og.d], wd_, lambda h: h.tensor_tensor(
                            out=w3.t[:, 64:128], in0=w3.t[:, 64:128], in1=og.t[:, 1, :], op=ALU.mult))
                        k.op(dve, wd_, wd_, lambda h: h.tensor_reduce(
                            out=w3.t[:, 128:129], in_=w3.t[:, 64:128], axis=AX.X, op=ALU.add))
                        k.op(dve, [T.d], wd_, lambda h, T=T: h.tensor_copy(out=w3.t[:, 129:130], in_=T.t[:, 0:1]))
                        k.op(dve, wd_, wd_, lambda h: h.tensor_scalar(
                            out=w3.t[:, 130:131], in0=w3.t[:, 129:130], scalar1=float(NTOK), scalar2=1.0e6, op0=ALU.is_ge, op1=ALU.mult))
                        k.op(dve, wd_, wd_, lambda h: h.scalar_tensor_tensor(
                            out=w3.t[:, 131:132], in0=w3.t[:, 128:129], scalar=float(NTOK), in1=w3.t[:, 129:130], op0=ALU.mult, op1=ALU.add))
                        k.op(dve, wd_, wd_, lambda h: h.tensor_tensor(
                            out=w3.t[:, 131:132], in0=w3.t[:, 131:132], in1=w3.t[:, 130:131], op=ALU.add))
                        k.op(dve, wd_, wd_, lambda h: h.tensor_scalar(
                            out=w3.t[:, 132:133], in0=w3.t[:, 131:132], scalar1=2.0, scalar2=None, op0=ALU.mult))
                        k.op(dve, wd_, wd_, lambda h: h.tensor_scalar(
                            out=w3.t[:, 133:134], in0=w3.t[:, 131:132], scalar1=2.0, scalar2=1.0, op0=ALU.mult, op1=ALU.add))
                        k.op(dve, wd_, [dsi.d], lambda h, st=st: h.tensor_copy(out=dsi.t[:, st, :], in_=w3.t[:, 132:134]))
                        hv = H.t[:].rearrange("t (p k) -> t k p", k=32)
                        for k8 in range(4):
                            p = ptr[k8 % 2]
                            for q8 in range(8):
                                kk = k8 * 8 + q8
                                k.op(pe, [H.d, identb.d], [p.d], lambda h, p=p, q8=q8, kk=kk, hv=hv: h.transpose(
                                    p.t[:, q8, :], hv[:, kk, :], identb.t[:]), inc=(q8 == 7))
                            if k8 % 2 == 0:
                                k.op(act, [p.d], [actT.d], lambda h, p=p, k8=k8, st=st: h.copy(
                                    out=actT.t[:, k8 * 8:(k8 + 1) * 8, st * 128:(st + 1) * 128], in_=p.t[:]))
                            else:
                                k.op(dve, [p.d], [actT.d], lambda h, p=p, k8=k8, st=st: h.tensor_copy(
                                    out=actT.t[:, k8 * 8:(k8 + 1) * 8, st * 128:(st + 1) * 128], in_=p.t[:]))

                def weights_gu(j):
                    ix = idxi[j % 2]
                    gsl_ = [ring_gather2(wg2, [ix.t[:, kh * 4 + q:kh * 4 + q + 1] for q in range(4)], ix.d) for kh in range(2)]
                    usl_ = [ring_gather2(wu2, [ix.t[:, kh * 4 + q:kh * 4 + q + 1] for q in range(4)], ix.d) for kh in range(2)]
                    return gsl_, usl_

                def weights_d(j):
                    ix = idxi[j % 2]
                    return [ring_gather2(wd2, [ix.t[:, 8 + c * 2 + dh:8 + c * 2 + dh + 1] for c in range(4)], ix.d) for dh in range(2)]

                def compute_gu(j, gsl_, usl_):
                    ab = actb[j % 2]
                    for which, slots in ((0, gsl_), (1, usl_)):
                        for c in range(4):
                            pairs = []
                            for kh in range(2):
                                wv = slots[kh].t[:, 0:8192].rearrange("p (a b) -> p a b", a=16)
                                pairs += [(wv[:, kk, c * 128:(c + 1) * 128], actT.t[:, kh * 16 + kk, :]) for kk in range(16)]
                            mm_group(pgu[c].t[:], pairs, pgu[c].d, [slots[0].d, slots[1].d, actT.d])
                            if which == 0:
                                k.op(act, [pgu[c].d], [sl_d[c]], lambda h, c=c: h.activation(
                                    out=slb.t[:, c, :], in_=pgu[c].t[:], func=AF.Silu))
                            else:
                                k.op(dve, [pgu[c].d, sl_d[c]], [ab.d], lambda h, c=c, ab=ab: h.tensor_tensor(
                                    out=ab.t[:, c, :], in0=slb.t[:, c, :], in1=pgu[c].t[:], op=ALU.mult))

                def compute_d(j, dsl_):
                    ab = actb[j % 2]
                    gs_, dsi = gsl[j % 2], dsti[j % 2]
                    for dh in range(2):
                        sD_ = dsl_[dh]
                        vD = sD_.t[:, 0:8192].rearrange("p (a b) -> p a b", a=4)
                        for st in range(4):
                            yi = cnts["yi"] % 4
                            cnts["yi"] += 1
                            Yb = ybq[yi]
                            for db in range(4):
                                p = pd[cnts["di"] % 2]
                                cnts["di"] += 1
                                mm_group(p.t[:], [(ab.t[:, c, st * 128:(st + 1) * 128], vD[:, c, db * 512:(db + 1) * 512])
                                                  for c in range(4)], p.d, [sD_.d, ab.d])
                                k.op(dve, [p.d, gs_.d], [Yb.d], lambda h, p=p, st=st, db=db, Yb=Yb: h.tensor_scalar(
                                    out=Yb.t[:, db * 512:(db + 1) * 512], in0=p.t[:], scalar1=gs_.t[:, st:st + 1], scalar2=None, op0=ALU.mult))
                            k.idma(y2[:, :], bass.IndirectOffsetOnAxis(ap=dsi.t[:, st, dh:dh + 1], axis=0), Yb.t[:, :], None,
                                   [Yb.d, dsi.d], [], ybq_ds[yi], bounds_check=reg_y, oob_is_err=False)

                prep(0)
                gsl_, usl_ = weights_gu(0)
                dsl_ = weights_d(0)
                for j in range(ntiles):
                    compute_gu(j, gsl_, usl_)
                    if j + 1 < ntiles:
                        prep(j + 1)
                        gsl_, usl_ = weights_gu(j + 1)
                    compute_d(j, dsl_)
                    if j + 1 < ntiles:
                        dsl_ = weights_d(j + 1)
            k.barrier()

            with contextlib.ExitStack() as s4:
                NB4 = 3
                yp = [sb(f"yp{i}", [128, 8, 512], F32, s4) for i in range(NB4)]
                xsb = [sb(f"xsb{i}", [128, 512], F32, s4) for i in range(NB4)]
                gfp = [sb(f"gfpS{i}", [128, 512], F32, s4) for i in range(NB4)]
                fgp = [sb(f"fgpS{i}", [128, 512], F32, s4) for i in range(2)]
                ob = [sb(f"obS{i}", [128, 512], F32, s4) for i in range(2)]
                x2 = sb("x2", [128, D], F32, s4)
                junk = sb("junkS", [128, D], BF16, s4)
                p_ds = [k.dsem() for _ in range(8)]
                py_ds = [k.dsem() for _ in range(3 * NB4)]
                o_ds2 = [k.dsem() for _ in range(2)]
                yv = y_s.rearrange("(k t) d -> t k d", k=8)
                for i in range(16):
                    r0 = i * 128
                    for pc in range(8):
                        i2 = pc % 2
                        i4 = (i * 8 + pc) % NB4
                        Y, X, Gp = yp[i4], xsb[i4], gfp[i4]
                        k.dma(sp, Y.t[:], yv[r0:r0 + 128, :, pc * 512:(pc + 1) * 512], [], [Y.d], py_ds[i4])
                        k.dma(sp, X.t[:], x1_s[r0:r0 + 128, pc * 512:(pc + 1) * 512], [], [X.d], py_ds[NB4 + i4])
                        k.dma(sp, Gp.t[:], modrows[0:1, 5 * D + pc * 512:5 * D + (pc + 1) * 512].to_broadcast([128, 512]),
                              [], [Gp.d], py_ds[2 * NB4 + i4])
                        for lvl in (4, 2, 1):
                            eng = pool if lvl == 2 else dve
                            k.op(eng, [Y.d], [Y.d], lambda h, Y=Y, lvl=lvl: h.tensor_tensor(
                                out=Y.t[:, 0:lvl, :], in0=Y.t[:, 0:lvl, :], in1=Y.t[:, lvl:2 * lvl, :], op=ALU.add))
                        k.op(dve, [Y.d, Gp.d], [Y.d], lambda h, Y=Y, Gp=Gp: h.tensor_tensor(
                            out=Y.t[:, 0, :], in0=Y.t[:, 0, :], in1=Gp.t[:], op=ALU.mult))
                        k.op(pool, [Y.d, X.d], [x2.d], lambda h, Y=Y, X=X, pc=pc: h.tensor_tensor(
                            out=x2.t[:, pc * 512:(pc + 1) * 512], in0=Y.t[:, 0, :], in1=X.t[:], op=ALU.add))
                    zero(sm.t[:, 12:13])
                    k.op(act, [x2.d], [junk.d, sm.d], lambda h: h.activation(
                        out=junk.t[:], in_=x2.t[:], func=AF.Square, accum_out=sm.t[:, 12:13]))
                    rstd_from_ssq(sm.t[:, 12:13], sm.t[:, 13:14], D, [sm.d], [sm.d])
                    for pc in range(8):
                        i2 = pc % 2
                        k.dma(sp, fgp[i2].t[:], final_g[0:1, pc * 512:(pc + 1) * 512].to_broadcast([128, 512]),
                              [], [fgp[i2].d], p_ds[6 + i2])
                        k.op(dve, [x2.d, sm.d, fgp[i2].d], [ob[i2].d], lambda h, pc=pc, i2=i2: h.scalar_tensor_tensor(
                            out=ob[i2].t[:], in0=x2.t[:, pc * 512:(pc + 1) * 512], scalar=sm.t[:, 13:14], in1=fgp[i2].t[:],
                            op0=ALU.mult, op1=ALU.mult))
                        k.dma(sp, out[r0:r0 + 128, pc * 512:(pc + 1) * 512], ob[i2].t[:], [ob[i2].d], [], o_ds2[i2])
        k.barrier()
    return nc


def _tables(na_rpb, half):
    def pair_rows(pp):
        if 2 <= pp <= 17:
            r = 32 * half + 2 * (pp - 2)
            return (r, r + 1)
        if half == 0:
            return {0: (6, 7), 1: None, 18: (32, 33), 19: (34, 35)}[pp]
        return {0: (28, 29), 1: (30, 31), 18: None, 19: (56, 57)}[pp]
    rep = {0: 5, 1: 0, 2: 1, 3: 14, 4: 15}
    ro = np.zeros((5, 5, 128, 128), np.int64)
    co = np.zeros((5, 5, 128, 128), np.int64)
    mk = np.zeros((5, 5, 128, 128), np.float32)
    kk = np.arange(128)
    q = np.arange(128)
    for cls, j in rep.items():
        qrow = 32 * half + 2 * j + q // 64
        qcol = q % 64
        r0 = np.clip(qrow - 4, 0, 56)
        c0 = np.clip(qcol - 8, 0, 48)
        for bl in range(5):
            rows = pair_rows(j + bl)
            if rows is None:
                continue
            krow = np.array(rows)[kk // 64]
            kcol = kk % 64
            valid = ((krow[:, None] >= r0[None, :]) & (krow[:, None] < r0[None, :] + 8) &
                     (kcol[:, None] >= c0[None, :]) & (kcol[:, None] < c0[None, :] + 16))
            mk[cls, bl] = valid.astype(np.float32)
            ro[cls, bl] = np.clip(krow[:, None] - qrow[None, :] + 7, 0, 14)
            co[cls, bl] = np.clip(kcol[:, None] - qcol[None, :] + 15, 0, 30)
    bt = na_rpb[:, ro, co]
    bt = np.ascontiguousarray(bt.transpose(0, 3, 1, 2, 4))
    mt = np.ascontiguousarray(mk.transpose(2, 0, 1, 3))
    return bt, mt


def _consts():
    c = np.zeros((128, 512), np.float32)
    p = np.arange(128, dtype=np.float32)
    c[:, 0:64] = np.arange(64, dtype=np.float32)[None, :]
    c[:, 64:160] = np.arange(96, dtype=np.float32)[None, :]
    c[:, 160] = p
    for m in range(8):
        c[:, 192 + m] = 8 * p + m
    for cc in range(4):
        for h in range(2):
            c[:, 200 + cc * 2 + h] = (cc * 128 + p) * 2 + h
    return c


def make_in_maps(inp):
    f = lambda a: np.ascontiguousarray(np.asarray(a, dtype=np.float32))
    x = f(inp["x"]); ctx = f(inp["ctx"]); c = f(inp["c"]); c_ctx = f(inp["c_ctx"])
    shared = {
        "w_ada": f(inp["w_ada"][0]),
        "b_ada2": f(np.broadcast_to(f(inp["b_ada"][0])[None, :], (2, 6 * D))),
        "gmix_fm": f(f(inp["norm_mix_g"][0]).reshape(32, 128).T),
        "gffn_fm": f(f(inp["norm_ffn_g"][0]).reshape(32, 128).T),
        "w_in": f(inp["w_in"][0]),
        "ln_g": f(inp["sg_ln_g"][0])[None, :], "ln_b": f(inp["sg_ln_b"][0])[None, :],
        "wsT": f(f(inp["sg_w_s"][0]).transpose(2, 0, 1)),
        "bsT": f(f(inp["sg_b_s"][0]).T),
        "gout": f(np.concatenate([f(inp["out_g_na"][0]), f(inp["out_g_sg"][0])]))[None, :],
        "w_out": f(inp["w_out"][0]),
        "w_router": f(inp["w_router"][0]), "rbias": f(inp["router_bias"][0])[None, :],
        "we_gate": f(inp["we_gate"][0]), "we_up": f(inp["we_up"][0]), "we_down": f(inp["we_down"][0]),
        "ws_gate": f(inp["ws_gate"][0]), "ws_up": f(inp["ws_up"][0]), "ws_down": f(inp["ws_down"][0]),
        "final_g": f(inp["final_g"])[None, :],
        "gffn_pk": f(f(inp["norm_ffn_g"][0]).reshape(128, 32)),
        "gffn_row": f(inp["norm_ffn_g"][0])[None, :],
        "consts": _consts(), "tri_in": f(np.triu(np.ones((128, 128), np.float32), 1)),
    }
    rpb = f(inp["na_rpb"][0])
    tabs = [_tables(rpb, h) for h in range(2)]
    maps = []
    for core in range(8):
        b, half = core // 2, core % 2
        xb = x[b]
        if half == 0:
            prs = [(6, 7), (6, 7), (32, 33), (34, 35)]
        else:
            prs = [(28, 29), (30, 31), (56, 57), (56, 57)]
        halo = np.concatenate([xb[r * 64:(r + 1) * 64] for pr in prs for r in pr], axis=0)
        m = dict(shared)
        m["x_own"] = f(xb[half * NTOK:(half + 1) * NTOK])
        m["x_hc"] = f(np.concatenate([halo, ctx[b]], axis=0))
        cc = np.stack([c[b], c_ctx], axis=-1)
        m["cT"] = f(cc.reshape(32, 128, 2).transpose(1, 0, 2))
        m["bias_tab"], m["mask_tab"] = tabs[half]
        maps.append(m)
    return maps


_NC_CACHE = {}


def kernel(**inputs):
    maps = make_in_maps(inputs)
    if "nc" not in _NC_CACHE:
        _NC_CACHE["nc"] = build_nc()
    res = run_bass_kernel_spmd(_NC_CACHE["nc"], maps, core_ids=list(range(8)))
    outs = [np.asarray(r["out"], dtype=np.float32).reshape(NTOK, D) for r in res.results]
    full = np.empty((4, 4096, D), np.float32)
    for core in range(8):
        b, half = core // 2, core % 2
        full[b, half * NTOK:(half + 1) * NTOK] = outs[core]
    return full
```

```python
import contextlib
import numpy as np
import concourse.bass as bass
import concourse.mybir as mybir
from concourse.bass_utils import run_bass_kernel_spmd

F32 = mybir.dt.float32
BF16 = mybir.dt.bfloat16
AF = mybir.ActivationFunctionType
ALU = mybir.AluOpType
AX = mybir.AxisListType

D = 4096
NTOK = 2048
NPOS = 2816
NH = 16
NE = 64
EPS = 1e-6
TCLS = {0: 1, 1: 2, 14: 3, 15: 4}


class Eng:
    def __init__(s, name, h, sem):
        s.name, s.h, s.sem, s.n, s.seen = name, h, sem, 0, {}


class DSem:
    def __init__(s, sem):
        s.sem, s.n = sem, 0


class Dep:
    __slots__ = ("w", "r")

    def __init__(s):
        s.w = None
        s.r = {}


class K:
    def __init__(s, nc, es):
        s.nc, s.es = nc, es
        s.nsem = 0
        s.pe = Eng("pe", nc.tensor, s._sem())
        s.act = Eng("act", nc.scalar, s._sem())
        s.dve = Eng("dve", nc.vector, s._sem())
        s.pool = Eng("pool", nc.gpsimd, s._sem())
        s.sp = Eng("sp", nc.sync, s._sem())
        s.engs = [s.pe, s.act, s.dve, s.pool, s.sp]
        s.dsems = []

    def _sem(s):
        s.nsem += 1
        return s.es.enter_context(s.nc.semaphore(f"sem{s.nsem}"))

    def dsem(s):
        d = DSem(s._sem())
        s.dsems.append(d)
        return d

    def need(s, eng, st):
        if st is None:
            return
        obj, cnt = st
        if cnt <= 0 or eng.seen.get(obj, 0) >= cnt:
            return
        if obj is eng and eng.name in ("pe", "sp"):
            return
        if isinstance(obj, Eng):
            assert obj.n >= cnt, f"pending stamp on {obj.name}"
        eng.h.wait_ge(obj.sem, cnt)
        eng.seen[obj] = cnt

    def op(s, eng, reads, writes, fn, inc=True):
        for d in reads:
            s.need(eng, d.w)
        for d in writes:
            s.need(eng, d.w)
            for o, c in d.r.items():
                if o is not eng:
                    s.need(eng, (o, c))
        ins = fn(eng.h)
        if inc:
            eng.n += 1
            ins.then_inc(eng.sem, 1)
            st = (eng, eng.n)
        else:
            st = (eng, eng.n + 1)
        for d in reads:
            if d.r.get(eng, 0) < st[1]:
                d.r[eng] = st[1]
        for d in writes:
            d.w = st
            d.r = {}
        return ins

    def dma(s, q, out_ap, in_ap, reads, writes, ds):
        for d in reads:
            s.need(q, d.w)
        for d in writes:
            s.need(q, d.w)
            for o, c in d.r.items():
                s.need(q, (o, c))
        ins = q.h.dma_start(out=out_ap, in_=in_ap)
        ds.n += 16
        ins.then_inc(ds.sem, 16)
        for d in reads:
            d.r[ds] = ds.n
        for d in writes:
            d.w = (ds, ds.n)
            d.r = {}

    def idma(s, out_ap, out_off, in_ap, in_off, reads, writes, ds, **kw):
        q = s.pool
        for d in reads:
            s.need(q, d.w)
        for d in writes:
            s.need(q, d.w)
            for o, c in d.r.items():
                s.need(q, (o, c))
        ins = q.h.indirect_dma_start(out=out_ap, out_offset=out_off, in_=in_ap, in_offset=in_off, **kw)
        ds.n += 16
        ins.then_inc(ds.sem, 16)
        for d in reads:
            d.r[ds] = ds.n
        for d in writes:
            d.w = (ds, ds.n)
            d.r = {}

    def barrier(s):
        sts = [(e, e.n) for e in s.engs if e.n > 0] + [(d, d.n) for d in s.dsems if d.n > 0]
        for e in s.engs:
            for st in sts:
                s.need(e, st)


class Buf:
    def __init__(s, t):
        s.t = t
        s.d = Dep()


def build_nc(dbg=False, stages="0ABCDEFS", nblocks=4, nexp=65, sparse=True, ntiles=128, TS=256):
    if sparse:
        nexp = 1
    nc = bass.Bass("TRN2", target_bir_lowering=False)
    es = contextlib.ExitStack()

    def din(name, shape, dt=F32):
        return nc.dram_tensor(name, list(shape), dt, kind="ExternalInput").ap()

    def dscr(name, shape, dt):
        return nc.dram_tensor(name, list(shape), dt, kind="ExternalOutput" if dbg else "Internal").ap()

    x_own = din("x_own", [NTOK, D])
    x_hc = din("x_hc", [768, D])
    cT = din("cT", [128, 32, 2])
    w_ada = din("w_ada", [D, 6 * D])
    b_ada2 = din("b_ada2", [2, 6 * D])
    gmix_fm = din("gmix_fm", [128, 32])
    gffn_fm = din("gffn_fm", [128, 32])
    w_in = din("w_in", [D, 10240])
    bias_tab = din("bias_tab", [NH, 128, 5, 5, 128])
    mask_tab = din("mask_tab", [128, 5, 5, 128])
    ln_g = din("ln_g", [1, 2048])
    ln_b = din("ln_b", [1, 2048])
    wsT = din("wsT", [128, 16, 128])
    bsT = din("bsT", [128, 16])
    gout = din("gout", [1, D])
    w_out = din("w_out", [D, D])
    w_router = din("w_router", [D, NE])
    rbias = din("rbias", [1, NE])
    we_gate = din("we_gate", [NE, D, 512])
    we_up = din("we_up", [NE, D, 512])
    we_down = din("we_down", [NE, 512, D])
    ws_gate = din("ws_gate", [D, 512])
    ws_up = din("ws_up", [D, 512])
    ws_down = din("ws_down", [512, D])
    final_g = din("final_g", [1, D])
    consts = din("consts", [128, 512])
    tri_in = din("tri_in", [128, 128])
    gffn_pk = din("gffn_pk", [128, 32])
    gffn_row = din("gffn_row", [1, D])
    out = nc.dram_tensor("out", [NTOK, D], F32, kind="ExternalOutput").ap()

    modrows = dscr("modrows", [2, 6 * D], F32)
    qT_s = dscr("qT_s", [NH, 128, NTOK], BF16)
    kT_s = dscr("kT_s", [NH, 128, NPOS], BF16)
    v_s = dscr("v_s", [NPOS, 2048], BF16)
    u_s = dscr("u_s", [NTOK, 2048], BF16)
    z_s = dscr("z_s", [NTOK, 2048], BF16)
    attn_s = dscr("attn_s", [NTOK, 2048], BF16)
    x1_s = dscr("x1_s", [NTOK, D], F32)
    gates_s = dscr("gates_s", [NTOK, 65], F32) if dbg else None
    I32 = mybir.dt.int32
    NSLOT = ntiles * 512
    hfn_s = dscr("hfn_s", [NTOK + 128, D], BF16)
    gts_s = dscr("gts_s", [NTOK + 128, NE], F32)
    tos_d = nc.dram_tensor("tos_d", [ntiles * TS, 16], I32, kind="ExternalOutput" if dbg else "Internal").ap()
    y_s = dscr("y_s", [8 * NTOK, D], F32)

    with es:
        k = K(nc, es)
        pe, act, dve, pool, sp = k.pe, k.act, k.dve, k.pool, k.sp

        uid = [0]

        def sb(name, shape, dt=F32, stack=es):
            uid[0] += 1
            return Buf(stack.enter_context(nc.sbuf_tensor(f"{name}_{uid[0]}", list(shape), dt)))

        def ps(name, shape, dt=F32, stack=es):
            uid[0] += 1
            return Buf(stack.enter_context(nc.psum_tensor(f"{name}_{uid[0]}", list(shape), dt)))

        NR = 4
        ring = [sb(f"ring{i}", [128, 4096 * 2], BF16) for i in range(NR)]
        ring_ds = [k.dsem() for _ in range(NR)]
        ring_i = [0]

        def ring_load(src_ap, view):
            i = ring_i[0] % NR
            ring_i[0] += 1
            a, b = view
            dst = ring[i].t[:, 0:a * b].rearrange("p (a b) -> p a b", a=a)
            k.dma(pool, dst, src_ap, [], [ring[i].d], ring_ds[i])
            return ring[i], dst

        actT = sb("actT", [128, 32, 512], BF16)
        modfm = sb("modfm", [128, 192, 2])
        gmm = sb("gmm", [128, 32, 2])
        gmf = sb("gmf", [128, 32])
        gmix_sb = sb("gmix_sb", [128, 32])
        gffn_sb = sb("gffn_sb", [128, 32])
        identf = sb("identf", [128, 128])
        identb = sb("identb", [128, 128], BF16)
        sm = sb("sm", [128, 16])
        misc_ds = k.dsem()

        k.op(pool, [], [identf.d], lambda h: h.memset(identf.t[:], 0.0))
        k.op(pool, [], [identf.d], lambda h: h.affine_select(
            out=identf.t[:], in_=identf.t[:], pattern=[[-1, 128]], compare_op=ALU.not_equal,
            fill=1.0, base=0, channel_multiplier=1))
        k.op(dve, [identf.d], [identb.d], lambda h: h.tensor_copy(out=identb.t[:], in_=identf.t[:]))
        k.dma(sp, gmix_sb.t[:], gmix_fm, [], [gmix_sb.d], misc_ds)
        k.dma(sp, gffn_sb.t[:], gffn_fm, [], [gffn_sb.d], misc_ds)

        def zero(ap):
            k.op(dve, [], [sm.d], lambda h: h.memset(ap, 0.0))

        def rstd_from_ssq(ssq_ap, out_ap, n, deps_r, deps_w):
            k.op(dve, deps_r, deps_w, lambda h: h.tensor_scalar(
                out=out_ap, in0=ssq_ap, scalar1=1.0 / n, scalar2=EPS, op0=ALU.mult, op1=ALU.add))
            k.op(act, deps_w, deps_w, lambda h: h.activation(out=out_ap, in_=out_ap, func=AF.Sqrt))
            k.op(dve, deps_w, deps_w, lambda h: h.reciprocal(out=out_ap, in_=out_ap))

        def mm_group(out_ap, pairs, psd, rdeps):
            n = len(pairs)
            for i, (l, r) in enumerate(pairs):
                k.op(pe, rdeps, [psd], lambda h, l=l, r=r, i=i: h.matmul(
                    out_ap, lhsT=l, rhs=r, start=(i == 0), stop=(i == n - 1)), inc=(i == n - 1))

        if "0" in stages:
            with contextlib.ExitStack() as st0:
                cs = sb("cs", [128, 32, 2], F32, st0)
                csb = sb("csb", [128, 32, 2], BF16, st0)
                id2 = sb("id2", [2, 2], F32, st0)
                bch = [sb(f"bch{i}", [2, 256], F32, st0) for i in range(2)]
                bch_ds = [k.dsem() for _ in range(2)]
                mev = [sb(f"mev{i}", [2, 256], F32, st0) for i in range(2)]
                mev_ds = [k.dsem() for _ in range(2)]
                pmod = [ps(f"pmod{i}", [2, 256], F32, st0) for i in range(2)]
                pfm = [ps(f"pfm{i}", [128, 2, 2], F32, st0) for i in range(2)]
                if sparse:
                    zrow = sb("zrow", [128, D], BF16, st0)
                    k.op(pool, [], [zrow.d], lambda h: h.memset(zrow.t[:], 0.0))
                    k.dma(sp, hfn_s[NTOK:NTOK + 128, :], zrow.t[:], [zrow.d], [], misc_ds)
                    zg = sb("zg", [128, NE], F32, st0)
                    k.op(pool, [], [zg.d], lambda h: h.memset(zg.t[:], 0.0))
                    k.dma(sp, gts_s[NTOK:NTOK + 128, :], zg.t[:], [zg.d], [], misc_ds)
                k.dma(sp, cs.t[:], cT, [], [cs.d], misc_ds)
                k.op(act, [cs.d], [csb.d], lambda h: h.activation(out=csb.t[:], in_=cs.t[:], func=AF.Silu))
                k.op(dve, [identf.d], [id2.d], lambda h: h.tensor_copy(out=id2.t[:], in_=identf.t[0:2, 0:2]))
                for cc in range(96):
                    c0 = cc * 256
                    i = cc % 2
                    slot, wv = ring_load(w_ada[:, c0:c0 + 256].rearrange("(k p) f -> p k f", p=128), (32, 256))
                    k.dma(sp, bch[i].t[:], b_ada2[:, c0:c0 + 256], [], [bch[i].d], bch_ds[i])
                    mm_group(pmod[i].t[:], [(csb.t[:, kk, :], wv[:, kk, :]) for kk in range(32)],
                             pmod[i].d, [csb.d, slot.d])
                    k.op(dve, [pmod[i].d, bch[i].d], [mev[i].d], lambda h, i=i: h.tensor_tensor(
                        out=mev[i].t[:], in0=pmod[i].t[:], in1=bch[i].t[:], op=ALU.add))
                    k.dma(sp, modrows[:, c0:c0 + 256], mev[i].t[:], [mev[i].d], [], mev_ds[i])
                    for hh in range(2):
                        mm_group(pfm[i].t[:, hh, :], [(mev[i].t[0:2, hh * 128:(hh + 1) * 128], id2.t[:])],
                                 pfm[i].d, [mev[i].d, id2.d])
                    k.op(act, [pfm[i].d], [modfm.d], lambda h, i=i, cc=cc: h.copy(
                        out=modfm.t[:, 2 * cc:2 * cc + 2, :], in_=pfm[i].t[:]))
                for j in range(2):
                    k.op(dve, [modfm.d, gmix_sb.d], [gmm.d], lambda h, j=j: h.scalar_tensor_tensor(
                        out=gmm.t[:, :, j], in0=modfm.t[:, 32:64, j], scalar=1.0, in1=gmix_sb.t[:],
                        op0=ALU.add, op1=ALU.mult))
                k.op(dve, [modfm.d, gffn_sb.d], [gmf.d], lambda h: h.scalar_tensor_tensor(
                    out=gmf.t[:], in0=modfm.t[:, 128:160, 0], scalar=1.0, in1=gffn_sb.t[:],
                    op0=ALU.add, op1=ALU.mult))
            k.barrier()

        if "A" in stages:
            with contextlib.ExitStack() as sA:
                xt = [sb(f"xt{i}", [128, D], F32, sA) for i in range(2)]
                xt_ds = [k.dsem() for _ in range(2)]
                junk = sb("junkA", [128, D], BF16, sA)
                stg = [sb(f"stgA{i}", [128, 4, 256], BF16, sA) for i in range(2)]
                stg_ds = [k.dsem() for _ in range(2)]
                ptr = [ps(f"ptrA{i}", [128, 4, 128], F32, sA) for i in range(2)]
                pmm = [ps(f"pmmA{i}", [128, 512], F32, sA) for i in range(4)]
                tix = [0]
                pix = [0]
                six = [0]
                for g in range(6):
                    ntok = 256 if g == 5 else 512
                    nst = ntok // 128
                    j = 1 if g == 5 else 0
                    for st in range(nst):
                        xb = xt[tix[0] % 2]
                        xds = xt_ds[tix[0] % 2]
                        tix[0] += 1
                        src = x_own[g * 512 + st * 128: g * 512 + (st + 1) * 128, :] if g < 4 else \
                            x_hc[(g - 4) * 512 + st * 128:(g - 4) * 512 + (st + 1) * 128, :]
                        k.dma(sp, xb.t[:], src, [], [xb.d], xds)
                        zero(sm.t[:, 0:1])
                        k.op(act, [xb.d], [junk.d, sm.d], lambda h, xb=xb: h.activation(
                            out=junk.t[:], in_=xb.t[:], func=AF.Square, accum_out=sm.t[:, 0:1]))
                        rstd_from_ssq(sm.t[:, 0:1], sm.t[:, 1:2], D, [sm.d], [sm.d])
                        k.op(dve, [sm.d, xb.d], [xb.d], lambda h, xb=xb: h.tensor_scalar(
                            out=xb.t[:], in0=xb.t[:], scalar1=sm.t[:, 1:2], scalar2=None, op0=ALU.mult))
                        for kk in range(32):
                            p = ptr[(kk // 4) % 2]
                            k.op(pe, [xb.d, identf.d], [p.d], lambda h, p=p, kk=kk, xb=xb: h.transpose(
                                p.t[:, kk % 4, :], xb.t[:, kk * 128:(kk + 1) * 128], identf.t[:]),
                                inc=(kk % 4 == 3))
                            if kk % 4 == 3:
                                for q4 in range(4):
                                    k4 = kk - 3 + q4
                                    k.op(act, [p.d, gmm.d, modfm.d], [actT.d], lambda h, p=p, q4=q4, k4=k4, st=st, j=j: h.activation(
                                        out=actT.t[:, k4, st * 128:(st + 1) * 128], in_=p.t[:, q4, :], func=AF.Identity,
                                        scale=gmm.t[:, k4, j:j + 1], bias=modfm.t[:, k4, j:j + 1]))
                    chunks = range(40) if g < 4 else range(8, 24)
                    for cc in chunks:
                        c0 = cc * 256
                        slot, wv = ring_load(w_in[:, c0:c0 + 256].rearrange("(k p) f -> p k f", p=128), (32, 256))
                        if cc < 16:
                            for hh in range(2):
                                head = (cc % 8) * 2 + hh
                                pm = pmm[pix[0] % 4]
                                pix[0] += 1
                                mm_group(pm.t[:, 0:ntok], [(wv[:, kk, hh * 128:(hh + 1) * 128], actT.t[:, kk, 0:ntok])
                                                            for kk in range(32)], pm.d, [slot.d, actT.d])
                                sg_ = stg[six[0] % 2]
                                sds = stg_ds[six[0] % 2]
                                six[0] += 1
                                sview = sg_.t[:].rearrange("p a b -> p (a b)")[:, 0:512]
                                if cc < 8:
                                    k.op(act, [pm.d], [sg_.d], lambda h, pm=pm, sview=sview, ntok=ntok: h.activation(
                                        out=sview[:, 0:ntok], in_=pm.t[:, 0:ntok], func=AF.Copy, scale=float(128 ** -0.5)))
                                    k.dma(sp, qT_s[head, :, g * 512:(g + 1) * 512], sview, [sg_.d], [], sds)
                                else:
                                    k.op(dve, [pm.d], [sg_.d], lambda h, pm=pm, sview=sview, ntok=ntok: h.tensor_copy(
                                        out=sview[:, 0:ntok], in_=pm.t[:, 0:ntok]))
                                    if g < 4:
                                        segs = [(0, 512, 256 + 512 * g)]
                                    elif g == 4:
                                        segs = [(0, 256, 0), (256, 512, 2304)]
                                    else:
                                        segs = [(0, 256, 2560)]
                                    for (a, b, p0) in segs:
                                        k.dma(sp, kT_s[head, :, p0:p0 + (b - a)], sview[:, a:b], [sg_.d], [], sds)
                        else:
                            sg_ = stg[six[0] % 2]
                            sds = stg_ds[six[0] % 2]
                            six[0] += 1
                            for st in range(nst):
                                pm = pmm[pix[0] % 4]
                                pix[0] += 1
                                mm_group(pm.t[:, 0:256], [(actT.t[:, kk, st * 128:(st + 1) * 128], wv[:, kk, :])
                                                           for kk in range(32)], pm.d, [slot.d, actT.d])
                                if cc < 24:
                                    k.op(dve, [pm.d], [sg_.d], lambda h, pm=pm, sg_=sg_, st=st: h.tensor_copy(
                                        out=sg_.t[:, st, :], in_=pm.t[:, 0:256]))
                                else:
                                    k.op(act, [pm.d], [sg_.d], lambda h, pm=pm, sg_=sg_, st=st: h.activation(
                                        out=sg_.t[:, st, :], in_=pm.t[:, 0:256], func=AF.Gelu_apprx_tanh))
                            if cc < 24:
                                vc = (cc - 16) * 256
                                if g < 4:
                                    rows = [(0, 4, 256 + 512 * g)]
                                elif g == 4:
                                    rows = [(0, 2, 0), (2, 4, 2304)]
                                else:
                                    rows = [(0, 2, 2560)]
                                for (a, b, p0) in rows:
                                    k.dma(sp, v_s[p0:p0 + (b - a) * 128, vc:vc + 256].rearrange("(s p) f -> p s f", p=128),
                                          sg_.t[:, a:b, :], [sg_.d], [], sds)
                            else:
                                dst = u_s if cc < 32 else z_s
                                uc = ((cc - 24) % 8) * 256
                                k.dma(sp, dst[g * 512:(g + 1) * 512, uc:uc + 256].rearrange("(s p) f -> p s f", p=128),
                                      sg_.t[:], [sg_.d], [], sds)
            k.barrier()

        if "B" in stages:
            with contextlib.ExitStack() as sB:
                mask = sb("mask", [128, 5, 640], F32, sB)
                k.dma(sp, mask.t[:], mask_tab.rearrange("p c b q -> p c (b q)"), [], [mask.d], misc_ds)
                k.op(pool, [mask.d], [mask.d], lambda h: h.tensor_scalar(
                    out=mask.t[:], in0=mask.t[:], scalar1=30000.0, scalar2=-30000.0, op0=ALU.mult, op1=ALU.add))
                KT = [sb(f"KT{i}", [128, NPOS], BF16, sB) for i in range(2)]
                QT = [sb(f"QT{i}", [128, NTOK], BF16, sB) for i in range(2)]
                VH = [sb(f"VH{i}", [128, 22, 132], BF16, sB) for i in range(2)]
                BI = [sb(f"BI{i}", [128, 5, 640], F32, sB) for i in range(2)]
                AH = [sb(f"AH{i}", [128, 16, 128], BF16, sB) for i in range(2)]
                hd_ds = [k.dsem() for _ in range(2)]
                ah_ds = [k.dsem() for _ in range(2)]
                ssb = [sb(f"ssb{i}", [128, 640], F32, sB) for i in range(2)]
                e1 = [sb(f"e1{i}", [128, 640], F32, sB) for i in range(2)]
                pT = [sb(f"pT{i}", [128, 896], BF16, sB) for i in range(2)]
                rec = sb("rec", [128, 2], F32, sB)
                Sps = [ps(f"Sps{i}", [128, 1024], F32, sB) for i in range(2)]
                Ops = [ps(f"Ops{i}", [128, 512], F32, sB) for i in range(2)]
                for i in range(2):
                    k.op(pool, [], [VH[i].d], lambda h, i=i: h.memset(VH[i].t[:, :, 128:129], 1.0))
                ui = 0
                for hd in range(NH):
                    b_ = hd % 2
                    deps_w = [KT[b_].d, QT[b_].d, VH[b_].d, BI[b_].d]
                    k.dma(sp, KT[b_].t[:], kT_s[hd], [], [KT[b_].d], hd_ds[b_])
                    k.dma(sp, QT[b_].t[:], qT_s[hd], [], [QT[b_].d], hd_ds[b_])
                    k.dma(sp, VH[b_].t[:, :, 0:128], v_s[:, hd * 128:(hd + 1) * 128].rearrange("(b p) d -> p b d", p=128),
                          [], [VH[b_].d], hd_ds[b_])
                    k.dma(sp, BI[b_].t[:], bias_tab[hd].rearrange("p c b q -> p c (b q)"), [], [BI[b_].d], hd_ds[b_])
                    fin = (hd_ds[b_], hd_ds[b_].n)
                    for d in deps_w:
                        d.w = fin
                    k.op(pool, [BI[b_].d, mask.d], [BI[b_].d], lambda h, b_=b_: h.tensor_tensor(
                        out=BI[b_].t[:], in0=BI[b_].t[:], in1=mask.t[:], op=ALU.add))
                    for j in range(16):
                        cls = TCLS.get(j, 0)
                        u2 = ui % 2
                        ui += 1
                        S, O = Sps[u2], Ops[u2]
                        blocks = [j + bl for bl in range(5)] + [20, 21]
                        for bi_, pb in enumerate(blocks):
                            k.op(pe, [KT[b_].d, QT[b_].d], [S.d], lambda h, S=S, bi_=bi_, pb=pb, j=j, b_=b_: h.matmul(
                                S.t[:, bi_ * 128:(bi_ + 1) * 128], lhsT=KT[b_].t[:, pb * 128:(pb + 1) * 128],
                                rhs=QT[b_].t[:, j * 128:(j + 1) * 128], start=True, stop=True), inc=(bi_ == 6))
                        k.op(dve, [S.d, BI[b_].d], [ssb[u2].d], lambda h, S=S, u2=u2, cls=cls, b_=b_: h.tensor_tensor(
                            out=ssb[u2].t[:], in0=S.t[:, 0:640], in1=BI[b_].t[:, cls, :], op=ALU.add))
                        k.op(act, [S.d], [pT[u2].d], lambda h, S=S, u2=u2: h.activation(
                            out=pT[u2].t[:, 640:896], in_=S.t[:, 640:896], func=AF.Exp))
                        k.op(act, [ssb[u2].d], [pT[u2].d], lambda h, u2=u2: h.activation(
                            out=pT[u2].t[:, 0:640], in_=ssb[u2].t[:], func=AF.Exp))
                        for bi_, pb in enumerate(blocks):
                            k.op(pe, [pT[u2].d, VH[b_].d], [O.d], lambda h, O=O, bi_=bi_, pb=pb, u2=u2, b_=b_: h.matmul(
                                O.t[:, 0:129], lhsT=pT[u2].t[:, bi_ * 128:(bi_ + 1) * 128], rhs=VH[b_].t[:, pb, 0:129],
                                start=(bi_ == 0), stop=(bi_ == 6)), inc=(bi_ == 6))
                        k.op(dve, [O.d], [rec.d], lambda h, O=O, u2=u2: h.reciprocal(
                            out=rec.t[:, u2:u2 + 1], in_=O.t[:, 128:129]))
                        k.op(dve, [O.d, rec.d], [AH[b_].d], lambda h, O=O, u2=u2, j=j, b_=b_: h.tensor_scalar(
                            out=AH[b_].t[:, j, :], in0=O.t[:, 0:128], scalar1=rec.t[:, u2:u2 + 1], scalar2=None, op0=ALU.mult))
                    k.dma(sp, attn_s[:, hd * 128:(hd + 1) * 128].rearrange("(j p) d -> p j d", p=128), AH[b_].t[:],
                          [AH[b_].d], [], ah_ds[b_])
            k.barrier()

        gall = sb("gall", [128, 16, NE])
        cst = sb("cst", [128, 512])
        eot = sb("eot", [128, 128])
        eotk = sb("eotk", [128, 128])
        sBlk = contextlib.ExitStack()
        wr_b = sb("wr_b", [128, 32, NE], BF16, sBlk)
        rb_bc = sb("rb_bc", [128, NE], F32, sBlk)
        wsT_b = sb("wsT_b", [128, 16, 128], BF16, sBlk)
        bsT_sb = sb("bsT_sb", [128, 16], F32, sBlk)
        if sparse:
            k.dma(sp, cst.t[:], consts, [], [cst.d], misc_ds)
        if any(s_ in stages for s_ in "CDEF"):
            k.dma(pool, wr_b.t[:], w_router.rearrange("(k p) e -> p k e", p=128), [], [wr_b.d], misc_ds)
            k.dma(sp, rb_bc.t[:], rbias.to_broadcast([128, NE]), [], [rb_bc.d], misc_ds)
            k.dma(pool, wsT_b.t[:], wsT, [], [wsT_b.d], misc_ds)
            k.dma(sp, bsT_sb.t[:], bsT, [], [bsT_sb.d], misc_ds)
            fin = (misc_ds, misc_ds.n)
            for b_ in (wr_b, rb_bc, wsT_b, bsT_sb):
                b_.d.w = fin

        for tb in range(nblocks if any(s_ in stages for s_ in "CDEF") else 0):
            if "C" in stages:
                with contextlib.ExitStack() as sC:
                    lng = sb("lng", [128, 2048], F32, sC)
                    lnb = sb("lnb", [128, 2048], F32, sC)
                    gob = sb("gob", [128, D], F32, sC)
                    tds = k.dsem() if tb == 0 else tds_keep[0]
                    if tb == 0:
                        tds_keep = [tds]
                    k.dma(sp, lng.t[:], ln_g.to_broadcast([128, 2048]), [], [lng.d], tds)
                    k.dma(sp, lnb.t[:], ln_b.to_broadcast([128, 2048]), [], [lnb.d], tds)
                    k.dma(sp, gob.t[:], gout.to_broadcast([128, D]), [], [gob.d], tds)
                    fin = (tds, tds.n)
                    for b_ in (lng, lnb, gob):
                        b_.d.w = fin
                    if tb == 0:
                        ld_ds = [k.dsem() for _ in range(2)]
                    ut = [sb(f"ut{i}", [128, 2048], BF16, sC) for i in range(2)]
                    zt = [sb(f"zt{i}", [128, 2048], BF16, sC) for i in range(2)]
                    at = [sb(f"at{i}", [128, 2048], BF16, sC) for i in range(2)]
                    fa = sb("fa", [128, 2048], F32, sC)
                    zn = sb("zn", [128, 2048], BF16, sC)
                    y = sb("y", [128, D], BF16, sC)
                    mix = ps("mix", [128, 2048], F32, sC)
                    ptc = [ps(f"ptc{i}", [128, 8, 128], BF16, sC) for i in range(2)]
                    for st in range(4):
                        r0 = tb * 512 + st * 128
                        i2 = st % 2
                        k.dma(sp, ut[i2].t[:], u_s[r0:r0 + 128, :], [], [ut[i2].d], ld_ds[i2])
                        k.dma(sp, zt[i2].t[:], z_s[r0:r0 + 128, :], [], [zt[i2].d], ld_ds[i2])
                        k.dma(sp, at[i2].t[:], attn_s[r0:r0 + 128, :], [], [at[i2].d], ld_ds[i2])
                        fin = (ld_ds[i2], ld_ds[i2].n)
                        for b_ in (ut[i2], zt[i2], at[i2]):
                            b_.d.w = fin
                        U, Z, A_ = ut[i2], zt[i2], at[i2]
                        zero(sm.t[:, 2:4])
                        k.op(act, [Z.d], [fa.d, sm.d], lambda h, Z=Z: h.activation(
                            out=fa.t[:], in_=Z.t[:], func=AF.Identity, accum_out=sm.t[:, 2:3]))
                        k.op(act, [Z.d], [y.d, sm.d], lambda h, Z=Z: h.activation(
                            out=y.t[:, 0:2048], in_=Z.t[:], func=AF.Square, accum_out=sm.t[:, 3:4]))
                        k.op(dve, [sm.d], [sm.d], lambda h: h.tensor_scalar(
                            out=sm.t[:, 4:5], in0=sm.t[:, 2:3], scalar1=1.0 / 2048, scalar2=None, op0=ALU.mult))
                        k.op(dve, [sm.d], [sm.d], lambda h: h.tensor_tensor(
                            out=sm.t[:, 5:6], in0=sm.t[:, 4:5], in1=sm.t[:, 4:5], op=ALU.mult))
                        k.op(dve, [sm.d], [sm.d], lambda h: h.scalar_tensor_tensor(
                            out=sm.t[:, 6:7], in0=sm.t[:, 3:4], scalar=1.0 / 2048, in1=sm.t[:, 5:6],
                            op0=ALU.mult, op1=ALU.subtract))
                        k.op(dve, [sm.d], [sm.d], lambda h: h.tensor_scalar(
                            out=sm.t[:, 6:7], in0=sm.t[:, 6:7], scalar1=EPS, scalar2=None, op0=ALU.add))
                        k.op(act, [sm.d], [sm.d], lambda h: h.activation(out=sm.t[:, 6:7], in_=sm.t[:, 6:7], func=AF.Sqrt))
                        k.op(dve, [sm.d], [sm.d], lambda h: h.reciprocal(out=sm.t[:, 6:7], in_=sm.t[:, 6:7]))
                        k.op(dve, [sm.d], [sm.d], lambda h: h.scalar_tensor_tensor(
                            out=sm.t[:, 7:8], in0=sm.t[:, 4:5], scalar=-1.0, in1=sm.t[:, 6:7],
                            op0=ALU.mult, op1=ALU.mult))
                        k.op(act, [sm.d, fa.d], [fa.d], lambda h: h.activation(
                            out=fa.t[:], in_=fa.t[:], func=AF.Identity, scale=sm.t[:, 6:7], bias=sm.t[:, 7:8]))
                        k.op(dve, [fa.d, lng.d], [fa.d], lambda h: h.tensor_tensor(
                            out=fa.t[:], in0=fa.t[:], in1=lng.t[:], op=ALU.mult))
                        k.op(pool, [fa.d, lnb.d], [zn.d], lambda h: h.tensor_tensor(
                            out=zn.t[:], in0=fa.t[:], in1=lnb.t[:], op=ALU.add))
                        for g in range(16):
                            k.op(pe, [zn.d, wsT_b.d], [mix.d], lambda h, g=g: h.matmul(
                                mix.t[:, g * 128:(g + 1) * 128], lhsT=wsT_b.t[:, g, :], rhs=zn.t[:, g * 128:(g + 1) * 128],
                                start=True, stop=True), inc=(g == 15))
                        for g in range(16):
                            k.op(dve, [mix.d, bsT_sb.d, U.d], [fa.d], lambda h, g=g, U=U: h.scalar_tensor_tensor(
                                out=fa.t[:, g * 128:(g + 1) * 128], in0=mix.t[:, g * 128:(g + 1) * 128],
                                scalar=bsT_sb.t[:, g:g + 1], in1=U.t[:, g * 128:(g + 1) * 128], op0=ALU.add, op1=ALU.mult))
                        zero(sm.t[:, 8:10])
                        k.op(act, [fa.d], [y.d, sm.d], lambda h: h.activation(
                            out=y.t[:, 2048:4096], in_=fa.t[:], func=AF.Square, accum_out=sm.t[:, 9:10]))
                        k.op(act, [A_.d], [y.d, sm.d], lambda h, A_=A_: h.activation(
                            out=y.t[:, 0:2048], in_=A_.t[:], func=AF.Square, accum_out=sm.t[:, 8:9]))
                        rstd_from_ssq(sm.t[:, 8:10], sm.t[:, 10:12], 2048, [sm.d], [sm.d])
                        k.op(dve, [A_.d, sm.d, gob.d], [y.d], lambda h, A_=A_: h.scalar_tensor_tensor(
                            out=y.t[:, 0:2048], in0=A_.t[:], scalar=sm.t[:, 10:11], in1=gob.t[:, 0:2048],
                            op0=ALU.mult, op1=ALU.mult))
                        k.op(dve, [fa.d, sm.d, gob.d], [y.d], lambda h: h.scalar_tensor_tensor(
                            out=y.t[:, 2048:4096], in0=fa.t[:], scalar=sm.t[:, 11:12], in1=gob.t[:, 2048:4096],
                            op0=ALU.mult, op1=ALU.mult))
                        for k8 in range(4):
                            p = ptc[k8 % 2]
                            for q8 in range(8):
                                kk = k8 * 8 + q8
                                k.op(pe, [y.d, identb.d], [p.d], lambda h, p=p, q8=q8, kk=kk: h.transpose(
                                    p.t[:, q8, :], y.t[:, kk * 128:(kk + 1) * 128], identb.t[:]), inc=(q8 == 7))
                            k.op(act, [p.d], [actT.d], lambda h, p=p, k8=k8, st=st: h.copy(
                                out=actT.t[:, k8 * 8:(k8 + 1) * 8, st * 128:(st + 1) * 128], in_=p.t[:]))
                k.barrier()

            with contextlib.ExitStack() as sD:
                acc = [sb(f"acc{i}", [128, D], F32, sD) for i in range(4)]
                gates = sb("gates", [128, 4, 65], F32, sD)
                if tb == 0:
                    acc_ds = [k.dsem() for _ in range(4)]
                    pc_ds = [k.dsem() for _ in range(2)]
                    x1_ds = [k.dsem() for _ in range(4)]
                    o_ds = [k.dsem() for _ in range(2)]
                x1d = [Dep() for _ in range(4)]
                if "D" in stages:
                    with contextlib.ExitStack() as sDD:
                        gmp = [sb(f"gmp{i}", [128, 256], F32, sDD) for i in range(2)]
                        tmp = [sb(f"tmpD{i}", [128, 256], F32, sDD) for i in range(2)]
                        hfn = [sb(f"hfn{i}", [128, 512], F32, sDD) for i in range(2)]
                        junk = sb("junkD", [128, D], BF16, sDD)
                        hfrow = junk
                        mp = [sb(f"mp{i}", [128, 512], F32, sDD) for i in range(3)] if sparse else None
                        if tb == 0:
                            hf_ds = k.dsem()
                            hf_keep = [hf_ds]
                            mp_ds = [k.dsem() for _ in range(3)]
                        hf_ds = hf_keep[0]
                        rt = sb("rt", [128, 512], F32, sDD)
                        pmm = [ps(f"pmmD{i}", [128, 512], F32, sDD) for i in range(4)]
                        ptr = [ps(f"ptrD{i}", [128, 4, 128], F32, sDD) for i in range(2)]
                        plg = ps("plg", [128, NE], F32, sDD)
                        for st in range(4):
                            r0 = tb * 512 + st * 128
                            k.dma(sp, acc[st].t[:], x_own[r0:r0 + 128, :], [], [acc[st].d], acc_ds[st])
                        pix = 0
                        for cc in range(16):
                            c0 = cc * 256
                            slot, wv = ring_load(w_out[:, c0:c0 + 256].rearrange("(k p) f -> p k f", p=128), (32, 256))
                            gp = gmp[cc % 2]
                            k.dma(sp, gp.t[:], modrows[0:1, 2 * D + c0:2 * D + c0 + 256].to_broadcast([128, 256]),
                                  [], [gp.d], pc_ds[cc % 2])
                            for st in range(4):
                                pm = pmm[pix % 4]
                                tm = tmp[pix % 2]
                                pix += 1
                                mm_group(pm.t[:, 0:256], [(actT.t[:, kk, st * 128:(st + 1) * 128], wv[:, kk, :])
                                                           for kk in range(32)], pm.d, [slot.d, actT.d])
                                k.op(dve, [pm.d, gp.d], [tm.d], lambda h, pm=pm, tm=tm, gp=gp: h.tensor_tensor(
                                    out=tm.t[:], in0=pm.t[:, 0:256], in1=gp.t[:], op=ALU.mult))
                                k.op(pool, [tm.d, acc[st].d], [acc[st].d], lambda h, tm=tm, st=st, c0=c0: h.tensor_tensor(
                                    out=acc[st].t[:, c0:c0 + 256], in0=acc[st].t[:, c0:c0 + 256], in1=tm.t[:], op=ALU.add))
                        for st in range(4):
                            r0 = tb * 512 + st * 128
                            A_ = acc[st]
                            k.dma(sp, x1_s[r0:r0 + 128, :], A_.t[:], [A_.d], [x1d[st]], x1_ds[st])
                            zero(sm.t[:, 0:1])
                            k.op(act, [A_.d], [junk.d, sm.d], lambda h, A_=A_: h.activation(
                                out=junk.t[:], in_=A_.t[:], func=AF.Square, accum_out=sm.t[:, 0:1]))
                            rstd_from_ssq(sm.t[:, 0:1], sm.t[:, 1:2], D, [sm.d], [sm.d])
                            for k4 in range(8):
                                hb = hfn[k4 % 2]
                                p = ptr[k4 % 2]
                                k.op(dve, [A_.d, sm.d], [hb.d], lambda h, A_=A_, hb=hb, k4=k4: h.tensor_scalar(
                                    out=hb.t[:], in0=A_.t[:, k4 * 512:(k4 + 1) * 512], scalar1=sm.t[:, 1:2], scalar2=None,
                                    op0=ALU.mult))
                                if sparse:
                                    cs_ = slice(k4 * 512, (k4 + 1) * 512)
                                    k.dma(sp, mp[0].t[:], modrows[0:1, 4 * D + k4 * 512:4 * D + (k4 + 1) * 512].to_broadcast([128, 512]), [], [mp[0].d], mp_ds[0])
                                    k.dma(sp, mp[1].t[:], gffn_row[0:1, cs_].to_broadcast([128, 512]), [], [mp[1].d], mp_ds[1])
                                    k.dma(sp, mp[2].t[:], modrows[0:1, 3 * D + k4 * 512:3 * D + (k4 + 1) * 512].to_broadcast([128, 512]), [], [mp[2].d], mp_ds[2])
                                    k.op(dve, [mp[0].d, mp[1].d], [mp[0].d], lambda h: h.scalar_tensor_tensor(
                                        out=mp[0].t[:], in0=mp[0].t[:], scalar=1.0, in1=mp[1].t[:], op0=ALU.add, op1=ALU.mult))
                                    k.op(pool, [mp[0].d, hb.d], [mp[0].d], lambda h, hb=hb: h.tensor_tensor(
                                        out=mp[0].t[:], in0=mp[0].t[:], in1=hb.t[:], op=ALU.mult))
                                    k.op(pool, [mp[0].d, mp[2].d], [hfrow.d], lambda h, cs_=cs_: h.tensor_tensor(
                                        out=hfrow.t[:, cs_], in0=mp[0].t[:], in1=mp[2].t[:], op=ALU.add))
                                for q4 in range(4):
                                    k.op(pe, [hb.d, identf.d], [p.d], lambda h, p=p, q4=q4, hb=hb: h.transpose(
                                        p.t[:, q4, :], hb.t[:, q4 * 128:(q4 + 1) * 128], identf.t[:]), inc=(q4 == 3))
                                for q4 in range(4):
                                    kk = k4 * 4 + q4
                                    k.op(act, [p.d, gmf.d, modfm.d], [actT.d], lambda h, p=p, q4=q4, kk=kk, st=st: h.activation(
                                        out=actT.t[:, kk, st * 128:(st + 1) * 128], in_=p.t[:, q4, :], func=AF.Identity,
                                        scale=gmf.t[:, kk:kk + 1], bias=modfm.t[:, 96 + kk, 0:1]))
                            if sparse:
                                k.dma(sp, hfn_s[r0:r0 + 128, :], hfrow.t[:], [hfrow.d], [], hf_ds)
                            mm_group(plg.t[:], [(actT.t[:, kk, st * 128:(st + 1) * 128], wr_b.t[:, kk, :]) for kk in range(32)],
                                     plg.d, [actT.d, wr_b.d])
                            S_ = rt.t[:, 0:64]
                            C_ = rt.t[:, 64:128]
                            M8 = rt.t[:, 128:192]
                            GS = rt.t[:, 192:200]
                            G8 = rt.t[:, 200:208]
                            GM = rt.t[:, 208:216]
                            CM = rt.t[:, 256:320]
                            T8 = rt.t[:, 320:328]
                            SEL = rt.t[:, 384:448]
                            WS = rt.t[:, 448:450]
                            rd = [rt.d]
                            k.op(act, [plg.d], rd, lambda h: h.activation(out=S_, in_=plg.t[:], func=AF.Sigmoid))
                            k.op(dve, rd + [rb_bc.d], rd, lambda h: h.tensor_tensor(out=C_, in0=S_, in1=rb_bc.t[:], op=ALU.add))
                            for g in range(8):
                                k.op(dve, rd, rd, lambda h, g=g: h.max(out=rt.t[:, 128 + g * 8:136 + g * 8],
                                                                       in_=rt.t[:, 64 + g * 8:72 + g * 8]))
                            m3 = M8.rearrange("p (g e) -> p g e", e=8)
                            k.op(dve, rd, rd, lambda h: h.tensor_tensor(out=GS, in0=m3[:, :, 0], in1=m3[:, :, 1], op=ALU.add))
                            k.op(dve, rd, rd, lambda h: h.max(out=G8, in_=GS))
                            k.op(dve, rd, rd, lambda h: h.tensor_scalar(out=GM, in0=GS, scalar1=rt.t[:, 203:204], scalar2=None,
                                                                        op0=ALU.is_ge))
                            for g in range(8):
                                k.op(dve, rd, rd, lambda h, g=g: h.tensor_scalar(
                                    out=rt.t[:, 256 + g * 8:264 + g * 8], in0=rt.t[:, 64 + g * 8:72 + g * 8], scalar1=10.0,
                                    scalar2=rt.t[:, 208 + g:209 + g], op0=ALU.add, op1=ALU.mult))
                            k.op(dve, rd, rd, lambda h: h.max(out=T8, in_=CM))
                            k.op(dve, rd, rd, lambda h: h.tensor_scalar(out=SEL, in0=CM, scalar1=rt.t[:, 327:328], scalar2=None,
                                                                        op0=ALU.is_ge))
                            k.op(dve, rd, rd, lambda h: h.tensor_tensor(out=SEL, in0=SEL, in1=S_, op=ALU.mult))
                            k.op(dve, rd, rd, lambda h: h.tensor_reduce(out=WS[:, 0:1], in_=SEL, axis=AX.X, op=ALU.add))
                            k.op(dve, rd, rd, lambda h: h.reciprocal(out=WS[:, 1:2], in_=WS[:, 0:1]))
                            k.op(dve, rd, [gates.d], lambda h, st=st: h.tensor_scalar(
                                out=gates.t[:, st, 0:64], in0=SEL, scalar1=rt.t[:, 449:450], scalar2=2.5, op0=ALU.mult, op1=ALU.mult))
                            k.op(dve, [], [gates.d], lambda h, st=st: h.memset(gates.t[:, st, 64:65], 1.0))
                            if sparse:
                                k.op(dve, [gates.d], [gall.d], lambda h, st=st, tb=tb: h.tensor_copy(
                                    out=gall.t[:, tb * 4 + st, :], in_=gates.t[:, st, 0:NE]))
                                k.dma(sp, gts_s[r0:r0 + 128, :], gates.t[:, st, 0:NE], [gates.d], [], hf_ds)
                            if dbg:
                                k.dma(sp, gates_s[r0:r0 + 128, :], gates.t[:, st, :], [gates.d], [], misc_ds)
                    k.barrier()

                if "E" in stages:
                    with contextlib.ExitStack() as sE:
                        actb = [sb(f"actb{i}", [128, 4, 512], BF16, sE) for i in range(2)]
                        slb = sb("slb", [128, 4, 512], BF16, sE)
                        sl_d = [Dep() for _ in range(4)]
                        pg = [ps(f"pg{i}", [128, 512], F32, sE) for i in range(2)]
                        pu = [ps(f"pu{i}", [128, 512], F32, sE) for i in range(2)]
                        pd = [ps(f"pd{i}", [128, 512], F32, sE) for i in range(4)]
                        gi = ui_ = di = 0
                        for e in range(nexp):
                            ee = e if nexp == 65 else (e if e < nexp - 1 else 64)
                            wg = we_gate[ee] if ee < 64 else ws_gate
                            wu = we_up[ee] if ee < 64 else ws_up
                            wd = we_down[ee] if ee < 64 else ws_down
                            ab = actb[e % 2]
                            for half in range(2):
                                c0 = half * 256
                                sG, vG = ring_load(wg[:, c0:c0 + 256].rearrange("(k p) f -> p k f", p=128), (32, 256))
                                sU, vU = ring_load(wu[:, c0:c0 + 256].rearrange("(k p) f -> p k f", p=128), (32, 256))
                                for cl in range(2):
                                    c = half * 2 + cl
                                    p = pg[gi % 2]
                                    gi += 1
                                    mm_group(p.t[:], [(vG[:, kk, cl * 128:(cl + 1) * 128], actT.t[:, kk, :]) for kk in range(32)],
                                             p.d, [sG.d, actT.d])
                                    k.op(act, [p.d], [sl_d[c]], lambda h, p=p, c=c: h.activation(
                                        out=slb.t[:, c, :], in_=p.t[:], func=AF.Silu))
                                for cl in range(2):
                                    c = half * 2 + cl
                                    p = pu[ui_ % 2]
                                    ui_ += 1
                                    mm_group(p.t[:], [(vU[:, kk, cl * 128:(cl + 1) * 128], actT.t[:, kk, :]) for kk in range(32)],
                                             p.d, [sU.d, actT.d])
                                    k.op(dve, [p.d, sl_d[c]], [ab.d], lambda h, p=p, c=c, ab=ab: h.tensor_tensor(
                                        out=ab.t[:, c, :], in0=slb.t[:, c, :], in1=p.t[:], op=ALU.mult))
                            for dh in range(2):
                                d0 = dh * 2048
                                sD_, vD = ring_load(wd[:, d0:d0 + 2048].rearrange("(c p) d -> p c d", p=128), (4, 2048))
                                for st in range(4):
                                    for db in range(4):
                                        p = pd[di % 4]
                                        di += 1
                                        mm_group(p.t[:], [(ab.t[:, c, st * 128:(st + 1) * 128], vD[:, c, db * 512:(db + 1) * 512])
                                                          for c in range(4)], p.d, [sD_.d, ab.d])
                                        oc = d0 + db * 512
                                        if e == 0:
                                            k.op(dve, [p.d, gates.d, x1d[st]], [acc[st].d], lambda h, p=p, st=st, oc=oc, ee=ee: h.tensor_scalar(
                                                out=acc[st].t[:, oc:oc + 512], in0=p.t[:], scalar1=gates.t[:, st, ee:ee + 1], scalar2=None,
                                                op0=ALU.mult))
                                        else:
                                            k.op(dve, [p.d, gates.d], [acc[st].d], lambda h, p=p, st=st, oc=oc, ee=ee: h.scalar_tensor_tensor(
                                                out=acc[st].t[:, oc:oc + 512], in0=p.t[:], scalar=gates.t[:, st, ee:ee + 1],
                                                in1=acc[st].t[:, oc:oc + 512], op0=ALU.mult, op1=ALU.add))
                    k.barrier()

                if "F" in stages:
                    with contextlib.ExitStack() as sF:
                        x1p = [sb(f"x1p{i}", [128, 512], F32, sF) for i in range(2)]
                        gfp = [sb(f"gfp{i}", [128, 512], F32, sF) for i in range(2)]
                        fgp = [sb(f"fgp{i}", [128, 512], F32, sF) for i in range(2)]
                        ob = [sb(f"ob{i}", [128, 512], F32, sF) for i in range(2)]
                        junk = sb("junkF", [128, D], BF16, sF) if not sparse else None
                        if tb == 0:
                            f_ds = [k.dsem() for _ in range(6)]
                        for st in range(4):
                            r0 = tb * 512 + st * 128
                            A_ = acc[st]
                            for pc in range(8):
                                i2 = pc % 2
                                k.dma(sp, x1p[i2].t[:], x1_s[r0:r0 + 128, pc * 512:(pc + 1) * 512], [x1d[st]], [x1p[i2].d], f_ds[i2])
                                k.dma(sp, gfp[i2].t[:], modrows[0:1, 5 * D + pc * 512:5 * D + (pc + 1) * 512].to_broadcast([128, 512]),
                                      [], [gfp[i2].d], f_ds[2 + i2])
                                k.op(dve, [A_.d, gfp[i2].d], [A_.d], lambda h, A_=A_, pc=pc, i2=i2: h.tensor_tensor(
                                    out=A_.t[:, pc * 512:(pc + 1) * 512], in0=A_.t[:, pc * 512:(pc + 1) * 512], in1=gfp[i2].t[:], op=ALU.mult))
                                k.op(pool, [A_.d, x1p[i2].d], [A_.d], lambda h, A_=A_, pc=pc, i2=i2: h.tensor_tensor(
                                    out=A_.t[:, pc * 512:(pc + 1) * 512], in0=A_.t[:, pc * 512:(pc + 1) * 512], in1=x1p[i2].t[:], op=ALU.add))
                            zero(sm.t[:, 12:13])
                            if sparse:
                                k.dma(sp, x1_s[r0:r0 + 128, :], A_.t[:], [A_.d], [x1d[st]], x1_ds[st])
                                continue
                            k.op(act, [A_.d], [junk.d, sm.d], lambda h, A_=A_: h.activation(
                                out=junk.t[:], in_=A_.t[:], func=AF.Square, accum_out=sm.t[:, 12:13]))
                            rstd_from_ssq(sm.t[:, 12:13], sm.t[:, 13:14], D, [sm.d], [sm.d])
                            for pc in range(8):
                                i2 = pc % 2
                                k.dma(sp, fgp[i2].t[:], final_g[0:1, pc * 512:(pc + 1) * 512].to_broadcast([128, 512]),
                                      [], [fgp[i2].d], f_ds[4 + i2])
                                k.op(dve, [A_.d, sm.d, fgp[i2].d], [ob[i2].d], lambda h, A_=A_, pc=pc, i2=i2: h.scalar_tensor_tensor(
                                    out=ob[i2].t[:], in0=A_.t[:, pc * 512:(pc + 1) * 512], scalar=sm.t[:, 13:14], in1=fgp[i2].t[:],
                                    op0=ALU.mult, op1=ALU.mult))
                                k.dma(sp, out[r0:r0 + 128, pc * 512:(pc + 1) * 512], ob[i2].t[:], [ob[i2].d], [], o_ds[i2])
                    k.barrier()
        sBlk.close()
        if sparse and "S" in stages:
            I32_ = mybir.dt.int32
            reg_tos = nc.gpsimd.to_reg(ntiles * TS - 1)
            reg_y = nc.gpsimd.to_reg(16 * NTOK - 1)
            iota_e = cst.t[:, 0:64]
            iota_j = cst.t[:, 256:384]
            pidx = cst.t[:, 160:161]
            base16 = cst.t[:, 192:208]
            with contextlib.ExitStack() as s2:
                selb = sb("selb", [128, 16, NE], BF16, s2)
                trif = sb("trif", [128, 128], F32, s2)
                trib = sb("trib", [128, 128], BF16, s2)
                oneb = sb("oneb", [128, 128], BF16, s2)
                cnt = sb("cnt", [128, NE], F32, s2)
                tl = sb("tl", [128, NE], F32, s2)
                stt = sb("stt", [128, NE + 1], F32, s2)
                wk = sb("wk", [128, 256], F32, s2)
                sI = [sb(f"sI{i}", [128, 8], I32_, s2) for i in range(2)]
                trow = [sb(f"trow{i}", [128, 16], I32_, s2) for i in range(2)]
                tinit = sb("tinit", [128, ntiles * TS // 128, 16], I32_, s2)
                pcn = ps("pcn", [128, NE], F32, s2)
                ppo = [ps(f"ppo{i}", [128, NE], F32, s2) for i in range(2)]
                tosD = Dep()
                sc_ds = [k.dsem() for _ in range(2)]
                k.dma(sp, trif.t[:], tri_in, [], [trif.d], misc_ds)
                k.op(dve, [trif.d], [trib.d], lambda h: h.tensor_copy(out=trib.t[:], in_=trif.t[:]))
                k.op(dve, [], [oneb.d], lambda h: h.memset(oneb.t[:], 1.0))
                k.op(pool, [], [tinit.d], lambda h: h.memset(tinit.t[:], NTOK))
                k.dma(sp, tos_d.rearrange("(a p) c -> p a c", p=128), tinit.t[:], [tinit.d], [tosD], misc_ds)
                k.op(dve, [gall.d], [selb.d], lambda h: h.tensor_scalar(
                    out=selb.t[:], in0=gall.t[:], scalar1=0.0, scalar2=None, op0=ALU.is_gt))
                mm_group(pcn.t[:], [(oneb.t[:], selb.t[:, i, :]) for i in range(16)], pcn.d, [oneb.d, selb.d])
                k.op(dve, [pcn.d], [cnt.d], lambda h: h.tensor_copy(out=cnt.t[:], in_=pcn.t[:]))
                k.op(dve, [cnt.d], [tl.d], lambda h: h.tensor_scalar(out=tl.t[:], in0=cnt.t[:], scalar1=0.0, scalar2=None, op0=ALU.is_gt))
                for jj in range(1, NTOK // TS):
                    k.op(dve, [cnt.d, tl.d], [tl.d], lambda h, jj=jj: h.scalar_tensor_tensor(
                        out=tl.t[:], in0=cnt.t[:], scalar=float(TS * jj), in1=tl.t[:], op0=ALU.is_gt, op1=ALU.add))
                k.op(dve, [], [stt.d], lambda h: h.memset(stt.t[:, 0:1], 0.0))
                for e in range(NE):
                    k.op(dve, [tl.d, stt.d], [stt.d], lambda h, e=e: h.tensor_tensor(
                        out=stt.t[:, e + 1:e + 2], in0=stt.t[:, e:e + 1], in1=tl.t[:, e:e + 1], op=ALU.add))
                k.op(dve, [], [eot.d], lambda h: h.memset(eot.t[:], -1.0))
                for e in range(NE):
                    k.op(dve, [stt.d, cst.d, eot.d], [eot.d], lambda h, e=e: h.scalar_tensor_tensor(
                        out=eot.t[:], in0=iota_j, scalar=stt.t[:, e:e + 1], in1=eot.t[:], op0=ALU.is_ge, op1=ALU.add))
                k.op(dve, [eot.d], [eotk.d], lambda h: h.tensor_scalar(
                    out=eotk.t[:], in0=eot.t[:], scalar1=1024.0, scalar2=None, op0=ALU.mult))
                k.op(dve, [stt.d, cst.d], [wk.d], lambda h: h.tensor_scalar(
                    out=wk.t[:, 0:128], in0=iota_j, scalar1=stt.t[:, NE:NE + 1], scalar2=1.0e7, op0=ALU.is_ge, op1=ALU.mult))
                pass
                for i in range(16):
                    pp = ppo[i % 2]
                    pairs = [(oneb.t[:], selb.t[:, i2, :]) for i2 in range(i)] + [(trib.t[:], selb.t[:, i, :])]
                    mm_group(pp.t[:], pairs, pp.d, [oneb.d, trib.d, selb.d])
                    W = wk
                    k.op(dve, [stt.d, pp.d], [W.d], lambda h, pp=pp: h.scalar_tensor_tensor(
                        out=wk.t[:, 0:64], in0=stt.t[:, 0:NE], scalar=float(TS), in1=pp.t[:], op0=ALU.mult, op1=ALU.add))
                    k.op(dve, [gall.d], [W.d], lambda h, i=i: h.tensor_scalar(
                        out=wk.t[:, 64:128], in0=gall.t[:, i, :], scalar1=0.0, scalar2=None, op0=ALU.is_gt))
                    k.op(dve, [W.d], [W.d], lambda h: h.scalar_tensor_tensor(
                        out=wk.t[:, 128:192], in0=wk.t[:, 0:64], scalar=1.0, in1=wk.t[:, 64:128], op0=ALU.add, op1=ALU.mult))
                    k.op(dve, [W.d], [W.d], lambda h: h.max(out=wk.t[:, 192:200], in_=wk.t[:, 128:192]))
                    k.op(dve, [W.d], [W.d], lambda h: h.tensor_scalar(
                        out=wk.t[:, 200:208], in0=wk.t[:, 192:200], scalar1=-1.0, scalar2=None, op0=ALU.add))
                    si = sI[i % 2]
                    tr = trow[i % 2]
                    k.op(dve, [W.d], [si.d], lambda h, si=si: h.tensor_copy(out=si.t[:], in_=wk.t[:, 200:208]))
                    k.op(dve, [cst.d], [W.d], lambda h, i=i: h.tensor_scalar(
                        out=wk.t[:, 208:224], in0=cst.t[:, 160:161].to_broadcast([128, 16]), scalar1=float(128 * i), scalar2=None, op0=ALU.add))
                    k.op(dve, [W.d], [tr.d], lambda h, tr=tr: h.tensor_copy(out=tr.t[:], in_=wk.t[:, 208:224]))
                    for kk in range(8):
                        k.idma(tos_d[:, :], bass.IndirectOffsetOnAxis(ap=si.t[:, kk:kk + 1], axis=0), tr.t[:, :], None,
                               [si.d, tr.d, tosD], [], sc_ds[i % 2], bounds_check=reg_tos, oob_is_err=False)
            k.barrier()

            with contextlib.ExitStack() as s3:
                gmfk = sb("gmfk", [128, 32], F32, s3)
                shfk = sb("shfk", [128, 32], F32, s3)
                gfk = sb("gfk", [128, 32], F32, s3)
                k.dma(sp, gfk.t[:], gffn_pk, [], [gfk.d], misc_ds)
                k.dma(sp, gmfk.t[:], modrows[0:1, 4 * D:5 * D].rearrange("o (p k) -> (o p) k", k=32), [], [gmfk.d], misc_ds)
                k.dma(sp, shfk.t[:], modrows[0:1, 3 * D:4 * D].rearrange("o (p k) -> (o p) k", k=32), [], [shfk.d], misc_ds)
                fin = (misc_ds, misc_ds.n)
                for b_ in (gfk, gmfk, shfk):
                    b_.d.w = fin
                k.op(dve, [gmfk.d, gfk.d], [gmfk.d], lambda h: h.scalar_tensor_tensor(
                    out=gmfk.t[:], in0=gmfk.t[:], scalar=1.0, in1=gfk.t[:], op0=ALU.add, op1=ALU.mult))
                hg = [sb(f"hg{i}", [128, D], BF16, s3) for i in range(2)]
                grow = [sb(f"grow{i}", [128, NE], F32, s3) for i in range(2)]
                tokI = [sb(f"tokI{i}", [128, 16], I32_, s3) for i in range(2)]
                idxf = sb("idxf", [128, 16], F32, s3)
                idxi = [sb(f"idxi{i}", [128, 16], I32_, s3) for i in range(2)]
                ohg = sb("ohg", [128, 2, NE], F32, s3)
                w3 = sb("w3", [128, 256], F32, s3)
                gsl = [sb(f"gsl{i}", [128, 4], F32, s3) for i in range(2)]
                dsti = [sb(f"dsti{i}", [128, 4, 2], I32_, s3) for i in range(2)]
                actb = [sb(f"actbS{i}", [128, 4, 512], BF16, s3) for i in range(2)]
                slb = sb("slbS", [128, 4, 512], BF16, s3)
                sl_d = [Dep() for _ in range(4)]
                pgu = [ps(f"pgu{i}", [128, 512], F32, s3) for i in range(4)]
                pd = [ps(f"pdS{i}", [128, 512], F32, s3) for i in range(2)]
                ptr = [ps(f"ptrS{i}", [128, 8, 128], BF16, s3) for i in range(2)]
                tk_ds = [k.dsem() for _ in range(2)]
                hg_ds = [k.dsem() for _ in range(2)]
                gr_ds = [k.dsem() for _ in range(2)]
                wg2 = we_gate.rearrange("e (p k) f -> (e p) (k f)", k=32).rearrange("r (q c) -> (r q) c", c=2048)
                wu2 = we_up.rearrange("e (p k) f -> (e p) (k f)", k=32).rearrange("r (q c) -> (r q) c", c=2048)
                wd2 = we_down.rearrange("e f (h c) -> (e f h) c", c=2048)

                def ring_gather(src2, cols):
                    i = ring_i[0] % NR
                    ring_i[0] += 1
                    for q, c in enumerate(cols):
                        k.idma(ring[i].t[:, q * 2048:(q + 1) * 2048], None, src2,
                               bass.IndirectOffsetOnAxis(ap=c, axis=0), [ixd], [ring[i].d], ring_ds[i])
                    ring[i].d.w = (ring_ds[i], ring_ds[i].n)
                    return ring[i]

                ring.extend([sb(f"ringx{i}", [128, 4096 * 2], BF16, s3) for i in range(2)])
                ring_ds.extend([k.dsem() for _ in range(2)])
                NR3 = 6
                ring_i[0] = 0
                ohgs = [ohg, sb("ohg2", [128, 2, NE], F32, s3)]
                reg_w = nc.gpsimd.to_reg(NE * 1024 - 1)
                ybq = [sb(f"ybq{i}", [128, 2048], F32, s3) for i in range(4)]
                ybq_ds = [k.dsem() for _ in range(4)]
                y2 = y_s.rearrange("r (h c) -> (r h) c", h=2)
                cnts = {"ui": 0, "di": 0, "yi": 0}

                def ring_gather2(src2, cols, ixd):
                    i = ring_i[0] % NR3
                    ring_i[0] += 1
                    rd = ring[i].d
                    k.need(pool, rd.w)
                    for o, c_ in rd.r.items():
                        k.need(pool, (o, c_))
                    for q, c in enumerate(cols):
                        k.idma(ring[i].t[:, q * 2048:(q + 1) * 2048], None, src2,
                               bass.IndirectOffsetOnAxis(ap=c, axis=0), [ixd], [], ring_ds[i])
                    rd.w = (ring_ds[i], ring_ds[i].n)
                    rd.r = {}
                    return ring[i]

                def prep(j):
                    ix = idxi[j % 2]
                    og = ohgs[j % 2]
                    k.op(dve, [eotk.d, cst.d], [idxf.d], lambda h: h.tensor_scalar(
                        out=idxf.t[:], in0=base16, scalar1=eotk.t[:, j:j + 1], scalar2=None, op0=ALU.add))
                    k.op(dve, [idxf.d], [ix.d], lambda h: h.tensor_copy(out=ix.t[:], in_=idxf.t[:]))
                    k.op(dve, [eot.d, cst.d], [og.d], lambda h: h.tensor_scalar(
                        out=og.t[:, 0, :], in0=iota_e, scalar1=eot.t[:, j:j + 1], scalar2=None, op0=ALU.is_equal))
                    k.op(dve, [eot.d, cst.d], [og.d], lambda h: h.tensor_scalar(
                        out=og.t[:, 1, :], in0=iota_e, scalar1=eot.t[:, j:j + 1], scalar2=None, op0=ALU.is_gt))
                    gs_, dsi = gsl[j % 2], dsti[j % 2]
                    for st in range(TS // 128):
                        u2 = cnts["ui"] % 2
                        cnts["ui"] += 1
                        s0 = j * TS + st * 128
                        T, H, G = tokI[u2], hg[u2], grow[u2]
                        k.dma(sp, T.t[:], tos_d[s0:s0 + 128, :], [], [T.d], tk_ds[u2])
                        k.idma(H.t[:], None, hfn_s[:, :], bass.IndirectOffsetOnAxis(ap=T.t[:, 0:1], axis=0), [T.d], [H.d], hg_ds[u2])
                        k.idma(G.t[:], None, gts_s[:, :], bass.IndirectOffsetOnAxis(ap=T.t[:, 0:1], axis=0), [T.d], [G.d], gr_ds[u2])
                        wd_ = [w3.d]
                        k.op(dve, [G.d, og.d], wd_, lambda h, G=G: h.tensor_tensor(
                            out=w3.t[:, 0:64], in0=G.t[:], in1=og.t[:, 0, :], op=ALU.mult))
                        k.op(dve, wd_, [gs_.d], lambda h, st=st: h.tensor_reduce(
                            out=gs_.t[:, st:st + 1], in_=w3.t[:, 0:64], axis=AX.X, op=ALU.add))
                        k.op(dve, [G.d], wd_, lambda h, G=G: h.tensor_scalar(
                            out=w3.t[:, 64:128], in0=G.t[:], scalar1=0.0, scalar2=None, op0=ALU.is_gt))
                        k.op(dve, wd_ + [og.d], wd_, lambda h: h.tensor_tensor(
                            out=w3.t[:, 64:128], in0=w3.t[:, 64:128], in1=og.t[:, 1, :], op=ALU.mult))
                        k.op(dve, wd_, wd_, lambda h: h.tensor_reduce(
                            out=w3.t[:, 128:129], in_=w3.t[:, 64:128], axis=AX.X, op=ALU.add))
                        k.op(dve, [T.d], wd_, lambda h, T=T: h.tensor_copy(out=w3.t[:, 129:130], in_=T.t[:, 0:1]))
                        k.op(dve, wd_, wd_, lambda h: h.tensor_scalar(
                            out=w3.t[:, 130:131], in0=w3.t[:, 129:130], scalar1=float(NTOK), scalar2=1.0e6, op0=ALU.is_ge, op1=ALU.mult))
                        k.op(dve, wd_, wd_, lambda h: h.scalar_tensor_tensor(
                            out=w3.t[:, 131:132], in0=w3.t[:, 128:129], scalar=float(NTOK), in1=w3.t[:, 129:130], op0=ALU.mult, op1=ALU.add))
                        k.op(dve, wd_, wd_, lambda h: h.tensor_tensor(
                            out=w3.t[:, 131:132], in0=w3.t[:, 131:132], in1=w3.t[:, 130:131], op=ALU.add))
                        k.op(dve, wd_, wd_, lambda h: h.tensor_scalar(
                            out=w3.t[:, 132:133], in0=w3.t[:, 131:132], scalar1=2.0, scalar2=None, op0=ALU.mult))
                        k.op(dve, wd_, wd_, lambda h: h.tensor_scalar(
                            out=w3.t[:, 133:134], in0=w3.t[:, 131:132], scalar1=2.0, scalar2=1.0, op0=ALU.mult, op1=ALU.add))
                        k.op(dve, wd_, [dsi.d], lambda h, st=st: h.tensor_copy(out=dsi.t[:, st, :], in_=w3.t[:, 132:134]))
                        hv = H.t[:].rearrange("t (p k) -> t k p", k=32)
                        for k8 in range(4):
                            p = ptr[k8 % 2]
                            for q8 in range(8):
                                kk = k8 * 8 + q8
                                k.op(pe, [H.d, identb.d], [p.d], lambda h, p=p, q8=q8, kk=kk, hv=hv: h.transpose(
                                    p.t[:, q8, :], hv[:, kk, :], identb.t[:]), inc=(q8 == 7))
                            if k8 % 2 == 0:
                                k.op(act, [p.d], [actT.d], lambda h, p=p, k8=k8, st=st: h.copy(
                                    out=actT.t[:, k8 * 8:(k8 + 1) * 8, st * 128:(st + 1) * 128], in_=p.t[:]))
                            else:
                                k.op(dve, [p.d], [actT.d], lambda h, p=p, k8=k8, st=st: h.tensor_copy(
                                    out=actT.t[:, k8 * 8:(k8 + 1) * 8, st * 128:(st + 1) * 128], in_=p.t[:]))

                def weights_gu(j):
                    ix = idxi[j % 2]
                    gsl_ = [ring_gather2(wg2, [ix.t[:, kh * 4 + q:kh * 4 + q + 1] for q in range(4)], ix.d) for kh in range(2)]
                    usl_ = [ring_gather2(wu2, [ix.t[:, kh * 4 + q:kh * 4 + q + 1] for q in range(4)], ix.d) for kh in range(2)]
                    return gsl_, usl_

                def weights_d(j):
                    ix = idxi[j % 2]
                    return [ring_gather2(wd2, [ix.t[:, 8 + c * 2 + dh:8 + c * 2 + dh + 1] for c in range(4)], ix.d) for dh in range(2)]

                def compute_gu(j, gsl_, usl_):
                    ab = actb[j % 2]
                    for which, slots in ((0, gsl_), (1, usl_)):
                        for c in range(4):
                            pairs = []
                            for kh in range(2):
                                wv = slots[kh].t[:, 0:8192].rearrange("p (a b) -> p a b", a=16)
                                pairs += [(wv[:, kk, c * 128:(c + 1) * 128], actT.t[:, kh * 16 + kk, 0:TS]) for kk in range(16)]
                            mm_group(pgu[c].t[:, 0:TS], pairs, pgu[c].d, [slots[0].d, slots[1].d, actT.d])
                            if which == 0:
                                k.op(act, [pgu[c].d], [sl_d[c]], lambda h, c=c: h.activation(
                                    out=slb.t[:, c, 0:TS], in_=pgu[c].t[:, 0:TS], func=AF.Silu))
                            else:
                                k.op(dve, [pgu[c].d, sl_d[c]], [ab.d], lambda h, c=c, ab=ab: h.tensor_tensor(
                                    out=ab.t[:, c, 0:TS], in0=slb.t[:, c, 0:TS], in1=pgu[c].t[:, 0:TS], op=ALU.mult))

                def compute_d(j, dsl_):
                    ab = actb[j % 2]
                    gs_, dsi = gsl[j % 2], dsti[j % 2]
                    for dh in range(2):
                        sD_ = dsl_[dh]
                        vD = sD_.t[:, 0:8192].rearrange("p (a b) -> p a b", a=4)
                        for st in range(TS // 128):
                            yi = cnts["yi"] % 4
                            cnts["yi"] += 1
                            Yb = ybq[yi]
                            for db in range(4):
                                p = pd[cnts["di"] % 2]
                                cnts["di"] += 1
                                mm_group(p.t[:], [(ab.t[:, c, st * 128:(st + 1) * 128], vD[:, c, db * 512:(db + 1) * 512])
                                                  for c in range(4)], p.d, [sD_.d, ab.d])
                                k.op(dve, [p.d, gs_.d], [Yb.d], lambda h, p=p, st=st, db=db, Yb=Yb: h.tensor_scalar(
                                    out=Yb.t[:, db * 512:(db + 1) * 512], in0=p.t[:], scalar1=gs_.t[:, st:st + 1], scalar2=None, op0=ALU.mult))
                            k.idma(y2[:, :], bass.IndirectOffsetOnAxis(ap=dsi.t[:, st, dh:dh + 1], axis=0), Yb.t[:, :], None,
                                   [Yb.d, dsi.d], [], ybq_ds[yi], bounds_check=reg_y, oob_is_err=False)

                prep(0)
                gsl_, usl_ = weights_gu(0)
                dsl_ = weights_d(0)
                for j in range(ntiles):
                    compute_gu(j, gsl_, usl_)
                    if j + 1 < ntiles:
                        prep(j + 1)
                        gsl_, usl_ = weights_gu(j + 1)
                    compute_d(j, dsl_)
                    if j + 1 < ntiles:
                        dsl_ = weights_d(j + 1)
            k.barrier()

            with contextlib.ExitStack() as s4:
                NB4 = 3
                yp = [sb(f"yp{i}", [128, 8, 512], F32, s4) for i in range(NB4)]
                xsb = [sb(f"xsb{i}", [128, 512], F32, s4) for i in range(NB4)]
                gfp = [sb(f"gfpS{i}", [128, 512], F32, s4) for i in range(NB4)]
                fgp = [sb(f"fgpS{i}", [128, 512], F32, s4) for i in range(2)]
                ob = [sb(f"obS{i}", [128, 512], F32, s4) for i in range(2)]
                x2 = sb("x2", [128, D], F32, s4)
                junk = sb("junkS", [128, D], BF16, s4)
                p_ds = [k.dsem() for _ in range(8)]
                py_ds = [k.dsem() for _ in range(3 * NB4)]
                o_ds2 = [k.dsem() for _ in range(2)]
                yv = y_s.rearrange("(k t) d -> t k d", k=8)
                for i in range(16):
                    r0 = i * 128
                    for pc in range(8):
                        i2 = pc % 2
                        i4 = (i * 8 + pc) % NB4
                        Y, X, Gp = yp[i4], xsb[i4], gfp[i4]
                        k.dma(sp, Y.t[:], yv[r0:r0 + 128, :, pc * 512:(pc + 1) * 512], [], [Y.d], py_ds[i4])
                        k.dma(sp, X.t[:], x1_s[r0:r0 + 128, pc * 512:(pc + 1) * 512], [], [X.d], py_ds[NB4 + i4])
                        k.dma(sp, Gp.t[:], modrows[0:1, 5 * D + pc * 512:5 * D + (pc + 1) * 512].to_broadcast([128, 512]),
                              [], [Gp.d], py_ds[2 * NB4 + i4])
                        for lvl in (4, 2, 1):
                            eng = pool if lvl == 2 else dve
                            k.op(eng, [Y.d], [Y.d], lambda h, Y=Y, lvl=lvl: h.tensor_tensor(
                                out=Y.t[:, 0:lvl, :], in0=Y.t[:, 0:lvl, :], in1=Y.t[:, lvl:2 * lvl, :], op=ALU.add))
                        k.op(dve, [Y.d, Gp.d], [Y.d], lambda h, Y=Y, Gp=Gp: h.tensor_tensor(
                            out=Y.t[:, 0, :], in0=Y.t[:, 0, :], in1=Gp.t[:], op=ALU.mult))
                        k.op(pool, [Y.d, X.d], [x2.d], lambda h, Y=Y, X=X, pc=pc: h.tensor_tensor(
                            out=x2.t[:, pc * 512:(pc + 1) * 512], in0=Y.t[:, 0, :], in1=X.t[:], op=ALU.add))
                    zero(sm.t[:, 12:13])
                    k.op(act, [x2.d], [junk.d, sm.d], lambda h: h.activation(
                        out=junk.t[:], in_=x2.t[:], func=AF.Square, accum_out=sm.t[:, 12:13]))
                    rstd_from_ssq(sm.t[:, 12:13], sm.t[:, 13:14], D, [sm.d], [sm.d])
                    for pc in range(8):
                        i2 = pc % 2
                        k.dma(sp, fgp[i2].t[:], final_g[0:1, pc * 512:(pc + 1) * 512].to_broadcast([128, 512]),
                              [], [fgp[i2].d], p_ds[6 + i2])
                        k.op(dve, [x2.d, sm.d, fgp[i2].d], [ob[i2].d], lambda h, pc=pc, i2=i2: h.scalar_tensor_tensor(
                            out=ob[i2].t[:], in0=x2.t[:, pc * 512:(pc + 1) * 512], scalar=sm.t[:, 13:14], in1=fgp[i2].t[:],
                            op0=ALU.mult, op1=ALU.mult))
                        k.dma(sp, out[r0:r0 + 128, pc * 512:(pc + 1) * 512], ob[i2].t[:], [ob[i2].d], [], o_ds2[i2])
        k.barrier()
    return nc


def _tables(na_rpb, half):
    def pair_rows(pp):
        if 2 <= pp <= 17:
            r = 32 * half + 2 * (pp - 2)
            return (r, r + 1)
        if half == 0:
            return {0: (6, 7), 1: None, 18: (32, 33), 19: (34, 35)}[pp]
        return {0: (28, 29), 1: (30, 31), 18: None, 19: (56, 57)}[pp]
    rep = {0: 5, 1: 0, 2: 1, 3: 14, 4: 15}
    ro = np.zeros((5, 5, 128, 128), np.int64)
    co = np.zeros((5, 5, 128, 128), np.int64)
    mk = np.zeros((5, 5, 128, 128), np.float32)
    kk = np.arange(128)
    q = np.arange(128)
    for cls, j in rep.items():
        qrow = 32 * half + 2 * j + q // 64
        qcol = q % 64
        r0 = np.clip(qrow - 4, 0, 56)
        c0 = np.clip(qcol - 8, 0, 48)
        for bl in range(5):
            rows = pair_rows(j + bl)
            if rows is None:
                continue
            krow = np.array(rows)[kk // 64]
            kcol = kk % 64
            valid = ((krow[:, None] >= r0[None, :]) & (krow[:, None] < r0[None, :] + 8) &
                     (kcol[:, None] >= c0[None, :]) & (kcol[:, None] < c0[None, :] + 16))
            mk[cls, bl] = valid.astype(np.float32)
            ro[cls, bl] = np.clip(krow[:, None] - qrow[None, :] + 7, 0, 14)
            co[cls, bl] = np.clip(kcol[:, None] - qcol[None, :] + 15, 0, 30)
    bt = na_rpb[:, ro, co]
    bt = np.ascontiguousarray(bt.transpose(0, 3, 1, 2, 4))
    mt = np.ascontiguousarray(mk.transpose(2, 0, 1, 3))
    return bt, mt


def _consts():
    c = np.zeros((128, 512), np.float32)
    p = np.arange(128, dtype=np.float32)
    c[:, 0:64] = np.arange(64, dtype=np.float32)[None, :]
    c[:, 64:160] = np.arange(96, dtype=np.float32)[None, :]
    c[:, 256:384] = np.arange(128, dtype=np.float32)[None, :]
    c[:, 160] = p
    for m in range(8):
        c[:, 192 + m] = 8 * p + m
    for cc in range(4):
        for h in range(2):
            c[:, 200 + cc * 2 + h] = (cc * 128 + p) * 2 + h
    return c


def make_in_maps(inp):
    f = lambda a: np.ascontiguousarray(np.asarray(a, dtype=np.float32))
    x = f(inp["x"]); ctx = f(inp["ctx"]); c = f(inp["c"]); c_ctx = f(inp["c_ctx"])
    shared = {
        "w_ada": f(inp["w_ada"][0]),
        "b_ada2": f(np.broadcast_to(f(inp["b_ada"][0])[None, :], (2, 6 * D))),
        "gmix_fm": f(f(inp["norm_mix_g"][0]).reshape(32, 128).T),
        "gffn_fm": f(f(inp["norm_ffn_g"][0]).reshape(32, 128).T),
        "w_in": f(inp["w_in"][0]),
        "ln_g": f(inp["sg_ln_g"][0])[None, :], "ln_b": f(inp["sg_ln_b"][0])[None, :],
        "wsT": f(f(inp["sg_w_s"][0]).transpose(2, 0, 1)),
        "bsT": f(f(inp["sg_b_s"][0]).T),
        "gout": f(np.concatenate([f(inp["out_g_na"][0]), f(inp["out_g_sg"][0])]))[None, :],
        "w_out": f(inp["w_out"][0]),
        "w_router": f(inp["w_router"][0]), "rbias": f(inp["router_bias"][0])[None, :],
        "we_gate": f(inp["we_gate"][0]), "we_up": f(inp["we_up"][0]), "we_down": f(inp["we_down"][0]),
        "ws_gate": f(inp["ws_gate"][0]), "ws_up": f(inp["ws_up"][0]), "ws_down": f(inp["ws_down"][0]),
        "final_g": f(inp["final_g"])[None, :],
        "gffn_pk": f(f(inp["norm_ffn_g"][0]).reshape(128, 32)),
        "gffn_row": f(inp["norm_ffn_g"][0])[None, :],
        "consts": _consts(), "tri_in": f(np.triu(np.ones((128, 128), np.float32), 1)),
    }
    rpb = f(inp["na_rpb"][0])
    tabs = [_tables(rpb, h) for h in range(2)]
    maps = []
    for core in range(8):
        b, half = core // 2, core % 2
        xb = x[b]
        if half == 0:
            prs = [(6, 7), (6, 7), (32, 33), (34, 35)]
        else:
            prs = [(28, 29), (30, 31), (56, 57), (56, 57)]
        halo = np.concatenate([xb[r * 64:(r + 1) * 64] for pr in prs for r in pr], axis=0)
        m = dict(shared)
        m["x_own"] = f(xb[half * NTOK:(half + 1) * NTOK])
        m["x_hc"] = f(np.concatenate([halo, ctx[b]], axis=0))
        cc = np.stack([c[b], c_ctx], axis=-1)
        m["cT"] = f(cc.reshape(32, 128, 2).transpose(1, 0, 2))
        m["bias_tab"], m["mask_tab"] = tabs[half]
        maps.append(m)
    return maps


_NC_CACHE = {}


def kernel(**inputs):
    maps = make_in_maps(inputs)
    if "nc" not in _NC_CACHE:
        _NC_CACHE["nc"] = build_nc()
    res = run_bass_kernel_spmd(_NC_CACHE["nc"], maps, core_ids=list(range(8)))
    outs = [np.asarray(r["out"], dtype=np.float32).reshape(NTOK, D) for r in res.results]
    full = np.empty((4, 4096, D), np.float32)
    for core in range(8):
        b, half = core // 2, core % 2
        full[b, half * NTOK:(half + 1) * NTOK] = outs[core]
    return full
```

```python
import contextlib
import numpy as np
import concourse.bass as bass
import concourse.mybir as mybir
from concourse.bass_utils import run_bass_kernel_spmd

F32 = mybir.dt.float32
BF16 = mybir.dt.bfloat16
AF = mybir.ActivationFunctionType
ALU = mybir.AluOpType
AX = mybir.AxisListType

D = 4096
NTOK = 2048
NPOS = 2816
NH = 16
NE = 64
EPS = 1e-6
TCLS = {0: 1, 1: 2, 14: 3, 15: 4}


class Eng:
    def __init__(s, name, h, sem):
        s.name, s.h, s.sem, s.n, s.seen = name, h, sem, 0, {}


class DSem:
    def __init__(s, sem):
        s.sem, s.n = sem, 0


class Dep:
    __slots__ = ("w", "r")

    def __init__(s):
        s.w = None
        s.r = {}


class K:
    def __init__(s, nc, es):
        s.nc, s.es = nc, es
        s.nsem = 0
        s.pe = Eng("pe", nc.tensor, s._sem())
        s.act = Eng("act", nc.scalar, s._sem())
        s.dve = Eng("dve", nc.vector, s._sem())
        s.pool = Eng("pool", nc.gpsimd, s._sem())
        s.sp = Eng("sp", nc.sync, s._sem())
        s.engs = [s.pe, s.act, s.dve, s.pool, s.sp]
        s.dsems = []

    def _sem(s):
        s.nsem += 1
        return s.es.enter_context(s.nc.semaphore(f"sem{s.nsem}"))

    def dsem(s):
        d = DSem(s._sem())
        s.dsems.append(d)
        return d

    def need(s, eng, st):
        if st is None:
            return
        obj, cnt = st
        if cnt <= 0 or eng.seen.get(obj, 0) >= cnt:
            return
        if obj is eng and eng.name in ("pe", "sp"):
            return
        if isinstance(obj, Eng):
            assert obj.n >= cnt, f"pending stamp on {obj.name}"
        eng.h.wait_ge(obj.sem, cnt)
        eng.seen[obj] = cnt

    def op(s, eng, reads, writes, fn, inc=True):
        for d in reads:
            s.need(eng, d.w)
        for d in writes:
            s.need(eng, d.w)
            for o, c in d.r.items():
                if o is not eng:
                    s.need(eng, (o, c))
        ins = fn(eng.h)
        if inc:
            eng.n += 1
            ins.then_inc(eng.sem, 1)
            st = (eng, eng.n)
        else:
            st = (eng, eng.n + 1)
        for d in reads:
            if d.r.get(eng, 0) < st[1]:
                d.r[eng] = st[1]
        for d in writes:
            d.w = st
            d.r = {}
        return ins

    def dma(s, q, out_ap, in_ap, reads, writes, ds):
        for d in reads:
            s.need(q, d.w)
        for d in writes:
            s.need(q, d.w)
            for o, c in d.r.items():
                s.need(q, (o, c))
        ins = q.h.dma_start(out=out_ap, in_=in_ap)
        ds.n += 16
        ins.then_inc(ds.sem, 16)
        for d in reads:
            d.r[ds] = ds.n
        for d in writes:
            d.w = (ds, ds.n)
            d.r = {}

    def idma(s, out_ap, out_off, in_ap, in_off, reads, writes, ds, **kw):
        q = s.pool
        for d in reads:
            s.need(q, d.w)
        for d in writes:
            s.need(q, d.w)
            for o, c in d.r.items():
                s.need(q, (o, c))
        ins = q.h.indirect_dma_start(out=out_ap, out_offset=out_off, in_=in_ap, in_offset=in_off, **kw)
        ds.n += 16
        ins.then_inc(ds.sem, 16)
        for d in reads:
            d.r[ds] = ds.n
        for d in writes:
            d.w = (ds, ds.n)
            d.r = {}

    def barrier(s):
        sts = [(e, e.n) for e in s.engs if e.n > 0] + [(d, d.n) for d in s.dsems if d.n > 0]
        for e in s.engs:
            for st in sts:
                s.need(e, st)


class Buf:
    def __init__(s, t):
        s.t = t
        s.d = Dep()


def build_nc(dbg=False, stages="0ABCDEFS", nblocks=4, nexp=65, sparse=True, ntiles=128, TS=256):
    if sparse:
        nexp = 1
    nc = bass.Bass("TRN2", target_bir_lowering=False)
    es = contextlib.ExitStack()

    def din(name, shape, dt=F32):
        return nc.dram_tensor(name, list(shape), dt, kind="ExternalInput").ap()

    def dscr(name, shape, dt):
        return nc.dram_tensor(name, list(shape), dt, kind="ExternalOutput" if dbg else "Internal").ap()

    x_own = din("x_own", [NTOK, D])
    x_hc = din("x_hc", [768, D])
    cT = din("cT", [128, 32, 2])
    w_ada = din("w_ada", [D, 6 * D])
    b_ada2 = din("b_ada2", [2, 6 * D])
    gmix_fm = din("gmix_fm", [128, 32])
    gffn_fm = din("gffn_fm", [128, 32])
    w_in = din("w_in", [D, 10240])
    bias_tab = din("bias_tab", [NH, 128, 5, 5, 128])
    mask_tab = din("mask_tab", [128, 5, 5, 128])
    ln_g = din("ln_g", [1, 2048])
    ln_b = din("ln_b", [1, 2048])
    wsT = din("wsT", [128, 16, 128])
    bsT = din("bsT", [128, 16])
    gout = din("gout", [1, D])
    w_out = din("w_out", [D, D])
    w_router = din("w_router", [D, NE])
    rbias = din("rbias", [1, NE])
    we_gate = din("we_gate", [NE, D, 512])
    we_up = din("we_up", [NE, D, 512])
    we_down = din("we_down", [NE, 512, D])
    ws_gate = din("ws_gate", [D, 512])
    ws_up = din("ws_up", [D, 512])
    ws_down = din("ws_down", [512, D])
    final_g = din("final_g", [1, D])
    consts = din("consts", [128, 512])
    tri_in = din("tri_in", [128, 128])
    gffn_pk = din("gffn_pk", [128, 32])
    gffn_row = din("gffn_row", [1, D])
    out = nc.dram_tensor("out", [NTOK, D], F32, kind="ExternalOutput").ap()

    modrows = dscr("modrows", [2, 6 * D], F32)
    qT_s = dscr("qT_s", [NH, 128, NTOK], BF16)
    kT_s = dscr("kT_s", [NH, 128, NPOS], BF16)
    v_s = dscr("v_s", [NPOS, 2048], BF16)
    u_s = dscr("u_s", [NTOK, 2048], BF16)
    z_s = dscr("z_s", [NTOK, 2048], BF16)
    attn_s = dscr("attn_s", [NTOK, 2048], BF16)
    x1_s = dscr("x1_s", [NTOK, D], F32)
    gates_s = dscr("gates_s", [NTOK, 65], F32) if dbg else None
    I32 = mybir.dt.int32
    NSLOT = ntiles * 512
    hfn_s = dscr("hfn_s", [NTOK + 128, D], BF16)
    gts_s = dscr("gts_s", [NTOK + 128, NE], F32)
    tos_d = nc.dram_tensor("tos_d", [ntiles * TS, 16], I32, kind="ExternalOutput" if dbg else "Internal").ap()
    y_s = dscr("y_s", [8 * NTOK, D], F32)

    with es:
        k = K(nc, es)
        pe, act, dve, pool, sp = k.pe, k.act, k.dve, k.pool, k.sp

        uid = [0]

        def sb(name, shape, dt=F32, stack=es):
            uid[0] += 1
            return Buf(stack.enter_context(nc.sbuf_tensor(f"{name}_{uid[0]}", list(shape), dt)))

        def ps(name, shape, dt=F32, stack=es):
            uid[0] += 1
            return Buf(stack.enter_context(nc.psum_tensor(f"{name}_{uid[0]}", list(shape), dt)))

        NR = 4
        ring = [sb(f"ring{i}", [128, 4096 * 2], BF16) for i in range(NR)]
        ring_ds = [k.dsem() for _ in range(NR)]
        ring_i = [0]

        def ring_load(src_ap, view):
            i = ring_i[0] % NR
            ring_i[0] += 1
            a, b = view
            dst = ring[i].t[:, 0:a * b].rearrange("p (a b) -> p a b", a=a)
            k.dma(pool, dst, src_ap, [], [ring[i].d], ring_ds[i])
            return ring[i], dst

        actT = sb("actT", [128, 32, 512], BF16)
        modfm = sb("modfm", [128, 192, 2])
        gmm = sb("gmm", [128, 32, 2])
        gmf = sb("gmf", [128, 32])
        gmix_sb = sb("gmix_sb", [128, 32])
        gffn_sb = sb("gffn_sb", [128, 32])
        identf = sb("identf", [128, 128])
        identb = sb("identb", [128, 128], BF16)
        sm = sb("sm", [128, 16])
        misc_ds = k.dsem()

        k.op(pool, [], [identf.d], lambda h: h.memset(identf.t[:], 0.0))
        k.op(pool, [], [identf.d], lambda h: h.affine_select(
            out=identf.t[:], in_=identf.t[:], pattern=[[-1, 128]], compare_op=ALU.not_equal,
            fill=1.0, base=0, channel_multiplier=1))
        k.op(dve, [identf.d], [identb.d], lambda h: h.tensor_copy(out=identb.t[:], in_=identf.t[:]))
        k.dma(sp, gmix_sb.t[:], gmix_fm, [], [gmix_sb.d], misc_ds)
        k.dma(sp, gffn_sb.t[:], gffn_fm, [], [gffn_sb.d], misc_ds)

        def zero(ap):
            k.op(dve, [], [sm.d], lambda h: h.memset(ap, 0.0))

        def rstd_from_ssq(ssq_ap, out_ap, n, deps_r, deps_w):
            k.op(dve, deps_r, deps_w, lambda h: h.tensor_scalar(
                out=out_ap, in0=ssq_ap, scalar1=1.0 / n, scalar2=EPS, op0=ALU.mult, op1=ALU.add))
            k.op(act, deps_w, deps_w, lambda h: h.activation(out=out_ap, in_=out_ap, func=AF.Sqrt))
            k.op(dve, deps_w, deps_w, lambda h: h.reciprocal(out=out_ap, in_=out_ap))

        def mm_group(out_ap, pairs, psd, rdeps):
            n = len(pairs)
            for i, (l, r) in enumerate(pairs):
                k.op(pe, rdeps, [psd], lambda h, l=l, r=r, i=i: h.matmul(
                    out_ap, lhsT=l, rhs=r, start=(i == 0), stop=(i == n - 1)), inc=(i == n - 1))

        if "0" in stages:
            with contextlib.ExitStack() as st0:
                cs = sb("cs", [128, 32, 2], F32, st0)
                csb = sb("csb", [128, 32, 2], BF16, st0)
                id2 = sb("id2", [2, 2], F32, st0)
                bch = [sb(f"bch{i}", [2, 512], F32, st0) for i in range(2)]
                bch_ds = [k.dsem() for _ in range(2)]
                mev = [sb(f"mev{i}", [2, 512], F32, st0) for i in range(2)]
                mev_ds = [k.dsem() for _ in range(2)]
                pmod = [ps(f"pmod{i}", [2, 512], F32, st0) for i in range(2)]
                pfm = [ps(f"pfm{i}", [128, 4, 2], F32, st0) for i in range(2)]
                if sparse:
                    zrow = sb("zrow", [128, D], BF16, st0)
                    k.op(pool, [], [zrow.d], lambda h: h.memset(zrow.t[:], 0.0))
                    k.dma(sp, hfn_s[NTOK:NTOK + 128, :], zrow.t[:], [zrow.d], [], misc_ds)
                    zg = sb("zg", [128, NE], F32, st0)
                    k.op(pool, [], [zg.d], lambda h: h.memset(zg.t[:], 0.0))
                    k.dma(sp, gts_s[NTOK:NTOK + 128, :], zg.t[:], [zg.d], [], misc_ds)
                k.dma(sp, cs.t[:], cT, [], [cs.d], misc_ds)
                k.op(act, [cs.d], [csb.d], lambda h: h.activation(out=csb.t[:], in_=cs.t[:], func=AF.Silu))
                k.op(dve, [identf.d], [id2.d], lambda h: h.tensor_copy(out=id2.t[:], in_=identf.t[0:2, 0:2]))
                for cc in range(48):
                    c0 = cc * 512
                    i = cc % 2
                    halves = [ring_load(w_ada[kh * 2048:(kh + 1) * 2048, c0:c0 + 512].rearrange("(k p) f -> p k f", p=128), (16, 512))
                              for kh in range(2)]
                    k.dma(sp, bch[i].t[:], b_ada2[:, c0:c0 + 512], [], [bch[i].d], bch_ds[i])
                    pairs = []
                    for kh in range(2):
                        pairs += [(csb.t[:, kh * 16 + kk, :], halves[kh][1][:, kk, :]) for kk in range(16)]
                    mm_group(pmod[i].t[:], pairs, pmod[i].d, [csb.d, halves[0][0].d, halves[1][0].d])
                    k.op(dve, [pmod[i].d, bch[i].d], [mev[i].d], lambda h, i=i: h.tensor_tensor(
                        out=mev[i].t[:], in0=pmod[i].t[:], in1=bch[i].t[:], op=ALU.add))
                    k.dma(sp, modrows[:, c0:c0 + 512], mev[i].t[:], [mev[i].d], [], mev_ds[i])
                    for hh in range(4):
                        mm_group(pfm[i].t[:, hh, :], [(mev[i].t[0:2, hh * 128:(hh + 1) * 128], id2.t[:])],
                                 pfm[i].d, [mev[i].d, id2.d])
                    k.op(act, [pfm[i].d], [modfm.d], lambda h, i=i, cc=cc: h.copy(
                        out=modfm.t[:, 4 * cc:4 * cc + 4, :], in_=pfm[i].t[:]))
                for j in range(2):
                    k.op(dve, [modfm.d, gmix_sb.d], [gmm.d], lambda h, j=j: h.scalar_tensor_tensor(
                        out=gmm.t[:, :, j], in0=modfm.t[:, 32:64, j], scalar=1.0, in1=gmix_sb.t[:],
                        op0=ALU.add, op1=ALU.mult))
                k.op(dve, [modfm.d, gffn_sb.d], [gmf.d], lambda h: h.scalar_tensor_tensor(
                    out=gmf.t[:], in0=modfm.t[:, 128:160, 0], scalar=1.0, in1=gffn_sb.t[:],
                    op0=ALU.add, op1=ALU.mult))
            k.barrier()

        if "A" in stages:
            with contextlib.ExitStack() as sA:
                xt = [sb(f"xt{i}", [128, D], F32, sA) for i in range(2)]
                xt_ds = [k.dsem() for _ in range(2)]
                junk = sb("junkA", [128, D], BF16, sA)
                stg = [sb(f"stgA{i}", [128, 4, 256], BF16, sA) for i in range(2)]
                stg_ds = [k.dsem() for _ in range(2)]
                ptr = [ps(f"ptrA{i}", [128, 4, 128], F32, sA) for i in range(2)]
                pmm = [ps(f"pmmA{i}", [128, 512], F32, sA) for i in range(4)]
                tix = [0]
                pix = [0]
                six = [0]
                for g in range(6):
                    ntok = 256 if g == 5 else 512
                    nst = ntok // 128
                    j = 1 if g == 5 else 0
                    for st in range(nst):
                        xb = xt[tix[0] % 2]
                        xds = xt_ds[tix[0] % 2]
                        tix[0] += 1
                        src = x_own[g * 512 + st * 128: g * 512 + (st + 1) * 128, :] if g < 4 else \
                            x_hc[(g - 4) * 512 + st * 128:(g - 4) * 512 + (st + 1) * 128, :]
                        k.dma(sp, xb.t[:], src, [], [xb.d], xds)
                        zero(sm.t[:, 0:1])
                        k.op(act, [xb.d], [junk.d, sm.d], lambda h, xb=xb: h.activation(
                            out=junk.t[:], in_=xb.t[:], func=AF.Square, accum_out=sm.t[:, 0:1]))
                        rstd_from_ssq(sm.t[:, 0:1], sm.t[:, 1:2], D, [sm.d], [sm.d])
                        k.op(dve, [sm.d, xb.d], [xb.d], lambda h, xb=xb: h.tensor_scalar(
                            out=xb.t[:], in0=xb.t[:], scalar1=sm.t[:, 1:2], scalar2=None, op0=ALU.mult))
                        for kk in range(32):
                            p = ptr[(kk // 4) % 2]
                            k.op(pe, [xb.d, identf.d], [p.d], lambda h, p=p, kk=kk, xb=xb: h.transpose(
                                p.t[:, kk % 4, :], xb.t[:, kk * 128:(kk + 1) * 128], identf.t[:]),
                                inc=(kk % 4 == 3))
                            if kk % 4 == 3:
                                for q4 in range(4):
                                    k4 = kk - 3 + q4
                                    k.op(act, [p.d, gmm.d, modfm.d], [actT.d], lambda h, p=p, q4=q4, k4=k4, st=st, j=j: h.activation(
                                        out=actT.t[:, k4, st * 128:(st + 1) * 128], in_=p.t[:, q4, :], func=AF.Identity,
                                        scale=gmm.t[:, k4, j:j + 1], bias=modfm.t[:, k4, j:j + 1]))
                    chunks = range(40) if g < 4 else range(8, 24)
                    for cc in chunks:
                        c0 = cc * 256
                        slot, wv = ring_load(w_in[:, c0:c0 + 256].rearrange("(k p) f -> p k f", p=128), (32, 256))
                        if cc < 16:
                            for hh in range(2):
                                head = (cc % 8) * 2 + hh
                                pm = pmm[pix[0] % 4]
                                pix[0] += 1
                                mm_group(pm.t[:, 0:ntok], [(wv[:, kk, hh * 128:(hh + 1) * 128], actT.t[:, kk, 0:ntok])
                                                            for kk in range(32)], pm.d, [slot.d, actT.d])
                                sg_ = stg[six[0] % 2]
                                sds = stg_ds[six[0] % 2]
                                six[0] += 1
                                sview = sg_.t[:].rearrange("p a b -> p (a b)")[:, 0:512]
                                if cc < 8:
                                    k.op(act, [pm.d], [sg_.d], lambda h, pm=pm, sview=sview, ntok=ntok: h.activation(
                                        out=sview[:, 0:ntok], in_=pm.t[:, 0:ntok], func=AF.Copy, scale=float(128 ** -0.5)))
                                    k.dma(sp, qT_s[head, :, g * 512:(g + 1) * 512], sview, [sg_.d], [], sds)
                                else:
                                    k.op(dve, [pm.d], [sg_.d], lambda h, pm=pm, sview=sview, ntok=ntok: h.tensor_copy(
                                        out=sview[:, 0:ntok], in_=pm.t[:, 0:ntok]))
                                    if g < 4:
                                        segs = [(0, 512, 256 + 512 * g)]
                                    elif g == 4:
                                        segs = [(0, 256, 0), (256, 512, 2304)]
                                    else:
                                        segs = [(0, 256, 2560)]
                                    for (a, b, p0) in segs:
                                        k.dma(sp, kT_s[head, :, p0:p0 + (b - a)], sview[:, a:b], [sg_.d], [], sds)
                        else:
                            sg_ = stg[six[0] % 2]
                            sds = stg_ds[six[0] % 2]
                            six[0] += 1
                            for st in range(nst):
                                pm = pmm[pix[0] % 4]
                                pix[0] += 1
                                mm_group(pm.t[:, 0:256], [(actT.t[:, kk, st * 128:(st + 1) * 128], wv[:, kk, :])
                                                           for kk in range(32)], pm.d, [slot.d, actT.d])
                                if cc < 24:
                                    k.op(dve, [pm.d], [sg_.d], lambda h, pm=pm, sg_=sg_, st=st: h.tensor_copy(
                                        out=sg_.t[:, st, :], in_=pm.t[:, 0:256]))
                                else:
                                    k.op(act, [pm.d], [sg_.d], lambda h, pm=pm, sg_=sg_, st=st: h.activation(
                                        out=sg_.t[:, st, :], in_=pm.t[:, 0:256], func=AF.Gelu_apprx_tanh))
                            if cc < 24:
                                vc = (cc - 16) * 256
                                if g < 4:
                                    rows = [(0, 4, 256 + 512 * g)]
                                elif g == 4:
                                    rows = [(0, 2, 0), (2, 4, 2304)]
                                else:
                                    rows = [(0, 2, 2560)]
                                for (a, b, p0) in rows:
                                    k.dma(sp, v_s[p0:p0 + (b - a) * 128, vc:vc + 256].rearrange("(s p) f -> p s f", p=128),
                                          sg_.t[:, a:b, :], [sg_.d], [], sds)
                            else:
                                dst = u_s if cc < 32 else z_s
                                uc = ((cc - 24) % 8) * 256
                                k.dma(sp, dst[g * 512:(g + 1) * 512, uc:uc + 256].rearrange("(s p) f -> p s f", p=128),
                                      sg_.t[:], [sg_.d], [], sds)
            k.barrier()

        if "B" in stages:
            with contextlib.ExitStack() as sB:
                mask = sb("mask", [128, 5, 640], F32, sB)
                k.dma(sp, mask.t[:], mask_tab.rearrange("p c b q -> p c (b q)"), [], [mask.d], misc_ds)
                k.op(pool, [mask.d], [mask.d], lambda h: h.tensor_scalar(
                    out=mask.t[:], in0=mask.t[:], scalar1=30000.0, scalar2=-30000.0, op0=ALU.mult, op1=ALU.add))
                KT = [sb(f"KT{i}", [128, NPOS], BF16, sB) for i in range(2)]
                QT = [sb(f"QT{i}", [128, NTOK], BF16, sB) for i in range(2)]
                VH = [sb(f"VH{i}", [128, 22, 132], BF16, sB) for i in range(2)]
                BI = [sb(f"BI{i}", [128, 5, 640], F32, sB) for i in range(2)]
                AH = [sb(f"AH{i}", [128, 16, 128], BF16, sB) for i in range(2)]
                hd_ds = [k.dsem() for _ in range(2)]
                ah_ds = [k.dsem() for _ in range(2)]
                ssb = [sb(f"ssb{i}", [128, 640], F32, sB) for i in range(2)]
                e1 = [sb(f"e1{i}", [128, 640], F32, sB) for i in range(2)]
                pT = [sb(f"pT{i}", [128, 896], BF16, sB) for i in range(2)]
                rec = sb("rec", [128, 2], F32, sB)
                Sps = [ps(f"Sps{i}", [128, 1024], F32, sB) for i in range(2)]
                Ops = [ps(f"Ops{i}", [128, 512], F32, sB) for i in range(2)]
                for i in range(2):
                    k.op(pool, [], [VH[i].d], lambda h, i=i: h.memset(VH[i].t[:, :, 128:129], 1.0))
                ui = 0
                for hd in range(NH):
                    b_ = hd % 2
                    deps_w = [KT[b_].d, QT[b_].d, VH[b_].d, BI[b_].d]
                    k.dma(sp, KT[b_].t[:], kT_s[hd], [], [KT[b_].d], hd_ds[b_])
                    k.dma(sp, QT[b_].t[:], qT_s[hd], [], [QT[b_].d], hd_ds[b_])
                    k.dma(sp, VH[b_].t[:, :, 0:128], v_s[:, hd * 128:(hd + 1) * 128].rearrange("(b p) d -> p b d", p=128),
                          [], [VH[b_].d], hd_ds[b_])
                    k.dma(sp, BI[b_].t[:], bias_tab[hd].rearrange("p c b q -> p c (b q)"), [], [BI[b_].d], hd_ds[b_])
                    fin = (hd_ds[b_], hd_ds[b_].n)
                    for d in deps_w:
                        d.w = fin
                    k.op(pool, [BI[b_].d, mask.d], [BI[b_].d], lambda h, b_=b_: h.tensor_tensor(
                        out=BI[b_].t[:], in0=BI[b_].t[:], in1=mask.t[:], op=ALU.add))
                    for j in range(16):
                        cls = TCLS.get(j, 0)
                        u2 = ui % 2
                        ui += 1
                        S, O = Sps[u2], Ops[u2]
                        blocks = [j + bl for bl in range(5)] + [20, 21]
                        for bi_, pb in enumerate(blocks):
                            k.op(pe, [KT[b_].d, QT[b_].d], [S.d], lambda h, S=S, bi_=bi_, pb=pb, j=j, b_=b_: h.matmul(
                                S.t[:, bi_ * 128:(bi_ + 1) * 128], lhsT=KT[b_].t[:, pb * 128:(pb + 1) * 128],
                                rhs=QT[b_].t[:, j * 128:(j + 1) * 128], start=True, stop=True), inc=(bi_ == 6))
                        k.op(dve, [S.d, BI[b_].d], [ssb[u2].d], lambda h, S=S, u2=u2, cls=cls, b_=b_: h.tensor_tensor(
                            out=ssb[u2].t[:], in0=S.t[:, 0:640], in1=BI[b_].t[:, cls, :], op=ALU.add))
                        k.op(act, [S.d], [pT[u2].d], lambda h, S=S, u2=u2: h.activation(
                            out=pT[u2].t[:, 640:896], in_=S.t[:, 640:896], func=AF.Exp))
                        k.op(act, [ssb[u2].d], [pT[u2].d], lambda h, u2=u2: h.activation(
                            out=pT[u2].t[:, 0:640], in_=ssb[u2].t[:], func=AF.Exp))
                        for bi_, pb in enumerate(blocks):
                            k.op(pe, [pT[u2].d, VH[b_].d], [O.d], lambda h, O=O, bi_=bi_, pb=pb, u2=u2, b_=b_: h.matmul(
                                O.t[:, 0:129], lhsT=pT[u2].t[:, bi_ * 128:(bi_ + 1) * 128], rhs=VH[b_].t[:, pb, 0:129],
                                start=(bi_ == 0), stop=(bi_ == 6)), inc=(bi_ == 6))
                        k.op(dve, [O.d], [rec.d], lambda h, O=O, u2=u2: h.reciprocal(
                            out=rec.t[:, u2:u2 + 1], in_=O.t[:, 128:129]))
                        k.op(dve, [O.d, rec.d], [AH[b_].d], lambda h, O=O, u2=u2, j=j, b_=b_: h.tensor_scalar(
                            out=AH[b_].t[:, j, :], in0=O.t[:, 0:128], scalar1=rec.t[:, u2:u2 + 1], scalar2=None, op0=ALU.mult))
                    k.dma(sp, attn_s[:, hd * 128:(hd + 1) * 128].rearrange("(j p) d -> p j d", p=128), AH[b_].t[:],
                          [AH[b_].d], [], ah_ds[b_])
            k.barrier()

        gall = sb("gall", [128, 16, NE])
        cst = sb("cst", [128, 512])
        eot = sb("eot", [128, 128])
        eotk = sb("eotk", [128, 128])
        sBlk = contextlib.ExitStack()
        wr_b = sb("wr_b", [128, 32, NE], BF16, sBlk)
        rb_bc = sb("rb_bc", [128, NE], F32, sBlk)
        wsT_b = sb("wsT_b", [128, 16, 128], BF16, sBlk)
        bsT_sb = sb("bsT_sb", [128, 16], F32, sBlk)
        if sparse:
            k.dma(sp, cst.t[:], consts, [], [cst.d], misc_ds)
        if any(s_ in stages for s_ in "CDEF"):
            k.dma(pool, wr_b.t[:], w_router.rearrange("(k p) e -> p k e", p=128), [], [wr_b.d], misc_ds)
            k.dma(sp, rb_bc.t[:], rbias.to_broadcast([128, NE]), [], [rb_bc.d], misc_ds)
            k.dma(pool, wsT_b.t[:], wsT, [], [wsT_b.d], misc_ds)
            k.dma(sp, bsT_sb.t[:], bsT, [], [bsT_sb.d], misc_ds)
            fin = (misc_ds, misc_ds.n)
            for b_ in (wr_b, rb_bc, wsT_b, bsT_sb):
                b_.d.w = fin

        for tb in range(nblocks if any(s_ in stages for s_ in "CDEF") else 0):
            if "C" in stages:
                with contextlib.ExitStack() as sC:
                    lng = sb("lng", [128, 2048], F32, sC)
                    lnb = sb("lnb", [128, 2048], F32, sC)
                    gob = sb("gob", [128, D], F32, sC)
                    tds = k.dsem() if tb == 0 else tds_keep[0]
                    if tb == 0:
                        tds_keep = [tds]
                    k.dma(sp, lng.t[:], ln_g.to_broadcast([128, 2048]), [], [lng.d], tds)
                    k.dma(sp, lnb.t[:], ln_b.to_broadcast([128, 2048]), [], [lnb.d], tds)
                    k.dma(sp, gob.t[:], gout.to_broadcast([128, D]), [], [gob.d], tds)
                    fin = (tds, tds.n)
                    for b_ in (lng, lnb, gob):
                        b_.d.w = fin
                    if tb == 0:
                        ld_ds = [k.dsem() for _ in range(2)]
                    ut = [sb(f"ut{i}", [128, 2048], BF16, sC) for i in range(2)]
                    zt = [sb(f"zt{i}", [128, 2048], BF16, sC) for i in range(2)]
                    at = [sb(f"at{i}", [128, 2048], BF16, sC) for i in range(2)]
                    fa = sb("fa", [128, 2048], F32, sC)
                    zn = sb("zn", [128, 2048], BF16, sC)
                    y = sb("y", [128, D], BF16, sC)
                    mix = ps("mix", [128, 2048], F32, sC)
                    ptc = [ps(f"ptc{i}", [128, 8, 128], BF16, sC) for i in range(2)]
                    for st in range(4):
                        r0 = tb * 512 + st * 128
                        i2 = st % 2
                        k.dma(sp, ut[i2].t[:], u_s[r0:r0 + 128, :], [], [ut[i2].d], ld_ds[i2])
                        k.dma(sp, zt[i2].t[:], z_s[r0:r0 + 128, :], [], [zt[i2].d], ld_ds[i2])
                        k.dma(sp, at[i2].t[:], attn_s[r0:r0 + 128, :], [], [at[i2].d], ld_ds[i2])
                        fin = (ld_ds[i2], ld_ds[i2].n)
                        for b_ in (ut[i2], zt[i2], at[i2]):
                            b_.d.w = fin
                        U, Z, A_ = ut[i2], zt[i2], at[i2]
                        zero(sm.t[:, 2:4])
                        k.op(act, [Z.d], [fa.d, sm.d], lambda h, Z=Z: h.activation(
                            out=fa.t[:], in_=Z.t[:], func=AF.Identity, accum_out=sm.t[:, 2:3]))
                        k.op(act, [Z.d], [y.d, sm.d], lambda h, Z=Z: h.activation(
                            out=y.t[:, 0:2048], in_=Z.t[:], func=AF.Square, accum_out=sm.t[:, 3:4]))
                        k.op(dve, [sm.d], [sm.d], lambda h: h.tensor_scalar(
                            out=sm.t[:, 4:5], in0=sm.t[:, 2:3], scalar1=1.0 / 2048, scalar2=None, op0=ALU.mult))
                        k.op(dve, [sm.d], [sm.d], lambda h: h.tensor_tensor(
                            out=sm.t[:, 5:6], in0=sm.t[:, 4:5], in1=sm.t[:, 4:5], op=ALU.mult))
                        k.op(dve, [sm.d], [sm.d], lambda h: h.scalar_tensor_tensor(
                            out=sm.t[:, 6:7], in0=sm.t[:, 3:4], scalar=1.0 / 2048, in1=sm.t[:, 5:6],
                            op0=ALU.mult, op1=ALU.subtract))
                        k.op(dve, [sm.d], [sm.d], lambda h: h.tensor_scalar(
                            out=sm.t[:, 6:7], in0=sm.t[:, 6:7], scalar1=EPS, scalar2=None, op0=ALU.add))
                        k.op(act, [sm.d], [sm.d], lambda h: h.activation(out=sm.t[:, 6:7], in_=sm.t[:, 6:7], func=AF.Sqrt))
                        k.op(dve, [sm.d], [sm.d], lambda h: h.reciprocal(out=sm.t[:, 6:7], in_=sm.t[:, 6:7]))
                        k.op(dve, [sm.d], [sm.d], lambda h: h.scalar_tensor_tensor(
                            out=sm.t[:, 7:8], in0=sm.t[:, 4:5], scalar=-1.0, in1=sm.t[:, 6:7],
                            op0=ALU.mult, op1=ALU.mult))
                        k.op(act, [sm.d, fa.d], [fa.d], lambda h: h.activation(
                            out=fa.t[:], in_=fa.t[:], func=AF.Identity, scale=sm.t[:, 6:7], bias=sm.t[:, 7:8]))
                        k.op(dve, [fa.d, lng.d], [fa.d], lambda h: h.tensor_tensor(
                            out=fa.t[:], in0=fa.t[:], in1=lng.t[:], op=ALU.mult))
                        k.op(pool, [fa.d, lnb.d], [zn.d], lambda h: h.tensor_tensor(
                            out=zn.t[:], in0=fa.t[:], in1=lnb.t[:], op=ALU.add))
                        for g in range(16):
                            k.op(pe, [zn.d, wsT_b.d], [mix.d], lambda h, g=g: h.matmul(
                                mix.t[:, g * 128:(g + 1) * 128], lhsT=wsT_b.t[:, g, :], rhs=zn.t[:, g * 128:(g + 1) * 128],
                                start=True, stop=True), inc=(g == 15))
                        for g in range(16):
                            k.op(dve, [mix.d, bsT_sb.d, U.d], [fa.d], lambda h, g=g, U=U: h.scalar_tensor_tensor(
                                out=fa.t[:, g * 128:(g + 1) * 128], in0=mix.t[:, g * 128:(g + 1) * 128],
                                scalar=bsT_sb.t[:, g:g + 1], in1=U.t[:, g * 128:(g + 1) * 128], op0=ALU.add, op1=ALU.mult))
                        zero(sm.t[:, 8:10])
                        k.op(act, [fa.d], [y.d, sm.d], lambda h: h.activation(
                            out=y.t[:, 2048:4096], in_=fa.t[:], func=AF.Square, accum_out=sm.t[:, 9:10]))
                        k.op(act, [A_.d], [y.d, sm.d], lambda h, A_=A_: h.activation(
                            out=y.t[:, 0:2048], in_=A_.t[:], func=AF.Square, accum_out=sm.t[:, 8:9]))
                        rstd_from_ssq(sm.t[:, 8:10], sm.t[:, 10:12], 2048, [sm.d], [sm.d])
                        k.op(dve, [A_.d, sm.d, gob.d], [y.d], lambda h, A_=A_: h.scalar_tensor_tensor(
                            out=y.t[:, 0:2048], in0=A_.t[:], scalar=sm.t[:, 10:11], in1=gob.t[:, 0:2048],
                            op0=ALU.mult, op1=ALU.mult))
                        k.op(dve, [fa.d, sm.d, gob.d], [y.d], lambda h: h.scalar_tensor_tensor(
                            out=y.t[:, 2048:4096], in0=fa.t[:], scalar=sm.t[:, 11:12], in1=gob.t[:, 2048:4096],
                            op0=ALU.mult, op1=ALU.mult))
                        for k8 in range(4):
                            p = ptc[k8 % 2]
                            for q8 in range(8):
                                kk = k8 * 8 + q8
                                k.op(pe, [y.d, identb.d], [p.d], lambda h, p=p, q8=q8, kk=kk: h.transpose(
                                    p.t[:, q8, :], y.t[:, kk * 128:(kk + 1) * 128], identb.t[:]), inc=(q8 == 7))
                            k.op(act, [p.d], [actT.d], lambda h, p=p, k8=k8, st=st: h.copy(
                                out=actT.t[:, k8 * 8:(k8 + 1) * 8, st * 128:(st + 1) * 128], in_=p.t[:]))
                k.barrier()

            with contextlib.ExitStack() as sD:
                acc = [sb(f"acc{i}", [128, D], F32, sD) for i in range(4)]
                gates = sb("gates", [128, 4, 65], F32, sD)
                if tb == 0:
                    acc_ds = [k.dsem() for _ in range(4)]
                    pc_ds = [k.dsem() for _ in range(2)]
                    x1_ds = [k.dsem() for _ in range(4)]
                    o_ds = [k.dsem() for _ in range(2)]
                x1d = [Dep() for _ in range(4)]
                if "D" in stages:
                    with contextlib.ExitStack() as sDD:
                        gmp = [sb(f"gmp{i}", [128, 256], F32, sDD) for i in range(2)]
                        tmp = [sb(f"tmpD{i}", [128, 256], F32, sDD) for i in range(2)]
                        hfn = [sb(f"hfn{i}", [128, 512], F32, sDD) for i in range(2)]
                        junk = sb("junkD", [128, D], BF16, sDD)
                        hfrow = junk
                        mp = [sb(f"mp{i}", [128, 512], F32, sDD) for i in range(3)] if sparse else None
                        if tb == 0:
                            hf_ds = k.dsem()
                            hf_keep = [hf_ds]
                            mp_ds = [k.dsem() for _ in range(3)]
                        hf_ds = hf_keep[0]
                        rt = sb("rt", [128, 512], F32, sDD)
                        pmm = [ps(f"pmmD{i}", [128, 512], F32, sDD) for i in range(4)]
                        ptr = [ps(f"ptrD{i}", [128, 4, 128], F32, sDD) for i in range(2)]
                        plg = ps("plg", [128, NE], F32, sDD)
                        for st in range(4):
                            r0 = tb * 512 + st * 128
                            k.dma(sp, acc[st].t[:], x_own[r0:r0 + 128, :], [], [acc[st].d], acc_ds[st])
                        pix = 0
                        for cc in range(16):
                            c0 = cc * 256
                            slot, wv = ring_load(w_out[:, c0:c0 + 256].rearrange("(k p) f -> p k f", p=128), (32, 256))
                            gp = gmp[cc % 2]
                            k.dma(sp, gp.t[:], modrows[0:1, 2 * D + c0:2 * D + c0 + 256].to_broadcast([128, 256]),
                                  [], [gp.d], pc_ds[cc % 2])
                            for st in range(4):
                                pm = pmm[pix % 4]
                                tm = tmp[pix % 2]
                                pix += 1
                                mm_group(pm.t[:, 0:256], [(actT.t[:, kk, st * 128:(st + 1) * 128], wv[:, kk, :])
                                                           for kk in range(32)], pm.d, [slot.d, actT.d])
                                k.op(dve, [pm.d, gp.d], [tm.d], lambda h, pm=pm, tm=tm, gp=gp: h.tensor_tensor(
                                    out=tm.t[:], in0=pm.t[:, 0:256], in1=gp.t[:], op=ALU.mult))
                                k.op(pool, [tm.d, acc[st].d], [acc[st].d], lambda h, tm=tm, st=st, c0=c0: h.tensor_tensor(
                                    out=acc[st].t[:, c0:c0 + 256], in0=acc[st].t[:, c0:c0 + 256], in1=tm.t[:], op=ALU.add))
                        for st in range(4):
                            r0 = tb * 512 + st * 128
                            A_ = acc[st]
                            k.dma(sp, x1_s[r0:r0 + 128, :], A_.t[:], [A_.d], [x1d[st]], x1_ds[st])
                            zero(sm.t[:, 0:1])
                            k.op(act, [A_.d], [junk.d, sm.d], lambda h, A_=A_: h.activation(
                                out=junk.t[:], in_=A_.t[:], func=AF.Square, accum_out=sm.t[:, 0:1]))
                            rstd_from_ssq(sm.t[:, 0:1], sm.t[:, 1:2], D, [sm.d], [sm.d])
                            for k4 in range(8):
                                hb = hfn[k4 % 2]
                                p = ptr[k4 % 2]
                                k.op(dve, [A_.d, sm.d], [hb.d], lambda h, A_=A_, hb=hb, k4=k4: h.tensor_scalar(
                                    out=hb.t[:], in0=A_.t[:, k4 * 512:(k4 + 1) * 512], scalar1=sm.t[:, 1:2], scalar2=None,
                                    op0=ALU.mult))
                                if sparse:
                                    cs_ = slice(k4 * 512, (k4 + 1) * 512)
                                    k.dma(sp, mp[0].t[:], modrows[0:1, 4 * D + k4 * 512:4 * D + (k4 + 1) * 512].to_broadcast([128, 512]), [], [mp[0].d], mp_ds[0])
                                    k.dma(sp, mp[1].t[:], gffn_row[0:1, cs_].to_broadcast([128, 512]), [], [mp[1].d], mp_ds[1])
                                    k.dma(sp, mp[2].t[:], modrows[0:1, 3 * D + k4 * 512:3 * D + (k4 + 1) * 512].to_broadcast([128, 512]), [], [mp[2].d], mp_ds[2])
                                    k.op(dve, [mp[0].d, mp[1].d], [mp[0].d], lambda h: h.scalar_tensor_tensor(
                                        out=mp[0].t[:], in0=mp[0].t[:], scalar=1.0, in1=mp[1].t[:], op0=ALU.add, op1=ALU.mult))
                                    k.op(pool, [mp[0].d, hb.d], [mp[0].d], lambda h, hb=hb: h.tensor_tensor(
                                        out=mp[0].t[:], in0=mp[0].t[:], in1=hb.t[:], op=ALU.mult))
                                    k.op(pool, [mp[0].d, mp[2].d], [hfrow.d], lambda h, cs_=cs_: h.tensor_tensor(
                                        out=hfrow.t[:, cs_], in0=mp[0].t[:], in1=mp[2].t[:], op=ALU.add))
                                for q4 in range(4):
                                    k.op(pe, [hb.d, identf.d], [p.d], lambda h, p=p, q4=q4, hb=hb: h.transpose(
                                        p.t[:, q4, :], hb.t[:, q4 * 128:(q4 + 1) * 128], identf.t[:]), inc=(q4 == 3))
                                for q4 in range(4):
                                    kk = k4 * 4 + q4
                                    k.op(act, [p.d, gmf.d, modfm.d], [actT.d], lambda h, p=p, q4=q4, kk=kk, st=st: h.activation(
                                        out=actT.t[:, kk, st * 128:(st + 1) * 128], in_=p.t[:, q4, :], func=AF.Identity,
                                        scale=gmf.t[:, kk:kk + 1], bias=modfm.t[:, 96 + kk, 0:1]))
                            if sparse:
                                k.dma(sp, hfn_s[r0:r0 + 128, :], hfrow.t[:], [hfrow.d], [], hf_ds)
                            mm_group(plg.t[:], [(actT.t[:, kk, st * 128:(st + 1) * 128], wr_b.t[:, kk, :]) for kk in range(32)],
                                     plg.d, [actT.d, wr_b.d])
                            S_ = rt.t[:, 0:64]
                            C_ = rt.t[:, 64:128]
                            M8 = rt.t[:, 128:192]
                            GS = rt.t[:, 192:200]
                            G8 = rt.t[:, 200:208]
                            GM = rt.t[:, 208:216]
                            CM = rt.t[:, 256:320]
                            T8 = rt.t[:, 320:328]
                            SEL = rt.t[:, 384:448]
                            WS = rt.t[:, 448:450]
                            rd = [rt.d]
                            k.op(act, [plg.d], rd, lambda h: h.activation(out=S_, in_=plg.t[:], func=AF.Sigmoid))
                            k.op(dve, rd + [rb_bc.d], rd, lambda h: h.tensor_tensor(out=C_, in0=S_, in1=rb_bc.t[:], op=ALU.add))
                            for g in range(8):
                                k.op(dve, rd, rd, lambda h, g=g: h.max(out=rt.t[:, 128 + g * 8:136 + g * 8],
                                                                       in_=rt.t[:, 64 + g * 8:72 + g * 8]))
                            m3 = M8.rearrange("p (g e) -> p g e", e=8)
                            k.op(dve, rd, rd, lambda h: h.tensor_tensor(out=GS, in0=m3[:, :, 0], in1=m3[:, :, 1], op=ALU.add))
                            k.op(dve, rd, rd, lambda h: h.max(out=G8, in_=GS))
                            k.op(dve, rd, rd, lambda h: h.tensor_scalar(out=GM, in0=GS, scalar1=rt.t[:, 203:204], scalar2=None,
                                                                        op0=ALU.is_ge))
                            for g in range(8):
                                k.op(dve, rd, rd, lambda h, g=g: h.tensor_scalar(
                                    out=rt.t[:, 256 + g * 8:264 + g * 8], in0=rt.t[:, 64 + g * 8:72 + g * 8], scalar1=10.0,
                                    scalar2=rt.t[:, 208 + g:209 + g], op0=ALU.add, op1=ALU.mult))
                            k.op(dve, rd, rd, lambda h: h.max(out=T8, in_=CM))
                            k.op(dve, rd, rd, lambda h: h.tensor_scalar(out=SEL, in0=CM, scalar1=rt.t[:, 327:328], scalar2=None,
                                                                        op0=ALU.is_ge))
                            k.op(dve, rd, rd, lambda h: h.tensor_tensor(out=SEL, in0=SEL, in1=S_, op=ALU.mult))
                            k.op(dve, rd, rd, lambda h: h.tensor_reduce(out=WS[:, 0:1], in_=SEL, axis=AX.X, op=ALU.add))
                            k.op(dve, rd, rd, lambda h: h.reciprocal(out=WS[:, 1:2], in_=WS[:, 0:1]))
                            k.op(dve, rd, [gates.d], lambda h, st=st: h.tensor_scalar(
                                out=gates.t[:, st, 0:64], in0=SEL, scalar1=rt.t[:, 449:450], scalar2=2.5, op0=ALU.mult, op1=ALU.mult))
                            k.op(dve, [], [gates.d], lambda h, st=st: h.memset(gates.t[:, st, 64:65], 1.0))
                            if sparse:
                                k.op(dve, [gates.d], [gall.d], lambda h, st=st, tb=tb: h.tensor_copy(
                                    out=gall.t[:, tb * 4 + st, :], in_=gates.t[:, st, 0:NE]))
                                k.dma(sp, gts_s[r0:r0 + 128, :], gates.t[:, st, 0:NE], [gates.d], [], hf_ds)
                            if dbg:
                                k.dma(sp, gates_s[r0:r0 + 128, :], gates.t[:, st, :], [gates.d], [], misc_ds)
                    k.barrier()

                if "E" in stages:
                    with contextlib.ExitStack() as sE:
                        actb = [sb(f"actb{i}", [128, 4, 512], BF16, sE) for i in range(2)]
                        slb = sb("slb", [128, 4, 512], BF16, sE)
                        sl_d = [Dep() for _ in range(4)]
                        pg = [ps(f"pg{i}", [128, 512], F32, sE) for i in range(2)]
                        pu = [ps(f"pu{i}", [128, 512], F32, sE) for i in range(2)]
                        pd = [ps(f"pd{i}", [128, 512], F32, sE) for i in range(4)]
                        gi = ui_ = di = 0
                        for e in range(nexp):
                            ee = e if nexp == 65 else (e if e < nexp - 1 else 64)
                            wg = we_gate[ee] if ee < 64 else ws_gate
                            wu = we_up[ee] if ee < 64 else ws_up
                            wd = we_down[ee] if ee < 64 else ws_down
                            ab = actb[e % 2]
                            for half in range(2):
                                c0 = half * 256
                                sG, vG = ring_load(wg[:, c0:c0 + 256].rearrange("(k p) f -> p k f", p=128), (32, 256))
                                sU, vU = ring_load(wu[:, c0:c0 + 256].rearrange("(k p) f -> p k f", p=128), (32, 256))
                                for cl in range(2):
                                    c = half * 2 + cl
                                    p = pg[gi % 2]
                                    gi += 1
                                    mm_group(p.t[:], [(vG[:, kk, cl * 128:(cl + 1) * 128], actT.t[:, kk, :]) for kk in range(32)],
                                             p.d, [sG.d, actT.d])
                                    k.op(act, [p.d], [sl_d[c]], lambda h, p=p, c=c: h.activation(
                                        out=slb.t[:, c, :], in_=p.t[:], func=AF.Silu))
                                for cl in range(2):
                                    c = half * 2 + cl
                                    p = pu[ui_ % 2]
                                    ui_ += 1
                                    mm_group(p.t[:], [(vU[:, kk, cl * 128:(cl + 1) * 128], actT.t[:, kk, :]) for kk in range(32)],
                                             p.d, [sU.d, actT.d])
                                    k.op(dve, [p.d, sl_d[c]], [ab.d], lambda h, p=p, c=c, ab=ab: h.tensor_tensor(
                                        out=ab.t[:, c, :], in0=slb.t[:, c, :], in1=p.t[:], op=ALU.mult))
                            for dh in range(2):
                                d0 = dh * 2048
                                sD_, vD = ring_load(wd[:, d0:d0 + 2048].rearrange("(c p) d -> p c d", p=128), (4, 2048))
                                for st in range(4):
                                    for db in range(4):
                                        p = pd[di % 4]
                                        di += 1
                                        mm_group(p.t[:], [(ab.t[:, c, st * 128:(st + 1) * 128], vD[:, c, db * 512:(db + 1) * 512])
                                                          for c in range(4)], p.d, [sD_.d, ab.d])
                                        oc = d0 + db * 512
                                        if e == 0:
                                            k.op(dve, [p.d, gates.d, x1d[st]], [acc[st].d], lambda h, p=p, st=st, oc=oc, ee=ee: h.tensor_scalar(
                                                out=acc[st].t[:, oc:oc + 512], in0=p.t[:], scalar1=gates.t[:, st, ee:ee + 1], scalar2=None,
                                                op0=ALU.mult))
                                        else:
                                            k.op(dve, [p.d, gates.d], [acc[st].d], lambda h, p=p, st=st, oc=oc, ee=ee: h.scalar_tensor_tensor(
                                                out=acc[st].t[:, oc:oc + 512], in0=p.t[:], scalar=gates.t[:, st, ee:ee + 1],
                                                in1=acc[st].t[:, oc:oc + 512], op0=ALU.mult, op1=ALU.add))
                    k.barrier()

                if "F" in stages:
                    with contextlib.ExitStack() as sF:
                        x1p = [sb(f"x1p{i}", [128, 512], F32, sF) for i in range(2)]
                        gfp = [sb(f"gfp{i}", [128, 512], F32, sF) for i in range(2)]
                        fgp = [sb(f"fgp{i}", [128, 512], F32, sF) for i in range(2)]
                        ob = [sb(f"ob{i}", [128, 512], F32, sF) for i in range(2)]
                        junk = sb("junkF", [128, D], BF16, sF) if not sparse else None
                        if tb == 0:
                            f_ds = [k.dsem() for _ in range(6)]
                        for st in range(4):
                            r0 = tb * 512 + st * 128
                            A_ = acc[st]
                            for pc in range(8):
                                i2 = pc % 2
                                k.dma(sp, x1p[i2].t[:], x1_s[r0:r0 + 128, pc * 512:(pc + 1) * 512], [x1d[st]], [x1p[i2].d], f_ds[i2])
                                k.dma(sp, gfp[i2].t[:], modrows[0:1, 5 * D + pc * 512:5 * D + (pc + 1) * 512].to_broadcast([128, 512]),
                                      [], [gfp[i2].d], f_ds[2 + i2])
                                k.op(dve, [A_.d, gfp[i2].d], [A_.d], lambda h, A_=A_, pc=pc, i2=i2: h.tensor_tensor(
                                    out=A_.t[:, pc * 512:(pc + 1) * 512], in0=A_.t[:, pc * 512:(pc + 1) * 512], in1=gfp[i2].t[:], op=ALU.mult))
                                k.op(pool, [A_.d, x1p[i2].d], [A_.d], lambda h, A_=A_, pc=pc, i2=i2: h.tensor_tensor(
                                    out=A_.t[:, pc * 512:(pc + 1) * 512], in0=A_.t[:, pc * 512:(pc + 1) * 512], in1=x1p[i2].t[:], op=ALU.add))
                            zero(sm.t[:, 12:13])
                            if sparse:
                                k.dma(sp, x1_s[r0:r0 + 128, :], A_.t[:], [A_.d], [x1d[st]], x1_ds[st])
                                continue
                            k.op(act, [A_.d], [junk.d, sm.d], lambda h, A_=A_: h.activation(
                                out=junk.t[:], in_=A_.t[:], func=AF.Square, accum_out=sm.t[:, 12:13]))
                            rstd_from_ssq(sm.t[:, 12:13], sm.t[:, 13:14], D, [sm.d], [sm.d])
                            for pc in range(8):
                                i2 = pc % 2
                                k.dma(sp, fgp[i2].t[:], final_g[0:1, pc * 512:(pc + 1) * 512].to_broadcast([128, 512]),
                                      [], [fgp[i2].d], f_ds[4 + i2])
                                k.op(dve, [A_.d, sm.d, fgp[i2].d], [ob[i2].d], lambda h, A_=A_, pc=pc, i2=i2: h.scalar_tensor_tensor(
                                    out=ob[i2].t[:], in0=A_.t[:, pc * 512:(pc + 1) * 512], scalar=sm.t[:, 13:14], in1=fgp[i2].t[:],
                                    op0=ALU.mult, op1=ALU.mult))
                                k.dma(sp, out[r0:r0 + 128, pc * 512:(pc + 1) * 512], ob[i2].t[:], [ob[i2].d], [], o_ds[i2])
                    k.barrier()
        sBlk.close()
        if sparse and "S" in stages:
            I32_ = mybir.dt.int32
            reg_tos = nc.gpsimd.to_reg(ntiles * TS - 1)
            reg_y = nc.gpsimd.to_reg(16 * NTOK - 1)
            iota_e = cst.t[:, 0:64]
            iota_j = cst.t[:, 256:384]
            pidx = cst.t[:, 160:161]
            base16 = cst.t[:, 192:208]
            with contextlib.ExitStack() as s2:
                selb = sb("selb", [128, 16, NE], BF16, s2)
                trif = sb("trif", [128, 128], F32, s2)
                trib = sb("trib", [128, 128], BF16, s2)
                oneb = sb("oneb", [128, 128], BF16, s2)
                cnt = sb("cnt", [128, NE], F32, s2)
                tl = sb("tl", [128, NE], F32, s2)
                stt = sb("stt", [128, NE + 1], F32, s2)
                wk = sb("wk", [128, 256], F32, s2)
                sI = [sb(f"sI{i}", [128, 8], I32_, s2) for i in range(2)]
                trow = [sb(f"trow{i}", [128, 16], I32_, s2) for i in range(2)]
                tinit = sb("tinit", [128, ntiles * TS // 128, 16], I32_, s2)
                pcn = ps("pcn", [128, NE], F32, s2)
                ppo = [ps(f"ppo{i}", [128, NE], F32, s2) for i in range(2)]
                tosD = Dep()
                sc_ds = [k.dsem() for _ in range(2)]
                k.dma(sp, trif.t[:], tri_in, [], [trif.d], misc_ds)
                k.op(dve, [trif.d], [trib.d], lambda h: h.tensor_copy(out=trib.t[:], in_=trif.t[:]))
                k.op(dve, [], [oneb.d], lambda h: h.memset(oneb.t[:], 1.0))
                k.op(pool, [], [tinit.d], lambda h: h.memset(tinit.t[:], NTOK))
                k.dma(sp, tos_d.rearrange("(a p) c -> p a c", p=128), tinit.t[:], [tinit.d], [tosD], misc_ds)
                k.op(dve, [gall.d], [selb.d], lambda h: h.tensor_scalar(
                    out=selb.t[:], in0=gall.t[:], scalar1=0.0, scalar2=None, op0=ALU.is_gt))
                mm_group(pcn.t[:], [(oneb.t[:], selb.t[:, i, :]) for i in range(16)], pcn.d, [oneb.d, selb.d])
                k.op(dve, [pcn.d], [cnt.d], lambda h: h.tensor_copy(out=cnt.t[:], in_=pcn.t[:]))
                k.op(dve, [cnt.d], [tl.d], lambda h: h.tensor_scalar(out=tl.t[:], in0=cnt.t[:], scalar1=0.0, scalar2=None, op0=ALU.is_gt))
                for jj in range(1, NTOK // TS):
                    k.op(dve, [cnt.d, tl.d], [tl.d], lambda h, jj=jj: h.scalar_tensor_tensor(
                        out=tl.t[:], in0=cnt.t[:], scalar=float(TS * jj), in1=tl.t[:], op0=ALU.is_gt, op1=ALU.add))
                k.op(dve, [], [stt.d], lambda h: h.memset(stt.t[:, 0:1], 0.0))
                for e in range(NE):
                    k.op(dve, [tl.d, stt.d], [stt.d], lambda h, e=e: h.tensor_tensor(
                        out=stt.t[:, e + 1:e + 2], in0=stt.t[:, e:e + 1], in1=tl.t[:, e:e + 1], op=ALU.add))
                k.op(dve, [], [eot.d], lambda h: h.memset(eot.t[:], -1.0))
                for e in range(NE):
                    k.op(dve, [stt.d, cst.d, eot.d], [eot.d], lambda h, e=e: h.scalar_tensor_tensor(
                        out=eot.t[:], in0=iota_j, scalar=stt.t[:, e:e + 1], in1=eot.t[:], op0=ALU.is_ge, op1=ALU.add))
                k.op(dve, [eot.d], [eotk.d], lambda h: h.tensor_scalar(
                    out=eotk.t[:], in0=eot.t[:], scalar1=1024.0, scalar2=None, op0=ALU.mult))
                k.op(dve, [stt.d, cst.d], [wk.d], lambda h: h.tensor_scalar(
                    out=wk.t[:, 0:128], in0=iota_j, scalar1=stt.t[:, NE:NE + 1], scalar2=1.0e7, op0=ALU.is_ge, op1=ALU.mult))
                pass
                for i in range(16):
                    pp = ppo[i % 2]
                    pairs = [(oneb.t[:], selb.t[:, i2, :]) for i2 in range(i)] + [(trib.t[:], selb.t[:, i, :])]
                    mm_group(pp.t[:], pairs, pp.d, [oneb.d, trib.d, selb.d])
                    W = wk
                    k.op(dve, [stt.d, pp.d], [W.d], lambda h, pp=pp: h.scalar_tensor_tensor(
                        out=wk.t[:, 0:64], in0=stt.t[:, 0:NE], scalar=float(TS), in1=pp.t[:], op0=ALU.mult, op1=ALU.add))
                    k.op(dve, [gall.d], [W.d], lambda h, i=i: h.tensor_scalar(
                        out=wk.t[:, 64:128], in0=gall.t[:, i, :], scalar1=0.0, scalar2=None, op0=ALU.is_gt))
                    k.op(dve, [W.d], [W.d], lambda h: h.scalar_tensor_tensor(
                        out=wk.t[:, 128:192], in0=wk.t[:, 0:64], scalar=1.0, in1=wk.t[:, 64:128], op0=ALU.add, op1=ALU.mult))
                    k.op(dve, [W.d], [W.d], lambda h: h.max(out=wk.t[:, 192:200], in_=wk.t[:, 128:192]))
                    k.op(dve, [W.d], [W.d], lambda h: h.tensor_scalar(
                        out=wk.t[:, 200:208], in0=wk.t[:, 192:200], scalar1=-1.0, scalar2=None, op0=ALU.add))
                    si = sI[i % 2]
                    tr = trow[i % 2]
                    k.op(dve, [W.d], [si.d], lambda h, si=si: h.tensor_copy(out=si.t[:], in_=wk.t[:, 200:208]))
                    k.op(dve, [cst.d], [W.d], lambda h, i=i: h.tensor_scalar(
                        out=wk.t[:, 208:224], in0=cst.t[:, 160:161].to_broadcast([128, 16]), scalar1=float(128 * i), scalar2=None, op0=ALU.add))
                    k.op(dve, [W.d], [tr.d], lambda h, tr=tr: h.tensor_copy(out=tr.t[:], in_=wk.t[:, 208:224]))
                    for kk in range(8):
                        k.idma(tos_d[:, :], bass.IndirectOffsetOnAxis(ap=si.t[:, kk:kk + 1], axis=0), tr.t[:, :], None,
                               [si.d, tr.d, tosD], [], sc_ds[i % 2], bounds_check=reg_tos, oob_is_err=False)
            k.barrier()

            with contextlib.ExitStack() as s3:
                gmfk = sb("gmfk", [128, 32], F32, s3)
                shfk = sb("shfk", [128, 32], F32, s3)
                gfk = sb("gfk", [128, 32], F32, s3)
                k.dma(sp, gfk.t[:], gffn_pk, [], [gfk.d], misc_ds)
                k.dma(sp, gmfk.t[:], modrows[0:1, 4 * D:5 * D].rearrange("o (p k) -> (o p) k", k=32), [], [gmfk.d], misc_ds)
                k.dma(sp, shfk.t[:], modrows[0:1, 3 * D:4 * D].rearrange("o (p k) -> (o p) k", k=32), [], [shfk.d], misc_ds)
                fin = (misc_ds, misc_ds.n)
                for b_ in (gfk, gmfk, shfk):
                    b_.d.w = fin
                k.op(dve, [gmfk.d, gfk.d], [gmfk.d], lambda h: h.scalar_tensor_tensor(
                    out=gmfk.t[:], in0=gmfk.t[:], scalar=1.0, in1=gfk.t[:], op0=ALU.add, op1=ALU.mult))
                hg = [sb(f"hg{i}", [128, D], BF16, s3) for i in range(2)]
                grow = [sb(f"grow{i}", [128, NE], F32, s3) for i in range(2)]
                tokI = [sb(f"tokI{i}", [128, 16], I32_, s3) for i in range(2)]
                idxf = sb("idxf", [128, 16], F32, s3)
                idxi = [sb(f"idxi{i}", [128, 16], I32_, s3) for i in range(2)]
                ohg = sb("ohg", [128, 2, NE], F32, s3)
                w3 = sb("w3", [128, 256], F32, s3)
                gsl = [sb(f"gsl{i}", [128, 4], F32, s3) for i in range(2)]
                dsti = [sb(f"dsti{i}", [128, 4, 2], I32_, s3) for i in range(2)]
                actb = [sb(f"actbS{i}", [128, 4, 512], BF16, s3) for i in range(2)]
                slb = sb("slbS", [128, 4, 512], BF16, s3)
                sl_d = [Dep() for _ in range(4)]
                pgu = [ps(f"pgu{i}", [128, 512], F32, s3) for i in range(4)]
                pd = [ps(f"pdS{i}", [128, 512], F32, s3) for i in range(2)]
                ptr = [ps(f"ptrS{i}", [128, 8, 128], BF16, s3) for i in range(2)]
                tk_ds = [k.dsem() for _ in range(2)]
                hg_ds = [k.dsem() for _ in range(2)]
                gr_ds = [k.dsem() for _ in range(2)]
                wg2 = we_gate.rearrange("e (p k) f -> (e p) (k f)", k=32).rearrange("r (q c) -> (r q) c", c=2048)
                wu2 = we_up.rearrange("e (p k) f -> (e p) (k f)", k=32).rearrange("r (q c) -> (r q) c", c=2048)
                wd2 = we_down.rearrange("e f (h c) -> (e f h) c", c=2048)

                def ring_gather(src2, cols):
                    i = ring_i[0] % NR
                    ring_i[0] += 1
                    for q, c in enumerate(cols):
                        k.idma(ring[i].t[:, q * 2048:(q + 1) * 2048], None, src2,
                               bass.IndirectOffsetOnAxis(ap=c, axis=0), [ixd], [ring[i].d], ring_ds[i])
                    ring[i].d.w = (ring_ds[i], ring_ds[i].n)
                    return ring[i]

                ring.extend([sb(f"ringx{i}", [128, 4096 * 2], BF16, s3) for i in range(2)])
                ring_ds.extend([k.dsem() for _ in range(2)])
                NR3 = 6
                ring_i[0] = 0
                ohgs = [ohg, sb("ohg2", [128, 2, NE], F32, s3)]
                reg_w = nc.gpsimd.to_reg(NE * 1024 - 1)
                ybq = [sb(f"ybq{i}", [128, 2048], F32, s3) for i in range(4)]
                ybq_ds = [k.dsem() for _ in range(4)]
                y2 = y_s.rearrange("r (h c) -> (r h) c", h=2)
                cnts = {"ui": 0, "di": 0, "yi": 0}

                def ring_gather2(src2, cols, ixd):
                    i = ring_i[0] % NR3
                    ring_i[0] += 1
                    rd = ring[i].d
                    k.need(pool, rd.w)
                    for o, c_ in rd.r.items():
                        k.need(pool, (o, c_))
                    for q, c in enumerate(cols):
                        k.idma(ring[i].t[:, q * 2048:(q + 1) * 2048], None, src2,
                               bass.IndirectOffsetOnAxis(ap=c, axis=0), [ixd], [], ring_ds[i])
                    rd.w = (ring_ds[i], ring_ds[i].n)
                    rd.r = {}
                    return ring[i]

                def prep(j):
                    ix = idxi[j % 2]
                    og = ohgs[j % 2]
                    k.op(dve, [eotk.d, cst.d], [idxf.d], lambda h: h.tensor_scalar(
                        out=idxf.t[:], in0=base16, scalar1=eotk.t[:, j:j + 1], scalar2=None, op0=ALU.add))
                    k.op(dve, [idxf.d], [ix.d], lambda h: h.tensor_copy(out=ix.t[:], in_=idxf.t[:]))
                    k.op(dve, [eot.d, cst.d], [og.d], lambda h: h.tensor_scalar(
                        out=og.t[:, 0, :], in0=iota_e, scalar1=eot.t[:, j:j + 1], scalar2=None, op0=ALU.is_equal))
                    k.op(dve, [eot.d, cst.d], [og.d], lambda h: h.tensor_scalar(
                        out=og.t[:, 1, :], in0=iota_e, scalar1=eot.t[:, j:j + 1], scalar2=None, op0=ALU.is_gt))
                    gs_, dsi = gsl[j % 2], dsti[j % 2]
                    for st in range(TS // 128):
                        u2 = cnts["ui"] % 2
                        cnts["ui"] += 1
                        s0 = j * TS + st * 128
                        T, H, G = tokI[u2], hg[u2], grow[u2]
                        k.dma(sp, T.t[:], tos_d[s0:s0 + 128, :], [], [T.d], tk_ds[u2])
                        k.idma(H.t[:], None, hfn_s[:, :], bass.IndirectOffsetOnAxis(ap=T.t[:, 0:1], axis=0), [T.d], [H.d], hg_ds[u2])
                        k.idma(G.t[:], None, gts_s[:, :], bass.IndirectOffsetOnAxis(ap=T.t[:, 0:1], axis=0), [T.d], [G.d], gr_ds[u2])
                        wd_ = [w3.d]
                        k.op(dve, [G.d, og.d], wd_, lambda h, G=G: h.tensor_tensor(
                            out=w3.t[:, 0:64], in0=G.t[:], in1=og.t[:, 0, :], op=ALU.mult))
                        k.op(dve, wd_, [gs_.d], lambda h, st=st: h.tensor_reduce(
                            out=gs_.t[:, st:st + 1], in_=w3.t[:, 0:64], axis=AX.X, op=ALU.add))
                        k.op(dve, [G.d], wd_, lambda h, G=G: h.tensor_scalar(
                            out=w3.t[:, 64:128], in0=G.t[:], scalar1=0.0, scalar2=None, op0=ALU.is_gt))
                        k.op(dve, wd_ + [og.d], wd_, lambda h: h.tensor_tensor(
                            out=w3.t[:, 64:128], in0=w3.t[:, 64:128], in1=og.t[:, 1, :], op=ALU.mult))
                        k.op(dve, wd_, wd_, lambda h: h.tensor_reduce(
                            out=w3.t[:, 128:129], in_=w3.t[:, 64:128], axis=AX.X, op=ALU.add))
                        k.op(dve, [T.d], wd_, lambda h, T=T: h.tensor_copy(out=w3.t[:, 129:130], in_=T.t[:, 0:1]))
                        k.op(dve, wd_, wd_, lambda h: h.tensor_scalar(
                            out=w3.t[:, 130:131], in0=w3.t[:, 129:130], scalar1=float(NTOK), scalar2=1.0e6, op0=ALU.is_ge, op1=ALU.mult))
                        k.op(dve, wd_, wd_, lambda h: h.scalar_tensor_tensor(
                            out=w3.t[:, 131:132], in0=w3.t[:, 128:129], scalar=float(NTOK), in1=w3.t[:, 129:130], op0=ALU.mult, op1=ALU.add))
                        k.op(dve, wd_, wd_, lambda h: h.tensor_tensor(
                            out=w3.t[:, 131:132], in0=w3.t[:, 131:132], in1=w3.t[:, 130:131], op=ALU.add))
                        k.op(dve, wd_, wd_, lambda h: h.tensor_scalar(
                            out=w3.t[:, 132:133], in0=w3.t[:, 131:132], scalar1=2.0, scalar2=None, op0=ALU.mult))
                        k.op(dve, wd_, wd_, lambda h: h.tensor_scalar(
                            out=w3.t[:, 133:134], in0=w3.t[:, 131:132], scalar1=2.0, scalar2=1.0, op0=ALU.mult, op1=ALU.add))
                        k.op(dve, wd_, [dsi.d], lambda h, st=st: h.tensor_copy(out=dsi.t[:, st, :], in_=w3.t[:, 132:134]))
                        hv = H.t[:].rearrange("t (p k) -> t k p", k=32)
                        for k8 in range(4):
                            p = ptr[k8 % 2]
                            for q8 in range(8):
                                kk = k8 * 8 + q8
                                k.op(pe, [H.d, identb.d], [p.d], lambda h, p=p, q8=q8, kk=kk, hv=hv: h.transpose(
                                    p.t[:, q8, :], hv[:, kk, :], identb.t[:]), inc=(q8 == 7))
                            if k8 % 2 == 0:
                                k.op(act, [p.d], [actT.d], lambda h, p=p, k8=k8, st=st: h.copy(
                                    out=actT.t[:, k8 * 8:(k8 + 1) * 8, st * 128:(st + 1) * 128], in_=p.t[:]))
                            else:
                                k.op(dve, [p.d], [actT.d], lambda h, p=p, k8=k8, st=st: h.tensor_copy(
                                    out=actT.t[:, k8 * 8:(k8 + 1) * 8, st * 128:(st + 1) * 128], in_=p.t[:]))

                def weights_gu(j):
                    ix = idxi[j % 2]
                    gsl_ = [ring_gather2(wg2, [ix.t[:, kh * 4 + q:kh * 4 + q + 1] for q in range(4)], ix.d) for kh in range(2)]
                    usl_ = [ring_gather2(wu2, [ix.t[:, kh * 4 + q:kh * 4 + q + 1] for q in range(4)], ix.d) for kh in range(2)]
                    return gsl_, usl_

                def weights_d(j):
                    ix = idxi[j % 2]
                    return [ring_gather2(wd2, [ix.t[:, 8 + c * 2 + dh:8 + c * 2 + dh + 1] for c in range(4)], ix.d) for dh in range(2)]

                def compute_gu(j, gsl_, usl_):
                    ab = actb[j % 2]
                    for which, slots in ((0, gsl_), (1, usl_)):
                        for c in range(4):
                            pairs = []
                            for kh in range(2):
                                wv = slots[kh].t[:, 0:8192].rearrange("p (a b) -> p a b", a=16)
                                pairs += [(wv[:, kk, c * 128:(c + 1) * 128], actT.t[:, kh * 16 + kk, 0:TS]) for kk in range(16)]
                            mm_group(pgu[c].t[:, 0:TS], pairs, pgu[c].d, [slots[0].d, slots[1].d, actT.d])
                            if which == 0:
                                k.op(act, [pgu[c].d], [sl_d[c]], lambda h, c=c: h.activation(
                                    out=slb.t[:, c, 0:TS], in_=pgu[c].t[:, 0:TS], func=AF.Silu))
                            else:
                                k.op(dve, [pgu[c].d, sl_d[c]], [ab.d], lambda h, c=c, ab=ab: h.tensor_tensor(
                                    out=ab.t[:, c, 0:TS], in0=slb.t[:, c, 0:TS], in1=pgu[c].t[:, 0:TS], op=ALU.mult))

                def compute_d(j, dsl_):
                    ab = actb[j % 2]
                    gs_, dsi = gsl[j % 2], dsti[j % 2]
                    for dh in range(2):
                        sD_ = dsl_[dh]
                        vD = sD_.t[:, 0:8192].rearrange("p (a b) -> p a b", a=4)
                        for st in range(TS // 128):
                            yi = cnts["yi"] % 4
                            cnts["yi"] += 1
                            Yb = ybq[yi]
                            for db in range(4):
                                p = pd[cnts["di"] % 2]
                                cnts["di"] += 1
                                mm_group(p.t[:], [(ab.t[:, c, st * 128:(st + 1) * 128], vD[:, c, db * 512:(db + 1) * 512])
                                                  for c in range(4)], p.d, [sD_.d, ab.d])
                                k.op(dve, [p.d, gs_.d], [Yb.d], lambda h, p=p, st=st, db=db, Yb=Yb: h.tensor_scalar(
                                    out=Yb.t[:, db * 512:(db + 1) * 512], in0=p.t[:], scalar1=gs_.t[:, st:st + 1], scalar2=None, op0=ALU.mult))
                            k.idma(y2[:, :], bass.IndirectOffsetOnAxis(ap=dsi.t[:, st, dh:dh + 1], axis=0), Yb.t[:, :], None,
                                   [Yb.d, dsi.d], [], ybq_ds[yi], bounds_check=reg_y, oob_is_err=False)

                prep(0)
                gsl_, usl_ = weights_gu(0)
                dsl_ = weights_d(0)
                for j in range(ntiles):
                    compute_gu(j, gsl_, usl_)
                    if j + 1 < ntiles:
                        prep(j + 1)
                        gsl_, usl_ = weights_gu(j + 1)
                    compute_d(j, dsl_)
                    if j + 1 < ntiles:
                        dsl_ = weights_d(j + 1)
            k.barrier()

            with contextlib.ExitStack() as s4:
                NB4 = 3
                yp = [sb(f"yp{i}", [128, 8, 512], F32, s4) for i in range(NB4)]
                xsb = [sb(f"xsb{i}", [128, 512], F32, s4) for i in range(NB4)]
                gfp = [sb(f"gfpS{i}", [128, 512], F32, s4) for i in range(NB4)]
                fgp = [sb(f"fgpS{i}", [128, 512], F32, s4) for i in range(2)]
                ob = [sb(f"obS{i}", [128, 512], F32, s4) for i in range(2)]
                x2 = sb("x2", [128, D], F32, s4)
                junk = sb("junkS", [128, D], BF16, s4)
                p_ds = [k.dsem() for _ in range(8)]
                py_ds = [k.dsem() for _ in range(3 * NB4)]
                o_ds2 = [k.dsem() for _ in range(2)]
                yv = y_s.rearrange("(k t) d -> t k d", k=8)
                for i in range(16):
                    r0 = i * 128
                    for pc in range(8):
                        i2 = pc % 2
                        i4 = (i * 8 + pc) % NB4
                        Y, X, Gp = yp[i4], xsb[i4], gfp[i4]
                        k.dma(sp, Y.t[:], yv[r0:r0 + 128, :, pc * 512:(pc + 1) * 512], [], [Y.d], py_ds[i4])
                        k.dma(sp, X.t[:], x1_s[r0:r0 + 128, pc * 512:(pc + 1) * 512], [], [X.d], py_ds[NB4 + i4])
                        k.dma(sp, Gp.t[:], modrows[0:1, 5 * D + pc * 512:5 * D + (pc + 1) * 512].to_broadcast([128, 512]),
                              [], [Gp.d], py_ds[2 * NB4 + i4])
                        for lvl in (4, 2, 1):
                            eng = pool if lvl == 2 else dve
                            k.op(eng, [Y.d], [Y.d], lambda h, Y=Y, lvl=lvl: h.tensor_tensor(
                                out=Y.t[:, 0:lvl, :], in0=Y.t[:, 0:lvl, :], in1=Y.t[:, lvl:2 * lvl, :], op=ALU.add))
                        k.op(dve, [Y.d, Gp.d], [Y.d], lambda h, Y=Y, Gp=Gp: h.tensor_tensor(
                            out=Y.t[:, 0, :], in0=Y.t[:, 0, :], in1=Gp.t[:], op=ALU.mult))
                        k.op(pool, [Y.d, X.d], [x2.d], lambda h, Y=Y, X=X, pc=pc: h.tensor_tensor(
                            out=x2.t[:, pc * 512:(pc + 1) * 512], in0=Y.t[:, 0, :], in1=X.t[:], op=ALU.add))
                    zero(sm.t[:, 12:13])
                    k.op(act, [x2.d], [junk.d, sm.d], lambda h: h.activation(
                        out=junk.t[:], in_=x2.t[:], func=AF.Square, accum_out=sm.t[:, 12:13]))
                    rstd_from_ssq(sm.t[:, 12:13], sm.t[:, 13:14], D, [sm.d], [sm.d])
                    for pc in range(8):
                        i2 = pc % 2
                        k.dma(sp, fgp[i2].t[:], final_g[0:1, pc * 512:(pc + 1) * 512].to_broadcast([128, 512]),
                              [], [fgp[i2].d], p_ds[6 + i2])
                        k.op(dve, [x2.d, sm.d, fgp[i2].d], [ob[i2].d], lambda h, pc=pc, i2=i2: h.scalar_tensor_tensor(
                            out=ob[i2].t[:], in0=x2.t[:, pc * 512:(pc + 1) * 512], scalar=sm.t[:, 13:14], in1=fgp[i2].t[:],
                            op0=ALU.mult, op1=ALU.mult))
                        k.dma(sp, out[r0:r0 + 128, pc * 512:(pc + 1) * 512], ob[i2].t[:], [ob[i2].d], [], o_ds2[i2])
        k.barrier()
    return nc


def _tables(na_rpb, half):
    def pair_rows(pp):
        if 2 <= pp <= 17:
            r = 32 * half + 2 * (pp - 2)
            return (r, r + 1)
        if half == 0:
            return {0: (6, 7), 1: None, 18: (32, 33), 19: (34, 35)}[pp]
        return {0: (28, 29), 1: (30, 31), 18: None, 19: (56, 57)}[pp]
    rep = {0: 5, 1: 0, 2: 1, 3: 14, 4: 15}
    ro = np.zeros((5, 5, 128, 128), np.int64)
    co = np.zeros((5, 5, 128, 128), np.int64)
    mk = np.zeros((5, 5, 128, 128), np.float32)
    kk = np.arange(128)
    q = np.arange(128)
    for cls, j in rep.items():
        qrow = 32 * half + 2 * j + q // 64
        qcol = q % 64
        r0 = np.clip(qrow - 4, 0, 56)
        c0 = np.clip(qcol - 8, 0, 48)
        for bl in range(5):
            rows = pair_rows(j + bl)
            if rows is None:
                continue
            krow = np.array(rows)[kk // 64]
            kcol = kk % 64
            valid = ((krow[:, None] >= r0[None, :]) & (krow[:, None] < r0[None, :] + 8) &
                     (kcol[:, None] >= c0[None, :]) & (kcol[:, None] < c0[None, :] + 16))
            mk[cls, bl] = valid.astype(np.float32)
            ro[cls, bl] = np.clip(krow[:, None] - qrow[None, :] + 7, 0, 14)
            co[cls, bl] = np.clip(kcol[:, None] - qcol[None, :] + 15, 0, 30)
    bt = na_rpb[:, ro, co]
    bt = np.ascontiguousarray(bt.transpose(0, 3, 1, 2, 4))
    mt = np.ascontiguousarray(mk.transpose(2, 0, 1, 3))
    return bt, mt


def _consts():
    c = np.zeros((128, 512), np.float32)
    p = np.arange(128, dtype=np.float32)
    c[:, 0:64] = np.arange(64, dtype=np.float32)[None, :]
    c[:, 64:160] = np.arange(96, dtype=np.float32)[None, :]
    c[:, 256:384] = np.arange(128, dtype=np.float32)[None, :]
    c[:, 160] = p
    for m in range(8):
        c[:, 192 + m] = 8 * p + m
    for cc in range(4):
        for h in range(2):
            c[:, 200 + cc * 2 + h] = (cc * 128 + p) * 2 + h
    return c


def make_in_maps(inp):
    f = lambda a: np.ascontiguousarray(np.asarray(a, dtype=np.float32))
    x = f(inp["x"]); ctx = f(inp["ctx"]); c = f(inp["c"]); c_ctx = f(inp["c_ctx"])
    shared = {
        "w_ada": f(inp["w_ada"][0]),
        "b_ada2": f(np.broadcast_to(f(inp["b_ada"][0])[None, :], (2, 6 * D))),
        "gmix_fm": f(f(inp["norm_mix_g"][0]).reshape(32, 128).T),
        "gffn_fm": f(f(inp["norm_ffn_g"][0]).reshape(32, 128).T),
        "w_in": f(inp["w_in"][0]),
        "ln_g": f(inp["sg_ln_g"][0])[None, :], "ln_b": f(inp["sg_ln_b"][0])[None, :],
        "wsT": f(f(inp["sg_w_s"][0]).transpose(2, 0, 1)),
        "bsT": f(f(inp["sg_b_s"][0]).T),
        "gout": f(np.concatenate([f(inp["out_g_na"][0]), f(inp["out_g_sg"][0])]))[None, :],
        "w_out": f(inp["w_out"][0]),
        "w_router": f(inp["w_router"][0]), "rbias": f(inp["router_bias"][0])[None, :],
        "we_gate": f(inp["we_gate"][0]), "we_up": f(inp["we_up"][0]), "we_down": f(inp["we_down"][0]),
        "ws_gate": f(inp["ws_gate"][0]), "ws_up": f(inp["ws_up"][0]), "ws_down": f(inp["ws_down"][0]),
        "final_g": f(inp["final_g"])[None, :],
        "gffn_pk": f(f(inp["norm_ffn_g"][0]).reshape(128, 32)),
        "gffn_row": f(inp["norm_ffn_g"][0])[None, :],
        "consts": _consts(), "tri_in": f(np.triu(np.ones((128, 128), np.float32), 1)),
    }
    rpb = f(inp["na_rpb"][0])
    tabs = [_tables(rpb, h) for h in range(2)]
    maps = []
    for core in range(8):
        b, half = core // 2, core % 2
        xb = x[b]
        if half == 0:
            prs = [(6, 7), (6, 7), (32, 33), (34, 35)]
        else:
            prs = [(28, 29), (30, 31), (56, 57), (56, 57)]
        halo = np.concatenate([xb[r * 64:(r + 1) * 64] for pr in prs for r in pr], axis=0)
        m = dict(shared)
        m["x_own"] = f(xb[half * NTOK:(half + 1) * NTOK])
        m["x_hc"] = f(np.concatenate([halo, ctx[b]], axis=0))
        cc = np.stack([c[b], c_ctx], axis=-1)
        m["cT"] = f(cc.reshape(32, 128, 2).transpose(1, 0, 2))
        m["bias_tab"], m["mask_tab"] = tabs[half]
        maps.append(m)
    return maps


_NC_CACHE = {}


def kernel(**inputs):
    maps = make_in_maps(inputs)
    if "nc" not in _NC_CACHE:
        _NC_CACHE["nc"] = build_nc()
    res = run_bass_kernel_spmd(_NC_CACHE["nc"], maps, core_ids=list(range(8)))
    outs = [np.asarray(r["out"], dtype=np.float32).reshape(NTOK, D) for r in res.results]
    full = np.empty((4, 4096, D), np.float32)
    for core in range(8):
        b, half = core // 2, core % 2
        full[b, half * NTOK:(half + 1) * NTOK] = outs[core]
    return full
```
